# Optimizing a Trainium2 kernel written in Bass

```python
import jax, jax.numpy as jnp
from jax import lax
import numpy as np

D_MODEL = 1024
BATCH = 8
SEQ = 4096
DEPTH = 4

N_MIXERS = 2
N_MLSTM_LAYERS = (DEPTH + 1) // 2
N_CONV_LAYERS = DEPTH // 2
MLSTM_INNER = 2 * D_MODEL
MLSTM_HEADS = 4
MLSTM_HEAD_DIM = MLSTM_INNER // MLSTM_HEADS
QKV_BLOCK = 4
MLSTM_CONV = 4
CHUNK = 64
CONF_KERNEL = 31
D_FF = 2816
FFN_CONV = 3
EPS = 1e-6

kernel_name = "hybrid_mlstm_conformer_convffn_adaln"


def rmsnorm(x, g):
    xf = x.astype(jnp.float32)
    y = xf * lax.rsqrt(jnp.mean(xf * xf, axis=-1, keepdims=True) + EPS)
    return (y * g).astype(x.dtype)


def layernorm(x, g, b):
    xf = x.astype(jnp.float32)
    mu = jnp.mean(xf, axis=-1, keepdims=True)
    var = jnp.mean(jnp.square(xf - mu), axis=-1, keepdims=True)
    return ((xf - mu) * lax.rsqrt(var + EPS) * g + b).astype(x.dtype)


def modulate(h, shift, scale):
    return h * (1.0 + scale[:, None, :]) + shift[:, None, :]


def causal_dwconv(x, w, b):
    K, C = w.shape
    y = lax.conv_general_dilated(
        x, w[:, None, :].astype(x.dtype), window_strides=(1,), padding=[(K - 1, 0)],
        dimension_numbers=("NWC", "WIO", "NWC"), feature_group_count=C)
    return y + b


def blockdiag(x, w):
    B, S, I = x.shape
    xb = x.reshape(B, S, I // QKV_BLOCK, QKV_BLOCK)
    return jnp.einsum("bsni,nio->bsno", xb, w).reshape(B, S, I)


def mlstm_chunkwise(q, k, v, i_pre, f_pre):
    B, H, S, dh = q.shape
    nc = S // CHUNK
    k = k * (dh ** -0.5)
    logf = jax.nn.log_sigmoid(f_pre)

    def chunked(t):
        return jnp.moveaxis(t.reshape(B, H, nc, CHUNK, *t.shape[3:]), 2, 0)

    causal = jnp.tril(jnp.ones((CHUNK, CHUNK), dtype=bool))

    def step(carry, inp):
        C, n, m = carry
        q_, k_, v_, i_, lf = inp
        g = jnp.cumsum(lf, axis=-1)
        G = g[..., -1]
        D = g[..., :, None] - g[..., None, :] + i_[..., None, :]
        D = jnp.where(causal, D, -jnp.inf)
        inter = m[..., None] + g
        m_row = jnp.maximum(inter, jnp.max(D, axis=-1))
        w_intra = jnp.exp(D - m_row[..., None])
        w_inter = jnp.exp(inter - m_row)
        s = jnp.einsum("bhld,bhsd->bhls", q_, k_) * w_intra
        num = (w_inter[..., None] * jnp.einsum("bhvk,bhlk->bhlv", C, q_)
               + jnp.einsum("bhls,bhsv->bhlv", s, v_))
        den = w_inter * jnp.einsum("bhk,bhlk->bhl", n, q_) + jnp.sum(s, axis=-1)
        h = num / jnp.maximum(jnp.abs(den), jnp.exp(-m_row))[..., None]
        decay = G[..., None] - g + i_
        m_new = jnp.maximum(m + G, jnp.max(decay, axis=-1))
        w_s = jnp.exp(decay - m_new[..., None])
        w_c = jnp.exp(m + G - m_new)
        C_new = w_c[..., None, None] * C + jnp.einsum("bhs,bhsv,bhsk->bhvk", w_s, v_, k_)
        n_new = w_c[..., None] * n + jnp.einsum("bhs,bhsk->bhk", w_s, k_)
        return (C_new, n_new, m_new), h

    init = (jnp.zeros((B, H, dh, dh), jnp.float32),
            jnp.zeros((B, H, dh), jnp.float32),
            jnp.zeros((B, H), jnp.float32))
    _, hs = lax.scan(step, init, (chunked(q), chunked(k), chunked(v), chunked(i_pre), chunked(logf)))
    hs = jnp.moveaxis(hs, 0, 2).reshape(B, H, S, dh)
    return hs.astype(v.dtype)


def mlstm_block(h, w_up, conv_w, conv_b, wq, wk, wv, w_if, b_if, ln_w, skip, w_down):
    B, S, _ = h.shape
    H, dh = MLSTM_HEADS, MLSTM_HEAD_DIM
    up = h @ w_up
    xm, z = jnp.split(up, 2, axis=-1)
    xc = jax.nn.silu(causal_dwconv(xm, conv_w, conv_b))
    q = blockdiag(xc, wq)
    k = blockdiag(xc, wk)
    v = blockdiag(xm, wv)
    gates = (jnp.concatenate([q, k, v], axis=-1) @ w_if + b_if).astype(jnp.float32)
    i_pre = jnp.transpose(gates[..., :H], (0, 2, 1))
    f_pre = jnp.transpose(gates[..., H:], (0, 2, 1))

    def heads(t):
        return jnp.transpose(t.reshape(B, S, H, dh), (0, 2, 1, 3))

    hc = mlstm_chunkwise(heads(q), heads(k), heads(v), i_pre, f_pre)
    hf = hc.astype(jnp.float32)
    mu = jnp.mean(hf, axis=-1, keepdims=True)
    var = jnp.mean(jnp.square(hf - mu), axis=-1, keepdims=True)
    hn = ((hf - mu) * lax.rsqrt(var + EPS)).astype(h.dtype)
    hn = jnp.transpose(hn, (0, 2, 1, 3)).reshape(B, S, MLSTM_INNER) * ln_w
    out = (hn + skip * xc) * jax.nn.silu(z)
    return out @ w_down


def conformer_conv_block(h, w_pw1, b_pw1, dw_w, dw_b, ln_g, ln_b, w_pw2, b_pw2):
    a, gte = jnp.split(h @ w_pw1 + b_pw1, 2, axis=-1)
    u = a * jax.nn.sigmoid(gte)
    u = causal_dwconv(u, dw_w, dw_b)
    u = jax.nn.silu(layernorm(u, ln_g, ln_b))
    return u @ w_pw2 + b_pw2


def conv_ffn(h, w_up, conv_w, conv_b, w_down):
    u = causal_dwconv(h @ w_up, conv_w, conv_b)
    gate, val = jnp.split(u, 2, axis=-1)
    return (jax.nn.silu(gate) * val) @ w_down


def setup_inputs(seed: int = 0) -> dict:
    key = jax.random.key(seed)
    ks = iter(jax.random.split(key, 40))
    D, I, H, F = D_MODEL, MLSTM_INNER, MLSTM_HEADS, D_FF
    NA, NB = N_MLSTM_LAYERS, N_CONV_LAYERS

    def nrm(shape, scale):
        return jax.random.normal(next(ks), shape, jnp.float32) * scale

    def gain(shape):
        return 1.0 + nrm(shape, 0.02)

    f_bias = jnp.broadcast_to(jnp.linspace(3.0, 6.0, H, dtype=jnp.float32), (NA, H))
    b_if = jnp.concatenate([nrm((NA, H), 0.1), f_bias + nrm((NA, H), 0.1)], axis=-1)
    return {
        "x": nrm((BATCH, SEQ, D), 1.0),
        "c": nrm((BATCH, D), 1.0),
        "ada_w": nrm((DEPTH, D, 6 * D), D ** -0.5),
        "ada_b": nrm((DEPTH, 6 * D), 0.02),
        "norm_mix_g": gain((DEPTH, D)),
        "norm_ffn_g": gain((DEPTH, D)),
        "final_g": gain((D,)),
        "mlstm_w_up": nrm((NA, D, 2 * I), D ** -0.5),
        "mlstm_conv_w": nrm((NA, MLSTM_CONV, I), MLSTM_CONV ** -0.5),
        "mlstm_conv_b": nrm((NA, I), 0.02),
        "mlstm_wq": nrm((NA, I // QKV_BLOCK, QKV_BLOCK, QKV_BLOCK), QKV_BLOCK ** -0.5),
        "mlstm_wk": nrm((NA, I // QKV_BLOCK, QKV_BLOCK, QKV_BLOCK), QKV_BLOCK ** -0.5),
        "mlstm_wv": nrm((NA, I // QKV_BLOCK, QKV_BLOCK, QKV_BLOCK), QKV_BLOCK ** -0.5),
        "mlstm_w_if": nrm((NA, 3 * I, 2 * H), (3 * I) ** -0.5),
        "mlstm_b_if": b_if,
        "mlstm_ln_w": gain((NA, I)),
        "mlstm_skip": gain((NA, I)),
        "mlstm_w_down": nrm((NA, I, D), I ** -0.5),
        "conf_w_pw1": nrm((NB, D, 2 * D), D ** -0.5),
        "conf_b_pw1": nrm((NB, 2 * D), 0.02),
        "conf_dw_w": nrm((NB, CONF_KERNEL, D), CONF_KERNEL ** -0.5),
        "conf_dw_b": nrm((NB, D), 0.02),
        "conf_ln_g": gain((NB, D)),
        "conf_ln_b": nrm((NB, D), 0.02),
        "conf_w_pw2": nrm((NB, D, D), D ** -0.5),
        "conf_b_pw2": nrm((NB, D), 0.02),
        "ffn_w_up": nrm((DEPTH, D, 2 * F), D ** -0.5),
        "ffn_conv_w": nrm((DEPTH, FFN_CONV, 2 * F), FFN_CONV ** -0.5),
        "ffn_conv_b": nrm((DEPTH, 2 * F), 0.02),
        "ffn_w_down": nrm((DEPTH, F, D), F ** -0.5),
    }


def reference(x, c, ada_w, ada_b, norm_mix_g, norm_ffn_g, final_g,
              mlstm_w_up, mlstm_conv_w, mlstm_conv_b, mlstm_wq, mlstm_wk, mlstm_wv,
              mlstm_w_if, mlstm_b_if, mlstm_ln_w, mlstm_skip, mlstm_w_down,
              conf_w_pw1, conf_b_pw1, conf_dw_w, conf_dw_b, conf_ln_g, conf_ln_b,
              conf_w_pw2, conf_b_pw2,
              ffn_w_up, ffn_conv_w, ffn_conv_b, ffn_w_down):
    c_act = jax.nn.silu(c)
    for layer in range(DEPTH):
        mod = c_act @ ada_w[layer] + ada_b[layer]
        sh1, sc1, g1, sh2, sc2, g2 = jnp.split(mod, 6, axis=-1)
        h = modulate(rmsnorm(x, norm_mix_g[layer]), sh1, sc1)
        j = layer // N_MIXERS
        if layer % N_MIXERS == 0:
            y = mlstm_block(h, mlstm_w_up[j], mlstm_conv_w[j], mlstm_conv_b[j],
                            mlstm_wq[j], mlstm_wk[j], mlstm_wv[j], mlstm_w_if[j], mlstm_b_if[j],
                            mlstm_ln_w[j], mlstm_skip[j], mlstm_w_down[j])
        else:
            y = conformer_conv_block(h, conf_w_pw1[j], conf_b_pw1[j], conf_dw_w[j], conf_dw_b[j],
                                     conf_ln_g[j], conf_ln_b[j], conf_w_pw2[j], conf_b_pw2[j])
        x = x + g1[:, None, :] * y
        h = modulate(rmsnorm(x, norm_ffn_g[layer]), sh2, sc2)
        x = x + g2[:, None, :] * conv_ffn(h, ffn_w_up[layer], ffn_conv_w[layer],
                                          ffn_conv_b[layer], ffn_w_down[layer])
    return rmsnorm(x, final_g)
```

```python
import collections
import numpy as np
import concourse.bass as bass
import concourse.mybir as mybir
from concourse.bass_utils import run_bass_kernel_spmd

F32 = mybir.dt.float32
BF16 = mybir.dt.bfloat16
AF = mybir.ActivationFunctionType
ALU = mybir.AluOpType
AX = mybir.AxisListType

D = 1024
DC = 8
S_FULL = 4096
T = 512
INNER = 2048
IC = 16
NH = 4
DFF = 2816
FC = 22
CONF_K = 31
EPS = 1e-6
DEPTH = 4
SLOT = 2048
NRING = 8
HB = 8
HF = 32


class Dep:
    __slots__ = ("name", "last_w", "readers")

    def __init__(self, name):
        self.name = name
        self.last_w = None
        self.readers = []


class View:
    __slots__ = ("ap", "deps", "tile")

    def __init__(self, ap, deps, tile=None):
        self.ap = ap
        self.deps = deps
        self.tile = tile


class Tile:
    def __init__(self, name, tensor, nparts=1):
        self.name = name
        self.t = tensor
        self.nparts = nparts
        self.deps = [Dep(f"{name}.{i}") for i in range(nparts)]
        self.dsem = None

    def __getitem__(self, key):
        ap = self.t[key]
        if self.nparts == 1:
            return View(ap, self.deps, self)
        k = key[1] if isinstance(key, tuple) and len(key) > 1 else slice(None)
        if isinstance(k, int):
            d = [self.deps[k]]
        else:
            d = list(self.deps[k])
        return View(ap, d, self)

    def v(self, ap, parts=None):
        return View(ap, self.deps if parts is None else [self.deps[p] for p in parts], self)


class Op:
    __slots__ = ("eng", "fn", "deps", "signal", "sigval", "dsem", "dval", "eidx")

    def __init__(self, eng, fn, dsem=None):
        self.eng = eng
        self.fn = fn
        self.deps = []
        self.signal = False
        self.sigval = None
        self.dsem = dsem
        self.dval = None


ENGS = ("pe", "act", "dve", "pool", "sp")


class Prog:
    def __init__(self, nc):
        self.nc = nc
        self.ops = []
        self.eng_objs = {"pe": nc.tensor, "act": nc.scalar, "dve": nc.vector,
                         "pool": nc.gpsimd, "sp": nc.sync}
        self.ecount = {e: 0 for e in ENGS}
        self.dsem_names = []
        self.group_dsems = set()

    def add(self, eng, fn, reads=(), writes=(), dsem=None):
        rd = []
        for v in reads:
            if isinstance(v, View):
                rd.extend(v.deps)
        wr = []
        for v in writes:
            if isinstance(v, View):
                wr.extend(v.deps)
        op = Op(eng, fn, dsem)
        op.eidx = self.ecount[eng]
        self.ecount[eng] += 1
        deps = set()
        for d in rd:
            if d.last_w is not None:
                deps.add(d.last_w)
        for d in wr:
            if d.last_w is not None:
                deps.add(d.last_w)
            for r in d.readers:
                deps.add(r)
        op.deps = list(deps)
        for d in rd:
            d.readers.append(op)
        for d in wr:
            d.last_w = op
            d.readers = []
        self.ops.append(op)
        return op

    def new_dsem(self, name, group=False):
        self.dsem_names.append(name)
        if group:
            self.group_dsems.add(name)
        return name

    def tile_dsem(self, tile):
        if tile.dsem is None:
            tile.dsem = self.new_dsem(f"t{len(self.dsem_names)}")
        return tile.dsem

    def emit(self):
        nc = self.nc
        for op in self.ops:
            for p in op.deps:
                if p.dsem is None:
                    if p.eng == "pe" and op.eng == "pe":
                        continue
                    p.signal = True
        cnt = {e: 0 for e in ENGS}
        dcnt = {n: 0 for n in self.dsem_names}
        for op in self.ops:
            if op.dsem is not None:
                dcnt[op.dsem] += 16
                op.dval = dcnt[op.dsem]
            elif op.signal:
                cnt[op.eng] += 1
                op.sigval = cnt[op.eng]
        sems = {}
        for e in ENGS:
            sems[e] = nc.alloc_semaphore(f"s_{e}")
        for n in self.dsem_names:
            sems[n] = nc.alloc_semaphore(f"d_{n}")
        known = {e: {} for e in ENGS}
        nw = 0
        for op in self.ops:
            eo = self.eng_objs[op.eng]
            need = {}
            for p in op.deps:
                if p.dsem is not None:
                    key = p.dsem
                    val = dcnt[key] if key in self.group_dsems else p.dval
                else:
                    if p.sigval is None:
                        continue
                    key = p.eng
                    val = p.sigval
                if need.get(key, 0) < val:
                    need[key] = val
            kn = known[op.eng]
            for key, val in need.items():
                if kn.get(key, 0) >= val:
                    continue
                eo.wait_ge(sems[key], val)
                kn[key] = val
                nw += 1
            ins = op.fn()
            if op.dsem is not None:
                ins.then_inc(sems[op.dsem], 16)
            elif op.signal:
                ins.then_inc(sems[op.eng], 1)
        self.nwaits = nw
        self.sems = sems
        self.dtotal = dcnt

    def mm(self, out, lhsT, rhs, start=True, stop=True):
        nc = self.nc
        return self.add("pe", lambda: nc.tensor.matmul(out.ap, lhsT.ap, rhs.ap, start=start, stop=stop),
                        reads=[lhsT, rhs], writes=[out])

    def transpose(self, out, in_, ident):
        nc = self.nc
        return self.add("pe", lambda: nc.tensor.transpose(out.ap, in_.ap, ident.ap),
                        reads=[in_, ident], writes=[out])

    def act(self, out, in_, func, bias=None, scale=None):
        nc = self.nc
        kw = {}
        if bias is not None:
            kw["bias"] = bias.ap if isinstance(bias, View) else bias
        if scale is not None:
            kw["scale"] = scale.ap if isinstance(scale, View) else scale
        return self.add("act", lambda: nc.scalar.activation(out.ap, in_.ap, func, **kw),
                        reads=[in_, bias, scale], writes=[out])

    def _veng(self, eng):
        return self.nc.vector if eng == "dve" else self.nc.gpsimd

    def tt(self, out, in0, in1, op, eng="dve"):
        e = self._veng(eng)
        return self.add(eng, lambda: e.tensor_tensor(out.ap, in0.ap, in1.ap, op), reads=[in0, in1], writes=[out])

    def ts(self, out, in0, s1, s2, op0, op1=None, eng="dve"):
        e = self._veng(eng)
        a1 = s1.ap if isinstance(s1, View) else s1
        a2 = s2.ap if isinstance(s2, View) else s2
        if op1 is None:
            return self.add(eng, lambda: e.tensor_single_scalar(out.ap, in0.ap, a1, op0), reads=[in0, s1], writes=[out])
        return self.add(eng, lambda: e.tensor_scalar(out.ap, in0.ap, a1, a2, op0, op1), reads=[in0, s1, s2], writes=[out])

    def stt(self, out, in0, scalar, in1, op0, op1):
        nc = self.nc
        a = scalar.ap if isinstance(scalar, View) else scalar
        return self.add("dve", lambda: nc.vector.scalar_tensor_tensor(out.ap, in0.ap, a, in1.ap, op0, op1),
                        reads=[in0, scalar, in1], writes=[out])

    def copy(self, out, in_, eng="dve"):
        if eng == "act":
            nc = self.nc
            return self.add("act", lambda: nc.scalar.copy(out.ap, in_.ap), reads=[in_], writes=[out])
        e = self._veng(eng)
        return self.add(eng, lambda: e.tensor_copy(out.ap, in_.ap), reads=[in_], writes=[out])

    def memset(self, out, val, eng="dve"):
        e = self._veng(eng)
        return self.add(eng, lambda: e.memset(out.ap, val), writes=[out])

    def scan(self, out, d0, d1, init, op0, op1):
        nc = self.nc
        return self.add("dve", lambda: nc.vector.tensor_tensor_scan(out.ap, d0.ap, d1.ap, init, op0, op1),
                        reads=[d0, d1], writes=[out])

    def reduce(self, out, in_, op):
        nc = self.nc
        return self.add("dve", lambda: nc.vector.tensor_reduce(out.ap, in_.ap, AX.X, op), reads=[in_], writes=[out])

    def recip(self, out, in_):
        nc = self.nc
        return self.add("dve", lambda: nc.vector.reciprocal(out.ap, in_.ap), reads=[in_], writes=[out])

    def bn_stats(self, out, in_):
        nc = self.nc
        return self.add("dve", lambda: nc.vector.bn_stats(out.ap, in_.ap), reads=[in_], writes=[out])

    def bn_aggr(self, out, in_):
        nc = self.nc
        return self.add("dve", lambda: nc.vector.bn_aggr(out.ap, in_.ap), reads=[in_], writes=[out])

    def dma(self, out, in_, eng="sp", dsem=None):
        eo = self.eng_objs[eng]
        oap = out.ap if isinstance(out, View) else out
        iap = in_.ap if isinstance(in_, View) else in_
        if dsem is None:
            sb = out if (isinstance(out, View) and out.tile is not None and not getattr(out.tile, "is_dram", False)) else in_
            dsem = self.tile_dsem(sb.tile)
        return self.add(eng, lambda: eo.dma_start(out=oap, in_=iap), reads=[in_], writes=[out], dsem=dsem)


class Pool:
    def __init__(self, tiles):
        self.free = collections.deque(tiles)
        self.n = len(tiles)
        self.low = len(tiles)

    def get(self):
        assert self.free, "pool exhausted"
        t = self.free.popleft()
        self.low = min(self.low, len(self.free))
        return t

    def put(self, *ts):
        for t in ts:
            self.free.append(t)


def kmajor_slots(W, ncols, col_idx=None):
    K, N = W.shape
    KC = K // 128
    if col_idx is not None:
        W = W[:, col_idx]
        N = W.shape[1]
    NS = N // ncols
    A = W.reshape(KC, 128, NS, ncols).transpose(2, 1, 0, 3)
    return np.ascontiguousarray(A).reshape(NS, 128, KC * ncols)


def fm_cols(v):
    v = np.asarray(v, np.float32)
    lead = v.shape[:-1]
    C = v.shape[-1] // 128
    A = v.reshape(*lead, C, 128)
    A = np.moveaxis(A, -1, 0)
    return np.ascontiguousarray(A).reshape(128, -1)


class PV:
    def __init__(self):
        self.cols = []
        self.off = {}
        self.n = 0

    def add(self, name, arr):
        arr = np.asarray(arr, np.float32)
        assert arr.shape[0] == 128
        self.off[name] = self.n
        self.cols.append(arr)
        self.n += arr.shape[1]

    def build(self):
        return np.ascontiguousarray(np.concatenate(self.cols, axis=1))


def blockdiag_chunks(w):
    out = np.zeros((16, 128, 128), np.float32)
    wr = w.reshape(16, 32, 4, 4)
    for b in range(32):
        out[:, 4 * b:4 * b + 4, 4 * b:4 * b + 4] = wr[:, b]
    return out


def host_prep(inp, layers):
    pv = PV()
    shared = {}
    ffn_cols = np.concatenate([np.concatenate([np.arange(j * 128, (j + 1) * 128),
                                               DFF + np.arange(j * 128, (j + 1) * 128)]) for j in range(FC)])
    conf_cols = np.concatenate([np.concatenate([np.arange(j * 128, (j + 1) * 128),
                                                D + np.arange(j * 128, (j + 1) * 128)]) for j in range(DC)])
    for l in layers:
        pv.add(f"adab{l}", fm_cols(inp["ada_b"][l]))
        pv.add(f"gmix{l}", fm_cols(inp["norm_mix_g"][l]))
        pv.add(f"gffn{l}", fm_cols(inp["norm_ffn_g"][l]))
        shared[f"ada{l}"] = kmajor_slots(inp["ada_w"][l], 256)
        shared[f"fup{l}"] = kmajor_slots(inp["ffn_w_up"][l], 256, ffn_cols)
        fd = inp["ffn_w_down"][l]
        A = fd.reshape(2, 11, 128, DC, 128).transpose(3, 0, 2, 1, 4)
        shared[f"fdn{l}"] = np.ascontiguousarray(A).reshape(16, 128, 11 * 128)
        cw = inp["ffn_conv_w"][l]
        cwg = fm_cols(cw[:, :DFF])
        cwv = fm_cols(cw[:, DFF:])
        pv.add(f"fcwg{l}", cwg)
        pv.add(f"fcwv{l}", cwv)
        pv.add(f"fcbg{l}", fm_cols(inp["ffn_conv_b"][l][:DFF]))
        pv.add(f"fcbv{l}", fm_cols(inp["ffn_conv_b"][l][DFF:]))
        j = l // 2
        if l % 2 == 0:
            shared[f"mup{l}"] = kmajor_slots(inp["mlstm_w_up"][j], 256)
            shared[f"mdn{l}"] = kmajor_slots(inp["mlstm_w_down"][j], 128)
            bq = blockdiag_chunks(inp["mlstm_wq"][j])
            bk = blockdiag_chunks(inp["mlstm_wk"][j])
            bv = blockdiag_chunks(inp["mlstm_wv"][j])
            B = np.stack([bq, bk, bv], axis=1)
            B = B.reshape(4, 4, 3, 128, 128).transpose(0, 3, 1, 2, 4)
            shared[f"wbd{l}"] = np.ascontiguousarray(B).reshape(4, 128, 4 * 3 * 128)
            wif = inp["mlstm_w_if"][j]
            W = np.zeros((48, 128, 36), np.float32)
            W[:, :, 0:4] = wif[:, 0:4].reshape(48, 128, 4)
            W[:, :, 32:36] = wif[:, 4:8].reshape(48, 128, 4)
            shared[f"wif{l}"] = np.ascontiguousarray(W.transpose(1, 0, 2)).reshape(1, 128, 48 * 36)
            pv.add(f"mcw{l}", fm_cols(inp["mlstm_conv_w"][j]))
            pv.add(f"mcb{l}", fm_cols(inp["mlstm_conv_b"][j]))
            pv.add(f"mlnw{l}", fm_cols(inp["mlstm_ln_w"][j]))
            pv.add(f"mskip{l}", fm_cols(inp["mlstm_skip"][j]))
            bif = np.zeros((128, 2), np.float32)
            bif[0:4, 0] = inp["mlstm_b_if"][j][0:4]
            bif[0:4, 1] = inp["mlstm_b_if"][j][4:8]
            pv.add(f"mbif{l}", bif)
        else:
            shared[f"cp1{l}"] = kmajor_slots(inp["conf_w_pw1"][j], 256, conf_cols)
            shared[f"cp2{l}"] = kmajor_slots(inp["conf_w_pw2"][j], 256)
            pv.add(f"cb1a{l}", fm_cols(inp["conf_b_pw1"][j][:D]))
            pv.add(f"cb1g{l}", fm_cols(inp["conf_b_pw1"][j][D:]))
            pv.add(f"cdw{l}", fm_cols(inp["conf_dw_w"][j]))
            pv.add(f"cdb{l}", fm_cols(inp["conf_dw_b"][j]))
            pv.add(f"clg{l}", fm_cols(inp["conf_ln_g"][j]))
            pv.add(f"clb{l}", fm_cols(inp["conf_ln_b"][j]))
            pv.add(f"cb2{l}", fm_cols(inp["conf_b_pw2"][j]))
    pv.add("fing", fm_cols(inp["final_g"]))
    ident = np.eye(128, dtype=np.float32)
    mask = np.triu(np.ones((128, 128), np.float32))
    self_ = np.zeros((128, 4), np.float32)
    for jj in range(4):
        self_[32 + jj, jj] = 1.0
    shared["consts"] = np.ascontiguousarray(np.concatenate([ident, mask, self_], axis=1))
    shared["pvec"] = pv.build()
    return shared, pv.off, pv.n


class Builder:
    def __init__(self, nt, layers, shared_shapes, pvoff, pvn, final_norm=True):
        self.nt = nt
        self.layers = layers
        self.final_norm = final_norm
        self.pvoff = pvoff
        nc = bass.Bass("TRN2", target_bir_lowering=False)
        self.nc = nc
        self.P = Prog(nc)
        P = self.P
        S = nt * T
        self.x_d = nc.dram_tensor("x", [128, DC, S], F32, kind="ExternalInput").ap()
        self.c_d = nc.dram_tensor("c", [128, DC], F32, kind="ExternalInput").ap()
        self.out_d = nc.dram_tensor("out", [128, DC, S], F32, kind="ExternalOutput").ap()
        self.w_d = {}
        for name, shp in shared_shapes.items():
            self.w_d[name] = nc.dram_tensor(name, list(shp), F32, kind="ExternalInput").ap()

        def sb(name, shape, dt=F32, nparts=1):
            return Tile(name, nc.alloc_sbuf_tensor(name, shape, dt), nparts)

        self.sb = sb
        self.X = sb("X", [128, DC, T], F32, nparts=DC)
        self.PVEC = sb("PVEC", [128, pvn], F32)
        self.CONST = sb("CONST", [128, 260], F32)
        self.IDB = sb("IDB", [128, 128], BF16)
        self.ONESB = sb("ONESB", [128, 128], BF16)
        self.ONESF = sb("ONESF", [128, 512], F32)
        self.CACT = sb("CACT", [128, DC], BF16)
        self.CIN = sb("CIN", [128, DC], F32)
        self.MOD = {l: sb(f"MOD{l}", [128, 48], F32) for l in layers}
        self.AB = {l: sb(f"AB{l}", [128, 16], F32) for l in layers}
        self.HALO_F = {l: sb(f"HALOF{l}", [128, 2 * FC, 2], BF16) for l in layers}
        self.HALO_M = {l: sb(f"HALOM{l}", [128, IC, 4], BF16) for l in layers if l % 2 == 0}
        self.HALO_C = {l: sb(f"HALOC{l}", [128, DC, 30], F32) for l in layers if l % 2 == 1}
        self.NST = {l: sb(f"NST{l}", [128, IC], F32) for l in layers if l % 2 == 0}
        self.MST = {l: sb(f"MST{l}", [4, 1], F32) for l in layers if l % 2 == 0}
        self.WIF = sb("WIF", [128, 48 * 36], BF16)
        self.MASKK = sb("MASKK", [128, 128], F32)
        self.CST = {}
        for l in layers:
            if l % 2 == 0:
                for h in range(NH):
                    t = Tile(f"CST{l}_{h}", nc.dram_tensor(f"cst{l}_{h}", [4, 128, T], F32, kind="Internal"))
                    t.is_dram = True
                    self.CST[(l, h)] = t
        self.RING = [sb(f"RING{i}", [128, SLOT], BF16) for i in range(NRING)]
        self.ring_i = 0
        NB, NF = 100, 14
        self.bpool = Pool([sb(f"B{i}", [128, HB + T], BF16) for i in range(NB)])
        self.fpool = Pool([sb(f"F{i}", [128, HF + T], F32) for i in range(NF)])
        self.banks = Pool([Tile(f"PS{i}", nc.alloc_psum_tensor(f"PS{i}", [128, 512], F32)) for i in range(8)])
        self.small_i = 0
        self.smalls = [sb(f"SMALL{i}", [128, 16], F32) for i in range(24)]

    def dump(self, name, view, shape, dt=F32):
        import os
        if not os.environ.get("MDUMP"):
            return
        if not hasattr(self, "dbg_dsem"):
            self.dbg_dsem = self.P.new_dsem("dbg", group=True)
            self.dumps = []
        if name in self.dumps:
            return
        self.dumps.append(name)
        d = self.nc.dram_tensor(name, list(shape), dt, kind="ExternalOutput").ap()
        self.P.dma(d, view, eng="sp", dsem=self.dbg_dsem)

    def small(self):
        t = self.smalls[self.small_i % len(self.smalls)]
        self.small_i += 1
        return t

    def pcol(self, name, c0, n=1):
        o = self.pvoff[name] + c0
        return self.PVEC[:, o:o + n]

    def wload(self, name, s, nelem):
        slot = self.RING[self.ring_i % NRING]
        self.ring_i += 1
        self.P.dma(slot[:, 0:nelem], self.w_d[name][s, :, :], eng="pool")
        return slot

    def prologue(self):
        P = self.P
        P.dma(self.PVEC[:, :], self.w_d["pvec"], eng="sp")
        P.dma(self.CONST[:, :], self.w_d["consts"], eng="sp")
        P.dma(self.CIN[:, :], self.c_d, eng="sp")
        P.copy(self.IDB[:, :], self.CONST[:, 0:128])
        P.ts(self.MASKK[:, :], self.CONST[:, 128:256], float(512 ** -0.5), None, ALU.mult)
        P.memset(self.ONESB[:, :], 1.0)
        P.memset(self.ONESF[:, :], 1.0, eng="pool")
        P.act(self.CACT[:, :], self.CIN[:, :], AF.Silu)
        for l in self.layers:
            P.memset(self.HALO_F[l][:, :, :], 0.0, eng="pool")
            if l % 2 == 0:
                P.memset(self.HALO_M[l][:, :, :], 0.0, eng="pool")
                P.memset(self.NST[l][:, :], 0.0, eng="pool")
                P.memset(self.MST[l][:, :], 0.0, eng="pool")
            else:
                P.memset(self.HALO_C[l][:, :, :], 0.0, eng="pool")

    def ada(self, l):
        P = self.P
        bank = self.banks.get()
        for s in range(24):
            slot = self.wload(f"ada{l}", s, 2048)
            w = slot.t[:, :].rearrange("p (k n) -> p k n", k=DC)
            for jj in range(2):
                oc = 2 * s + jj
                for kc in range(DC):
                    P.mm(bank[:, oc:oc + 1], slot.v(w[:, kc, jj * 128:(jj + 1) * 128]), self.CACT[:, kc:kc + 1],
                         start=(kc == 0), stop=(kc == DC - 1))
        M = self.MOD[l]
        P.tt(M[:, :], bank[:, 0:48], self.pcol(f"adab{l}", 0, 48), ALU.add)
        self.banks.put(bank)
        AB = self.AB[l]
        P.stt(AB[:, 0:8], M[:, 8:16], 1.0, self.pcol(f"gmix{l}", 0, 8), ALU.add, ALU.mult)
        P.stt(AB[:, 8:16], M[:, 32:40], 1.0, self.pcol(f"gffn{l}", 0, 8), ALU.add, ALU.mult)

    def rstd_from_bank(self, bank):
        P = self.P
        r = self.fpool.get()
        P.ts(r[:, 0:T], bank[:, 0:T], 1.0 / D, EPS, ALU.mult, ALU.add)
        P.act(r[:, 0:T], r[:, 0:T], AF.Sqrt)
        P.recip(r[:, 0:T], r[:, 0:T])
        return r

    def sumsq(self):
        P = self.P
        bank = self.banks.get()
        for dc in range(DC):
            sq = self.bpool.get()
            P.act(sq[:, 0:T], self.X[:, dc, :], AF.Square)
            P.mm(bank[:, 0:T], self.ONESB[:, :], sq[:, 0:T], start=(dc == 0), stop=(dc == DC - 1))
            self.bpool.put(sq)
        r = self.rstd_from_bank(bank)
        self.banks.put(bank)
        return r

    def norm_mod(self, l, which):
        P = self.P
        r = self.sumsq()
        a0 = 0 if which == 0 else 8
        sh0 = 0 if which == 0 else 24
        H = []
        for dc in range(DC):
            tmp = self.fpool.get()
            P.tt(tmp[:, 0:T], self.X[:, dc, :], r[:, 0:T], ALU.mult)
            h = self.bpool.get()
            P.act(h[:, 0:T], tmp[:, 0:T], AF.Identity, bias=self.MOD[l][:, sh0 + dc:sh0 + dc + 1],
                  scale=self.AB[l][:, a0 + dc:a0 + dc + 1])
            self.fpool.put(tmp)
            H.append(h)
        self.fpool.put(r)
        return H

    def ffn(self, l, ti):
        P = self.P
        H = self.norm_mod(l, 1)
        HID = []
        halo = self.HALO_F[l]
        for j in range(FC):
            slot = self.wload(f"fup{l}", j, 2048)
            w = slot.t[:, :].rearrange("p (k n) -> p k n", k=DC)
            accs = []
            for gv in range(2):
                bank = self.banks.get()
                for kc in range(DC):
                    P.mm(bank[:, 0:T], slot.v(w[:, kc, gv * 128:(gv + 1) * 128]), H[kc][:, 0:T],
                         start=(kc == 0), stop=(kc == DC - 1))
                u = self.bpool.get()
                hj = 2 * j + gv
                P.copy(u[:, 0:2], halo[:, hj, :], eng="pool")
                P.copy(u[:, 2:2 + T], bank[:, 0:T], eng="act")
                nm = "fcwg" if gv == 0 else "fcwv"
                nb = "fcbg" if gv == 0 else "fcbv"
                acc = self.fpool.get()
                P.act(acc[:, 0:T], bank[:, 0:T], AF.Identity, bias=self.pcol(f"{nb}{l}", j),
                      scale=self.pcol(f"{nm}{l}", 2 * FC + j))
                self.banks.put(bank)
                P.stt(acc[:, 0:T], u[:, 1:1 + T], self.pcol(f"{nm}{l}", 1 * FC + j), acc[:, 0:T], ALU.mult, ALU.add)
                P.stt(acc[:, 0:T], u[:, 0:T], self.pcol(f"{nm}{l}", 0 * FC + j), acc[:, 0:T], ALU.mult, ALU.add)
                P.copy(halo[:, hj, :], u[:, T:T + 2], eng="pool")
                self.bpool.put(u)
                accs.append(acc)
            sg = self.fpool.get()
            P.act(sg[:, 0:T], accs[0][:, 0:T], AF.Silu)
            hid = self.bpool.get()
            P.tt(hid[:, 0:T], sg[:, 0:T], accs[1][:, 0:T], ALU.mult)
            self.fpool.put(sg, accs[0], accs[1])
            HID.append(hid)
        self.bpool.put(*H)
        for dc in range(DC):
            bank = self.banks.get()
            for half in range(2):
                slot = self.wload(f"fdn{l}", dc * 2 + half, 11 * 128)
                w = slot.t[:, 0:11 * 128].rearrange("p (k n) -> p k n", k=11)
                for k in range(11):
                    kc = half * 11 + k
                    P.mm(bank[:, 0:T], slot.v(w[:, k, :]), HID[kc][:, 0:T], start=(kc == 0), stop=(kc == FC - 1))
            P.stt(self.X[:, dc, :], bank[:, 0:T], self.MOD[l][:, 40 + dc:41 + dc], self.X[:, dc, :], ALU.mult, ALU.add)
            self.banks.put(bank)
        self.bpool.put(*HID)

    def conformer(self, l, ti):
        P = self.P
        H = self.norm_mod(l, 0)
        halo = self.HALO_C[l]
        ACC = []
        mbank = self.banks.get()
        sbank = self.banks.get()
        for j in range(DC):
            slot = self.wload(f"cp1{l}", j, 2048)
            w = slot.t[:, :].rearrange("p (k n) -> p k n", k=DC)
            ba = self.banks.get()
            bg = self.banks.get()
            for kc in range(DC):
                P.mm(ba[:, 0:T], slot.v(w[:, kc, 0:128]), H[kc][:, 0:T], start=(kc == 0), stop=(kc == DC - 1))
            for kc in range(DC):
                P.mm(bg[:, 0:T], slot.v(w[:, kc, 128:256]), H[kc][:, 0:T], start=(kc == 0), stop=(kc == DC - 1))
            sig = self.fpool.get()
            P.act(sig[:, 0:T], bg[:, 0:T], AF.Sigmoid, bias=self.pcol(f"cb1g{l}", j))
            self.banks.put(bg)
            u = self.fpool.get()
            P.copy(u[:, 2:32], halo[:, j, :], eng="pool")
            P.stt(u[:, 32:32 + T], ba[:, 0:T], self.pcol(f"cb1a{l}", j), sig[:, 0:T], ALU.add, ALU.mult)
            self.banks.put(ba)
            self.fpool.put(sig)
            acc = self.fpool.get()
            P.ts(acc[:, 0:T], u[:, 2:2 + T], self.pcol(f"cdw{l}", 0 * DC + j), self.pcol(f"cdb{l}", j), ALU.mult, ALU.add)
            for k in range(1, CONF_K):
                P.stt(acc[:, 0:T], u[:, 2 + k:2 + k + T], self.pcol(f"cdw{l}", k * DC + j), acc[:, 0:T], ALU.mult, ALU.add)
            P.copy(halo[:, j, :], u[:, 2 + T:32 + T], eng="pool")
            self.fpool.put(u)
            ub = self.bpool.get()
            us = self.bpool.get()
            P.copy(ub[:, 0:T], acc[:, 0:T], eng="act")
            P.act(us[:, 0:T], acc[:, 0:T], AF.Square)
            P.mm(mbank[:, 0:T], self.ONESB[:, :], ub[:, 0:T], start=(j == 0), stop=(j == DC - 1))
            P.mm(sbank[:, 0:T], self.ONESB[:, :], us[:, 0:T], start=(j == 0), stop=(j == DC - 1))
            self.bpool.put(ub, us)
            ACC.append(acc)
        self.bpool.put(*H)
        mean = self.fpool.get()
        rstd = self.fpool.get()
        P.ts(mean[:, 0:T], mbank[:, 0:T], 1.0 / D, None, ALU.mult)
        P.tt(rstd[:, 0:T], mean[:, 0:T], mean[:, 0:T], ALU.mult)
        P.stt(rstd[:, 0:T], sbank[:, 0:T], 1.0 / D, rstd[:, 0:T], ALU.mult, ALU.subtract)
        P.ts(rstd[:, 0:T], rstd[:, 0:T], EPS, None, ALU.add)
        P.act(rstd[:, 0:T], rstd[:, 0:T], AF.Sqrt)
        P.recip(rstd[:, 0:T], rstd[:, 0:T])
        self.banks.put(mbank, sbank)
        Y = []
        for j in range(DC):
            acc = ACC[j]
            P.tt(acc[:, 0:T], acc[:, 0:T], mean[:, 0:T], ALU.subtract)
            P.tt(acc[:, 0:T], acc[:, 0:T], rstd[:, 0:T], ALU.mult)
            y = self.bpool.get()
            P.act(y[:, 0:T], acc[:, 0:T], AF.Silu, bias=self.pcol(f"clb{l}", j), scale=self.pcol(f"clg{l}", j))
            self.fpool.put(acc)
            Y.append(y)
        self.fpool.put(mean, rstd)
        for s in range(4):
            slot = self.wload(f"cp2{l}", s, 2048)
            w = slot.t[:, :].rearrange("p (k n) -> p k n", k=DC)
            for jj in range(2):
                dc = 2 * s + jj
                bank = self.banks.get()
                for kc in range(DC):
                    P.mm(bank[:, 0:T], slot.v(w[:, kc, jj * 128:(jj + 1) * 128]), Y[kc][:, 0:T],
                         start=(kc == 0), stop=(kc == DC - 1))
                tmp = self.fpool.get()
                P.ts(tmp[:, 0:T], bank[:, 0:T], self.pcol(f"cb2{l}", dc), self.MOD[l][:, 16 + dc:17 + dc], ALU.add, ALU.mult)
                self.banks.put(bank)
                P.tt(self.X[:, dc, :], self.X[:, dc, :], tmp[:, 0:T], ALU.add)
                self.fpool.put(tmp)
        self.bpool.put(*Y)

    def mlstm(self, l, ti):
        P = self.P
        nc = self.nc
        H = self.norm_mod(l, 0)
        halo = self.HALO_M[l]
        XM, XC, Q, KF = [], [], [], []
        gbank = self.banks.get()
        wif = self.WIF
        self.P.dma(wif[:, 0:48 * 36], self.w_d[f"wif{l}"][0, :, :], eng="pool")
        wifv = wif.t[:, 0:48 * 36].rearrange("p (c m) -> p c m", c=48)
        kscale = float(512 ** -0.5)
        wbd = None
        for c in range(IC):
            if c % 2 == 0:
                upslot = self.wload(f"mup{l}", c // 2, 2048)
                upw = upslot.t[:, :].rearrange("p (k n) -> p k n", k=DC)
            if c % 4 == 0:
                wbd = self.wload(f"wbd{l}", c // 4, 1536)
                wbdv = wbd.t[:, 0:1536].rearrange("p (c q m) -> p c q m", c=4, q=3)
            jj = c % 2
            bank = self.banks.get()
            for kc in range(DC):
                P.mm(bank[:, 0:T], upslot.v(upw[:, kc, jj * 128:(jj + 1) * 128]), H[kc][:, 0:T],
                     start=(kc == 0), stop=(kc == DC - 1))
            xm = self.bpool.get()
            P.copy(xm[:, HB - 4:HB], halo[:, c, :], eng="pool")
            P.copy(xm[:, HB:HB + T], bank[:, 0:T], eng="act")
            acc = self.fpool.get()
            P.act(acc[:, 0:T], bank[:, 0:T], AF.Identity, bias=self.pcol(f"mcb{l}", c), scale=self.pcol(f"mcw{l}", 3 * IC + c))
            self.banks.put(bank)
            for k in range(3):
                P.stt(acc[:, 0:T], xm[:, HB - 3 + k:HB - 3 + k + T], self.pcol(f"mcw{l}", k * IC + c), acc[:, 0:T], ALU.mult, ALU.add)
            P.copy(halo[:, c, :], xm[:, HB + T - 4:HB + T], eng="pool")
            xc = self.bpool.get()
            P.act(xc[:, 0:T], acc[:, 0:T], AF.Silu)
            self.fpool.put(acc)
            cc = c % 4
            bq = self.banks.get()
            P.mm(bq[:, 0:T], wbd.v(wbdv[:, cc, 0, :]), xc[:, 0:T])
            q = self.bpool.get()
            P.copy(q[:, 0:T], bq[:, 0:T], eng="act")
            self.banks.put(bq)
            bk = self.banks.get()
            P.mm(bk[:, 0:T], wbd.v(wbdv[:, cc, 1, :]), xc[:, 0:T])
            kf = self.bpool.get()
            P.copy(kf[:, 0:T], bk[:, 0:T])
            self.banks.put(bk)
            bv = self.banks.get()
            P.mm(bv[:, 0:T], wbd.v(wbdv[:, cc, 2, :]), xm[:, HB:HB + T])
            vf = self.bpool.get()
            P.copy(vf[:, 0:T], bv[:, 0:T], eng="act")
            self.banks.put(bv)
            P.mm(gbank[0:36, 0:T], wif.v(wifv[:, c, :]), q[:, 0:T], start=(c == 0), stop=False)
            P.mm(gbank[0:36, 0:T], wif.v(wifv[:, 16 + c, :]), kf[:, 0:T], start=False, stop=False)
            P.mm(gbank[0:36, 0:T], wif.v(wifv[:, 32 + c, :]), vf[:, 0:T], start=False, stop=(c == IC - 1))
            self.bpool.put(vf)
            XM.append(xm); XC.append(xc); Q.append(q); KF.append(kf)
            if c == 0:
                self.dump("d_xm0", xm[:, HB:HB + T], [128, T], BF16)
                self.dump("d_xc0", xc[:, 0:T], [128, T], BF16)
                self.dump("d_q0", q[:, 0:T], [128, T], BF16)
                self.dump("d_k0", kf[:, 0:T], [128, T], BF16)
        import os
        DBG = int(os.environ.get("MDBG", "99"))
        if DBG == 1:
            return
        GS = self.fpool.get()
        P.copy(GS[0:36, 0:T], gbank[0:36, 0:T], eng="act")
        self.dump("d_gs", GS[0:36, 0:T], [36, T])
        P.mm(gbank[0:4, 0:T], self.CONST[0:36, 256:260], GS[0:36, 0:T])
        ipre = self.fpool.get()
        ob = self.pvoff[f"mbif{l}"]
        P.ts(ipre[0:4, 0:T], GS[0:4, 0:T], self.PVEC[0:4, ob:ob + 1], None, ALU.add)
        lf = self.fpool.get()
        P.act(lf[0:4, 0:T], gbank[0:4, 0:T], AF.Sigmoid, bias=self.PVEC[0:4, ob + 1:ob + 2])
        self.banks.put(gbank)
        self.fpool.put(GS)
        P.act(lf[0:4, 0:T], lf[0:4, 0:T], AF.Ln)
        g = self.fpool.get()
        P.scan(g[0:4, 0:T], self.ONESF[0:4, 0:T], lf[0:4, 0:T], 0.0, ALU.mult, ALU.add)
        self.fpool.put(lf)
        a = ipre
        P.tt(a[0:4, 0:T], ipre[0:4, 0:T], g[0:4, 0:T], ALU.subtract)
        sm = self.small()
        mst = self.MST[l]
        P.reduce(sm[0:4, 0:4], a.v(a.t[0:4, 0:T].rearrange("p (b t) -> p b t", b=4)), ALU.max)
        P.tt(sm[0:4, 4:5], sm[0:4, 0:1], mst[0:4, 0:1], ALU.max)
        for lb in range(1, 4):
            P.tt(sm[0:4, 4 + lb:5 + lb], sm[0:4, lb:lb + 1], sm[0:4, 3 + lb:4 + lb], ALU.max)
        P.ts(sm[0:4, 8:12], sm[0:4, 4:8], -1.0, None, ALU.mult)
        P.act(sm[0:4, 12:16], sm[0:4, 8:12], AF.Exp, bias=mst[0:4, 0:1])
        E = []
        fl = self.fpool.get()
        for lb in range(4):
            e = self.fpool.get()
            n = 128 * (lb + 1)
            P.act(e[0:4, 0:n], a[0:4, 0:n], AF.Exp, bias=sm[0:4, 8 + lb:9 + lb])
            P.act(fl[0:4, lb * 128:n], g[0:4, lb * 128:n], AF.Exp, bias=sm[0:4, 8 + lb:9 + lb], scale=-1.0)
            E.append(e)
        self.dump("d_e", E[3][0:4, 0:T], [4, T])
        self.dump("d_fl", fl[0:4, 0:T], [4, T])
        self.dump("d_g", g[0:4, 0:T], [4, T])
        self.dump("d_sm", sm[0:4, 0:16], [4, 16])
        P.tt(mst[0:4, 0:1], g[0:4, T - 1:T], sm[0:4, 7:8], ALU.add)
        self.fpool.put(g, a)
        if DBG == 2:
            return
        tb = self.banks.get()
        for lb in range(4):
            for sbi in range(lb + 1):
                c0 = (lb * 4 + sbi) * 4
                P.transpose(tb[:, c0:c0 + 4], E[lb][0:4, sbi * 128:(sbi + 1) * 128], self.CONST[0:4, 0:4])
            P.transpose(tb[:, 64 + lb * 4:68 + lb * 4], fl[0:4, lb * 128:(lb + 1) * 128], self.CONST[0:4, 0:4])
        dg = self.small()
        for lb in range(4):
            P.ts(dg[0:4, lb * 4:lb * 4 + 4], self.CONST[0:4, 0:4], sm[0:4, 12 + lb:13 + lb], None, ALU.mult)
        P.mm(tb[:, 80:96], self.ONESF[0:4, 0:128], dg[0:4, 0:16])
        TM = self.small_big()
        P.memset(TM[:, 0:64], 0.0, eng="pool")
        for lb in range(4):
            c0 = lb * 16
            P.copy(TM[:, c0:c0 + 4 * (lb + 1)], tb[:, c0:c0 + 4 * (lb + 1)])
        P.copy(TM[:, 64:96], tb[:, 64:96])
        self.banks.put(tb)
        self.fpool.put(fl, *E)
        self.dump("d_tm", TM[:, 0:96], [128, 96])
        ETM = lambda lb, sbi, h: TM[:, (lb * 4 + sbi) * 4 + h:(lb * 4 + sbi) * 4 + h + 1]
        WCB = lambda h: TM[:, 80 + 12 + h:80 + 13 + h]
        nst = self.NST[l]
        NB = self.small_bf()
        P.copy(NB[:, 0:16], nst[:, 0:16])
        if DBG == 3:
            return
        for h in range(NH):
            wbd = self.wload(f"wbd{l}", h, 1536)
            wbdv = wbd.t[:, 0:1536].rearrange("p (c q m) -> p c q m", c=4, q=3)
            KTM, VTM = [], []
            for sbi in range(4):
                bk = self.banks.get()
                bv = self.banks.get()
                for j in range(4):
                    c = 4 * h + j
                    P.mm(bk[:, j * 128:(j + 1) * 128], XC[c][:, sbi * 128:(sbi + 1) * 128], wbd.v(wbdv[:, j, 1, :]))
                    P.mm(bv[:, j * 128:(j + 1) * 128], XM[c][:, HB + sbi * 128:HB + (sbi + 1) * 128], wbd.v(wbdv[:, j, 2, :]))
                kt = self.bpool.get()
                vt = self.bpool.get()
                P.ts(kt[:, 0:T], bk[:, 0:T], ETM(3, sbi, h), kscale, ALU.mult, ALU.mult)
                P.copy(vt[:, 0:T], bv[:, 0:T], eng="act")
                self.banks.put(bk, bv)
                KTM.append(kt); VTM.append(vt)
            if DBG == 4:
                return
            C32, CB = [], []
            cst = self.CST[(l, h)]
            for kc in range(4):
                c32 = self.fpool.get()
                if ti == 0:
                    P.memset(c32[:, 0:T], 0.0, eng="pool")
                else:
                    P.dma(c32[:, 0:T], cst[kc, :, :], eng="sp")
                cb = self.bpool.get()
                P.copy(cb[:, 0:T], c32[:, 0:T], eng="act")
                C32.append(c32); CB.append(cb)
            if DBG == 5:
                return
            PT = []
            for sbi in range(4):
                bs = self.banks.get()
                n = T - sbi * 128
                for kc in range(4):
                    c = 4 * h + kc
                    P.mm(bs[:, 0:n], KF[c][:, sbi * 128:(sbi + 1) * 128], Q[c][:, sbi * 128:T], start=(kc == 0), stop=(kc == 3))
                pt = self.bpool.get()
                for lb in range(sbi, 4):
                    src = bs[:, (lb - sbi) * 128:(lb - sbi + 1) * 128]
                    dst = pt[:, lb * 128:(lb + 1) * 128]
                    if lb == sbi:
                        P.stt(dst, src, ETM(lb, sbi, h), self.MASKK[:, :], ALU.mult, ALU.mult)
                    else:
                        P.ts(dst, src, ETM(lb, sbi, h), kscale, ALU.mult, ALU.mult)
                self.banks.put(bs)
                PT.append(pt)
            if DBG == 6:
                return
            dbank = self.banks.get()
            for lb in range(4):
                for kc in range(4):
                    c = 4 * h + kc
                    P.mm(dbank[:, lb:lb + 1], Q[c][:, lb * 128:(lb + 1) * 128], NB[:, c:c + 1], start=(kc == 0), stop=(kc == 3))
            for lb in range(4):
                for sbi in range(lb + 1):
                    P.mm(dbank[:, 4 + lb:5 + lb], PT[sbi][:, lb * 128:(lb + 1) * 128], self.ONESB[:, 0:1], start=(sbi == 0), stop=(sbi == lb))
            dn = self.small()
            P.copy(dn[:, 0:8], dbank[:, 0:8])
            self.banks.put(dbank)
            wib = TM.v(TM.t[:, 80 + h:96:4])
            P.tt(dn[:, 8:12], dn[:, 0:4], wib, ALU.mult)
            P.tt(dn[:, 8:12], dn[:, 8:12], dn[:, 4:8], ALU.add)
            P.act(dn[:, 8:12], dn[:, 8:12], AF.Abs)
            P.tt(dn[:, 8:12], dn[:, 8:12], TM.v(TM.t[:, 64 + h:80:4]), ALU.max)
            P.recip(dn[:, 8:12], dn[:, 8:12])
            P.tt(dn[:, 12:16], dn[:, 8:12], wib, ALU.mult)
            self.dump("d_dn", dn[:, 0:16], [128, 16])
            self.dump("d_pt0", PT[0][:, 0:T], [128, T], BF16)
            self.dump("d_kt0", KTM[0][:, 0:T], [128, T], BF16)
            self.dump("d_vt0", VTM[0][:, 0:T], [128, T], BF16)
            HN = []
            for lb in range(4):
                b1 = self.banks.get()
                b2 = self.banks.get()
                for kc in range(4):
                    c = 4 * h + kc
                    P.mm(b1[:, 0:T], Q[c][:, lb * 128:(lb + 1) * 128], CB[kc][:, 0:T], start=(kc == 0), stop=(kc == 3))
                for sbi in range(lb + 1):
                    P.mm(b2[:, 0:T], PT[sbi][:, lb * 128:(lb + 1) * 128], VTM[sbi][:, 0:T], start=(sbi == 0), stop=(sbi == lb))
                hu = self.fpool.get()
                P.act(hu[:, 0:T], b2[:, 0:T], AF.Identity, scale=dn[:, 8 + lb:9 + lb])
                self.banks.put(b2)
                P.stt(hu[:, 0:T], b1[:, 0:T], dn[:, 12 + lb:13 + lb], hu[:, 0:T], ALU.mult, ALU.add)
                self.banks.put(b1)
                st = self.small()
                P.bn_stats(st[:, 0:6], hu[:, 0:T])
                P.bn_aggr(st[:, 6:8], st[:, 0:6])
                P.ts(st[:, 8:9], st[:, 7:8], EPS, None, ALU.add)
                P.act(st[:, 8:9], st[:, 8:9], AF.Sqrt)
                P.recip(st[:, 9:10], st[:, 8:9])
                P.stt(st[:, 10:11], st[:, 6:7], -1.0, st[:, 9:10], ALU.mult, ALU.mult)
                hn = self.bpool.get()
                P.act(hn[:, 0:T], hu[:, 0:T], AF.Identity, bias=st[:, 10:11], scale=st[:, 9:10])
                self.fpool.put(hu)
                HN.append(hn)
                if lb == 0:
                    self.dump("d_hn0", hn[:, 0:T], [128, T], BF16)
                    self.dump("d_st0", st[:, 0:12], [128, 12])
            self.bpool.put(*PT)
            self.bpool.put(*CB)
            if DBG == 7:
                return
            SZ = []
            for j in range(4):
                c = 4 * h + j
                if j % 2 == 0:
                    upslot = self.wload(f"mup{l}", 8 + c // 2, 2048)
                    upw = upslot.t[:, :].rearrange("p (k n) -> p k n", k=DC)
                bank = self.banks.get()
                for kc in range(DC):
                    P.mm(bank[:, 0:T], upslot.v(upw[:, kc, (j % 2) * 128:(j % 2 + 1) * 128]), H[kc][:, 0:T],
                         start=(kc == 0), stop=(kc == DC - 1))
                sz = self.bpool.get()
                P.act(sz[:, 0:T], bank[:, 0:T], AF.Silu)
                self.banks.put(bank)
                SZ.append(sz)
            if DBG == 8:
                return
            for j in range(4):
                c = 4 * h + j
                tbk = self.banks.get()
                tv = tbk.t[:, :].bitcast(BF16)
                for lb in range(4):
                    P.transpose(tbk.v(tv[:, lb * 128:(lb + 1) * 128]), HN[lb][:, j * 128:(j + 1) * 128], self.IDB[:, :])
                skx = self.fpool.get()
                P.act(skx[:, 0:T], XC[c][:, 0:T], AF.Identity, scale=self.pcol(f"mskip{l}", c))
                P.stt(skx[:, 0:T], tbk.v(tv[:, 0:T]), self.pcol(f"mlnw{l}", c), skx[:, 0:T], ALU.mult, ALU.add)
                self.banks.put(tbk)
                P.tt(Q[c][:, 0:T], skx[:, 0:T], SZ[j][:, 0:T], ALU.mult)
                self.fpool.put(skx)
            self.bpool.put(*HN)
            self.bpool.put(*SZ)
            if DBG == 9:
                return
            for kc in range(4):
                ub = self.banks.get()
                for sbi in range(4):
                    P.mm(ub[:, 0:T], KTM[sbi][:, kc * 128:(kc + 1) * 128], VTM[sbi][:, 0:T], start=(sbi == 0), stop=(sbi == 3))
                P.stt(C32[kc][:, 0:T], C32[kc][:, 0:T], WCB(h), ub[:, 0:T], ALU.mult, ALU.add)
                self.banks.put(ub)
                if ti < self.nt - 1:
                    P.dma(cst[kc, :, :], C32[kc][:, 0:T], eng="sp")
            self.fpool.put(*C32)
            nbk = self.banks.get()
            for kc in range(4):
                for sbi in range(4):
                    P.mm(nbk[:, kc:kc + 1], KTM[sbi][:, kc * 128:(kc + 1) * 128], self.ONESB[:, 0:1], start=(sbi == 0), stop=(sbi == 3))
            P.stt(nst[:, 4 * h:4 * h + 4], nst[:, 4 * h:4 * h + 4], WCB(h), nbk[:, 0:4], ALU.mult, ALU.add)
            self.banks.put(nbk)
            self.bpool.put(*KTM)
            self.bpool.put(*VTM)
        self.bpool.put(*H)
        self.bpool.put(*XM)
        self.bpool.put(*XC)
        self.bpool.put(*KF)
        for dc in range(DC):
            slot = self.wload(f"mdn{l}", dc, 2048)
            w = slot.t[:, :].rearrange("p (k n) -> p k n", k=IC)
            bank = self.banks.get()
            for kc in range(IC):
                P.mm(bank[:, 0:T], slot.v(w[:, kc, :]), Q[kc][:, 0:T], start=(kc == 0), stop=(kc == IC - 1))
            P.stt(self.X[:, dc, :], bank[:, 0:T], self.MOD[l][:, 16 + dc:17 + dc], self.X[:, dc, :], ALU.mult, ALU.add)
            self.banks.put(bank)
        self.bpool.put(*Q)

    _sb_i = 0

    def small_big(self):
        if not hasattr(self, "_tm"):
            self._tm = [self.sb(f"TM{i}", [128, 96], F32) for i in range(2)]
        t = self._tm[Builder._sb_i % 2]
        Builder._sb_i += 1
        return t

    def small_bf(self):
        if not hasattr(self, "_nb"):
            self._nb = [self.sb(f"NBF{i}", [128, 16], BF16) for i in range(2)]
            self._nbi = 0
        t = self._nb[self._nbi % 2]
        self._nbi += 1
        return t

    def build(self):
        P = self.P
        self.prologue()
        for ti in range(self.nt):
            P.dma(self.X[:, :, :], self.x_d[:, :, ti * T:(ti + 1) * T], eng="sp")
            for l in self.layers:
                if ti == 0:
                    self.ada(l)
                if l % 2 == 0:
                    self.mlstm(l, ti)
                else:
                    self.conformer(l, ti)
                import os
                if int(os.environ.get("MDBG", "99")) >= 99:
                    self.ffn(l, ti)
            if self.final_norm:
                r = self.sumsq()
                for dc in range(DC):
                    P.tt(self.X[:, dc, :], self.X[:, dc, :], r[:, 0:T], ALU.mult)
                    P.ts(self.X[:, dc, :], self.X[:, dc, :], self.pcol("fing", dc), None, ALU.mult)
                self.fpool.put(r)
            P.dma(self.out_d[:, :, ti * T:(ti + 1) * T], self.X[:, :, :], eng="sp")
        P.emit()
        self.nc.sync.wait_ge(P.sems[self.X.dsem], P.dtotal[self.X.dsem])
        if hasattr(self, "dbg_dsem"):
            self.nc.sync.wait_ge(P.sems[self.dbg_dsem], P.dtotal[self.dbg_dsem])
        return self.nc


def run(inputs, nt=S_FULL // T, layers=tuple(range(DEPTH)), final_norm=True, ncores=8, trace=False):
    inp = {k: np.asarray(v) for k, v in inputs.items()}
    shared, pvoff, pvn = host_prep(inp, layers)
    S = nt * T
    b = Builder(nt, list(layers), {k: v.shape for k, v in shared.items()}, pvoff, pvn, final_norm=final_norm)
    nc = b.build()
    in_maps = []
    for core in range(ncores):
        xb = inp["x"][core, :S, :]
        xt = np.ascontiguousarray(xb.T.reshape(DC, 128, S).transpose(1, 0, 2))
        ct = np.ascontiguousarray(inp["c"][core].reshape(DC, 128).T)
        m = {"x": xt, "c": ct}
        m.update(shared)
        in_maps.append(m)
    res = run_bass_kernel_spmd(nc, in_maps, core_ids=list(range(ncores)), trace=trace)
    outs = []
    for core in range(ncores):
        o = res.results[core]["out"]
        outs.append(np.ascontiguousarray(o.transpose(1, 0, 2).reshape(D, S).T))
    return np.stack(outs, axis=0).astype(np.float32), res, b


def kernel(**inputs):
    out, _, _ = run(inputs)
    return out
```

```python
import collections
import numpy as np
import concourse.bass as bass
import concourse.mybir as mybir
from concourse.bass_utils import run_bass_kernel_spmd

F32 = mybir.dt.float32
BF16 = mybir.dt.bfloat16
AF = mybir.ActivationFunctionType
ALU = mybir.AluOpType
AX = mybir.AxisListType

D = 1024
DC = 8
S_FULL = 4096
T = 512
INNER = 2048
IC = 16
NH = 4
DFF = 2816
FC = 22
CONF_K = 31
EPS = 1e-6
DEPTH = 4
SLOT = 2048
NRING = 8
HB = 8
HF = 32


class Dep:
    __slots__ = ("name", "last_w", "readers")

    def __init__(self, name):
        self.name = name
        self.last_w = None
        self.readers = {}


class View:
    __slots__ = ("ap", "deps", "tile")

    def __init__(self, ap, deps, tile=None):
        self.ap = ap
        self.deps = deps
        self.tile = tile


class Tile:
    def __init__(self, name, tensor, nparts=1):
        self.name = name
        self.t = tensor
        self.nparts = nparts
        self.deps = [Dep(f"{name}.{i}") for i in range(nparts)]
        self.dsem = None

    def __getitem__(self, key):
        ap = self.t[key]
        if self.nparts == 1:
            return View(ap, self.deps, self)
        k = key[1] if isinstance(key, tuple) and len(key) > 1 else slice(None)
        if isinstance(k, int):
            d = [self.deps[k]]
        else:
            d = list(self.deps[k])
        return View(ap, d, self)

    def v(self, ap, parts=None):
        return View(ap, self.deps if parts is None else [self.deps[p] for p in parts], self)


class Op:
    __slots__ = ("eng", "fn", "deps", "signal", "sigval", "dsem", "dval", "eidx")

    def __init__(self, eng, fn, dsem=None):
        self.eng = eng
        self.fn = fn
        self.deps = []
        self.signal = False
        self.sigval = None
        self.dsem = dsem
        self.dval = None


ENGS = ("pe", "act", "dve", "pool", "sp")


class Prog:
    def __init__(self, nc):
        self.nc = nc
        self.ops = []
        self.eng_objs = {"pe": nc.tensor, "act": nc.scalar, "dve": nc.vector,
                         "pool": nc.gpsimd, "sp": nc.sync}
        self.ecount = {e: 0 for e in ENGS}
        self.dsem_names = []
        self.group_dsems = set()

    def add(self, eng, fn, reads=(), writes=(), dsem=None):
        rd = []
        for v in reads:
            if isinstance(v, View):
                rd.extend(v.deps)
        wr = []
        for v in writes:
            if isinstance(v, View):
                wr.extend(v.deps)
        op = Op(eng, fn, dsem)
        op.eidx = self.ecount[eng]
        self.ecount[eng] += 1
        deps = set()
        for d in rd:
            if d.last_w is not None:
                deps.add(d.last_w)
        for d in wr:
            if d.last_w is not None:
                deps.add(d.last_w)
            for r in d.readers.values():
                deps.add(r)
        deps.discard(op)
        op.deps = list(deps)
        rkey = eng if dsem is None else ("d", dsem)
        for d in rd:
            d.readers[rkey] = op
        for d in wr:
            d.last_w = op
            d.readers = {}
        self.ops.append(op)
        return op

    def new_dsem(self, name, group=False):
        self.dsem_names.append(name)
        if group:
            self.group_dsems.add(name)
        return name

    def tile_dsem(self, tile):
        if tile.dsem is None:
            tile.dsem = self.new_dsem(f"t{len(self.dsem_names)}")
        return tile.dsem

    def emit(self):
        nc = self.nc
        for op in self.ops:
            for p in op.deps:
                if p.dsem is None:
                    if p.eng == "pe" and op.eng == "pe":
                        continue
                    p.signal = True
        cnt = {e: 0 for e in ENGS}
        dcnt = {n: 0 for n in self.dsem_names}
        for op in self.ops:
            if op.dsem is not None:
                dcnt[op.dsem] += 16
                op.dval = dcnt[op.dsem]
            elif op.signal:
                cnt[op.eng] += 1
                op.sigval = cnt[op.eng]
        sems = {}
        for e in ENGS:
            sems[e] = nc.alloc_semaphore(f"s_{e}")
        for n in self.dsem_names:
            sems[n] = nc.alloc_semaphore(f"d_{n}")
        known = {e: {} for e in ENGS}
        nw = 0
        for op in self.ops:
            eo = self.eng_objs[op.eng]
            need = {}
            for p in op.deps:
                if p.dsem is not None:
                    key = p.dsem
                    val = dcnt[key] if key in self.group_dsems else p.dval
                else:
                    if p.sigval is None or (p.eng == "pe" and op.eng == "pe"):
                        continue
                    key = p.eng
                    val = p.sigval
                if need.get(key, 0) < val:
                    need[key] = val
            kn = known[op.eng]
            for key, val in need.items():
                if kn.get(key, 0) >= val:
                    continue
                eo.wait_ge(sems[key], val)
                kn[key] = val
                nw += 1
            ins = op.fn()
            if op.dsem is not None:
                ins.then_inc(sems[op.dsem], 16)
            elif op.signal:
                ins.then_inc(sems[op.eng], 1)
        self.nwaits = nw
        self.sems = sems
        self.dtotal = dcnt

    def mm(self, out, lhsT, rhs, start=True, stop=True):
        nc = self.nc
        return self.add("pe", lambda: nc.tensor.matmul(out.ap, lhsT.ap, rhs.ap, start=start, stop=stop),
                        reads=[lhsT, rhs], writes=[out])

    def transpose(self, out, in_, ident):
        nc = self.nc
        return self.add("pe", lambda: nc.tensor.transpose(out.ap, in_.ap, ident.ap),
                        reads=[in_, ident], writes=[out])

    def act(self, out, in_, func, bias=None, scale=None):
        nc = self.nc
        kw = {}
        if bias is not None:
            kw["bias"] = bias.ap if isinstance(bias, View) else bias
        if scale is not None:
            kw["scale"] = scale.ap if isinstance(scale, View) else scale
        return self.add("act", lambda: nc.scalar.activation(out.ap, in_.ap, func, **kw),
                        reads=[in_, bias, scale], writes=[out])

    def _veng(self, eng):
        return self.nc.vector if eng == "dve" else self.nc.gpsimd

    def tt(self, out, in0, in1, op, eng="dve"):
        e = self._veng(eng)
        return self.add(eng, lambda: e.tensor_tensor(out.ap, in0.ap, in1.ap, op), reads=[in0, in1], writes=[out])

    def ts(self, out, in0, s1, s2, op0, op1=None, eng="dve"):
        e = self._veng(eng)
        a1 = s1.ap if isinstance(s1, View) else s1
        a2 = s2.ap if isinstance(s2, View) else s2
        if op1 is None:
            return self.add(eng, lambda: e.tensor_single_scalar(out.ap, in0.ap, a1, op0), reads=[in0, s1], writes=[out])
        return self.add(eng, lambda: e.tensor_scalar(out.ap, in0.ap, a1, a2, op0, op1), reads=[in0, s1, s2], writes=[out])

    def stt(self, out, in0, scalar, in1, op0, op1):
        nc = self.nc
        a = scalar.ap if isinstance(scalar, View) else scalar
        return self.add("dve", lambda: nc.vector.scalar_tensor_tensor(out.ap, in0.ap, a, in1.ap, op0, op1),
                        reads=[in0, scalar, in1], writes=[out])

    def copy(self, out, in_, eng="dve"):
        if eng == "act":
            nc = self.nc
            return self.add("act", lambda: nc.scalar.copy(out.ap, in_.ap), reads=[in_], writes=[out])
        e = self._veng(eng)
        return self.add(eng, lambda: e.tensor_copy(out.ap, in_.ap), reads=[in_], writes=[out])

    def memset(self, out, val, eng="dve"):
        e = self._veng(eng)
        return self.add(eng, lambda: e.memset(out.ap, val), writes=[out])

    def scan(self, out, d0, d1, init, op0, op1):
        nc = self.nc
        return self.add("dve", lambda: nc.vector.tensor_tensor_scan(out.ap, d0.ap, d1.ap, init, op0, op1),
                        reads=[d0, d1], writes=[out])

    def reduce(self, out, in_, op):
        nc = self.nc
        return self.add("dve", lambda: nc.vector.tensor_reduce(out.ap, in_.ap, AX.X, op), reads=[in_], writes=[out])

    def recip(self, out, in_):
        nc = self.nc
        return self.add("dve", lambda: nc.vector.reciprocal(out.ap, in_.ap), reads=[in_], writes=[out])

    def bn_stats(self, out, in_):
        nc = self.nc
        return self.add("dve", lambda: nc.vector.bn_stats(out.ap, in_.ap), reads=[in_], writes=[out])

    def bn_aggr(self, out, in_):
        nc = self.nc
        return self.add("dve", lambda: nc.vector.bn_aggr(out.ap, in_.ap), reads=[in_], writes=[out])

    def dma(self, out, in_, eng="sp", dsem=None):
        eo = self.eng_objs[eng]
        oap = out.ap if isinstance(out, View) else out
        iap = in_.ap if isinstance(in_, View) else in_
        if dsem is None:
            sb = out if (isinstance(out, View) and out.tile is not None and not getattr(out.tile, "is_dram", False)) else in_
            dsem = self.tile_dsem(sb.tile)
        return self.add(eng, lambda: eo.dma_start(out=oap, in_=iap), reads=[in_], writes=[out], dsem=dsem)


class Pool:
    def __init__(self, tiles):
        self.free = collections.deque(tiles)
        self.n = len(tiles)
        self.low = len(tiles)

    def get(self):
        assert self.free, "pool exhausted"
        t = self.free.popleft()
        self.low = min(self.low, len(self.free))
        return t

    def put(self, *ts):
        for t in ts:
            self.free.append(t)


def kmajor_slots(W, ncols, col_idx=None):
    K, N = W.shape
    KC = K // 128
    if col_idx is not None:
        W = W[:, col_idx]
        N = W.shape[1]
    NS = N // ncols
    A = W.reshape(KC, 128, NS, ncols).transpose(2, 1, 0, 3)
    return np.ascontiguousarray(A).reshape(NS, 128, KC * ncols)


def fm_cols(v):
    v = np.asarray(v, np.float32)
    lead = v.shape[:-1]
    C = v.shape[-1] // 128
    A = v.reshape(*lead, C, 128)
    A = np.moveaxis(A, -1, 0)
    return np.ascontiguousarray(A).reshape(128, -1)


class PV:
    def __init__(self):
        self.cols = []
        self.off = {}
        self.n = 0

    def add(self, name, arr):
        arr = np.asarray(arr, np.float32)
        assert arr.shape[0] == 128
        self.off[name] = self.n
        self.cols.append(arr)
        self.n += arr.shape[1]

    def build(self):
        return np.ascontiguousarray(np.concatenate(self.cols, axis=1))


def blockdiag_chunks(w):
    out = np.zeros((16, 128, 128), np.float32)
    wr = w.reshape(16, 32, 4, 4)
    for b in range(32):
        out[:, 4 * b:4 * b + 4, 4 * b:4 * b + 4] = wr[:, b]
    return out


def host_prep(inp, layers):
    pv = PV()
    shared = {}
    ffn_cols = np.concatenate([np.concatenate([np.arange(j * 128, (j + 1) * 128),
                                               DFF + np.arange(j * 128, (j + 1) * 128)]) for j in range(FC)])
    conf_cols = np.concatenate([np.concatenate([np.arange(j * 128, (j + 1) * 128),
                                                D + np.arange(j * 128, (j + 1) * 128)]) for j in range(DC)])
    for l in layers:
        pv.add(f"adab{l}", fm_cols(inp["ada_b"][l]))
        pv.add(f"gmix{l}", fm_cols(inp["norm_mix_g"][l]))
        pv.add(f"gffn{l}", fm_cols(inp["norm_ffn_g"][l]))
        shared[f"ada{l}"] = kmajor_slots(inp["ada_w"][l], 256)
        shared[f"fup{l}"] = kmajor_slots(inp["ffn_w_up"][l], 256, ffn_cols)
        fd = inp["ffn_w_down"][l]
        A = fd.reshape(2, 11, 128, DC, 128).transpose(3, 0, 2, 1, 4)
        shared[f"fdn{l}"] = np.ascontiguousarray(A).reshape(16, 128, 11 * 128)
        cw = inp["ffn_conv_w"][l]
        cwg = fm_cols(cw[:, :DFF])
        cwv = fm_cols(cw[:, DFF:])
        pv.add(f"fcwg{l}", cwg)
        pv.add(f"fcwv{l}", cwv)
        pv.add(f"fcbg{l}", fm_cols(inp["ffn_conv_b"][l][:DFF]))
        pv.add(f"fcbv{l}", fm_cols(inp["ffn_conv_b"][l][DFF:]))
        j = l // 2
        if l % 2 == 0:
            shared[f"mup{l}"] = kmajor_slots(inp["mlstm_w_up"][j], 256)
            shared[f"mdn{l}"] = kmajor_slots(inp["mlstm_w_down"][j], 128)
            bq = blockdiag_chunks(inp["mlstm_wq"][j])
            bk = blockdiag_chunks(inp["mlstm_wk"][j])
            bv = blockdiag_chunks(inp["mlstm_wv"][j])
            B = np.stack([bq, bk, bv], axis=1)
            B = B.reshape(4, 4, 3, 128, 128).transpose(0, 3, 1, 2, 4)
            shared[f"wbd{l}"] = np.ascontiguousarray(B).reshape(4, 128, 4 * 3 * 128)
            wif = inp["mlstm_w_if"][j]
            W = np.zeros((48, 128, 36), np.float32)
            W[:, :, 0:4] = wif[:, 0:4].reshape(48, 128, 4)
            W[:, :, 32:36] = wif[:, 4:8].reshape(48, 128, 4)
            shared[f"wif{l}"] = np.ascontiguousarray(W.transpose(1, 0, 2)).reshape(1, 128, 48 * 36)
            pv.add(f"mcw{l}", fm_cols(inp["mlstm_conv_w"][j]))
            pv.add(f"mcb{l}", fm_cols(inp["mlstm_conv_b"][j]))
            pv.add(f"mlnw{l}", fm_cols(inp["mlstm_ln_w"][j]))
            pv.add(f"mskip{l}", fm_cols(inp["mlstm_skip"][j]))
            bif = np.zeros((128, 2), np.float32)
            bif[0:4, 0] = inp["mlstm_b_if"][j][0:4]
            bif[0:4, 1] = inp["mlstm_b_if"][j][4:8]
            pv.add(f"mbif{l}", bif)
        else:
            shared[f"cp1{l}"] = kmajor_slots(inp["conf_w_pw1"][j], 256, conf_cols)
            shared[f"cp2{l}"] = kmajor_slots(inp["conf_w_pw2"][j], 256)
            pv.add(f"cb1a{l}", fm_cols(inp["conf_b_pw1"][j][:D]))
            pv.add(f"cb1g{l}", fm_cols(inp["conf_b_pw1"][j][D:]))
            pv.add(f"cdw{l}", fm_cols(inp["conf_dw_w"][j]))
            pv.add(f"cdb{l}", fm_cols(inp["conf_dw_b"][j]))
            pv.add(f"clg{l}", fm_cols(inp["conf_ln_g"][j]))
            pv.add(f"clb{l}", fm_cols(inp["conf_ln_b"][j]))
            pv.add(f"cb2{l}", fm_cols(inp["conf_b_pw2"][j]))
    pv.add("fing", fm_cols(inp["final_g"]))
    ident = np.eye(128, dtype=np.float32)
    mask = np.triu(np.ones((128, 128), np.float32))
    self_ = np.zeros((128, 4), np.float32)
    for jj in range(4):
        self_[32 + jj, jj] = 1.0
    shared["consts"] = np.ascontiguousarray(np.concatenate([ident, mask, self_], axis=1))
    shared["pvec"] = pv.build()
    return shared, pv.off, pv.n


class Builder:
    def __init__(self, nt, layers, shared_shapes, pvoff, pvn, final_norm=True):
        self.nt = nt
        self.layers = layers
        self.final_norm = final_norm
        self.pvoff = pvoff
        nc = bass.Bass("TRN2", target_bir_lowering=False)
        self.nc = nc
        self.P = Prog(nc)
        P = self.P
        S = nt * T
        self.x_d = nc.dram_tensor("x", [128, DC, S], F32, kind="ExternalInput").ap()
        self.c_d = nc.dram_tensor("c", [128, DC], F32, kind="ExternalInput").ap()
        self.out_d = nc.dram_tensor("out", [128, DC, S], F32, kind="ExternalOutput").ap()
        self.w_d = {}
        for name, shp in shared_shapes.items():
            self.w_d[name] = nc.dram_tensor(name, list(shp), F32, kind="ExternalInput").ap()

        def sb(name, shape, dt=F32, nparts=1):
            return Tile(name, nc.alloc_sbuf_tensor(name, shape, dt), nparts)

        self.sb = sb
        self.X = sb("X", [128, DC, T], F32, nparts=DC)
        self.PVEC = sb("PVEC", [128, pvn], F32)
        self.CONST = sb("CONST", [128, 260], F32)
        self.PVECB = sb("PVECB", [128, pvn], BF16)
        self.IDB = sb("IDB", [128, 128], BF16)
        self.ONESB = sb("ONESB", [128, 128], BF16)
        self.ONESF = sb("ONESF", [128, 512], F32)
        self.CACT = sb("CACT", [128, DC], BF16)
        self.CIN = sb("CIN", [128, DC], F32)
        self.MOD = {l: sb(f"MOD{l}", [128, 48], F32) for l in layers}
        self.AB = {l: sb(f"AB{l}", [128, 16], F32) for l in layers}
        self.HALO_F = {l: sb(f"HALOF{l}", [128, 2 * FC, 2], BF16) for l in layers}
        self.HALO_M = {l: sb(f"HALOM{l}", [128, IC, 4], BF16) for l in layers if l % 2 == 0}
        self.HALO_C = {l: sb(f"HALOC{l}", [128, DC, 30], BF16) for l in layers if l % 2 == 1}
        self.NST = {l: sb(f"NST{l}", [128, IC], F32) for l in layers if l % 2 == 0}
        self.MST = {l: sb(f"MST{l}", [4, 1], F32) for l in layers if l % 2 == 0}
        self.WIF = sb("WIF", [128, 48 * 36], BF16)
        self.MASKK = sb("MASKK", [128, 128], F32)
        self.CST = {}
        for l in layers:
            if l % 2 == 0:
                for h in range(NH):
                    t = Tile(f"CST{l}_{h}", nc.dram_tensor(f"cst{l}_{h}", [4, 128, T], F32, kind="Internal"))
                    t.is_dram = True
                    self.CST[(l, h)] = t
        self.RING = [sb(f"RING{i}", [128, SLOT], BF16) for i in range(NRING)]
        self.ring_i = 0
        NB, NF = 100, 14
        self.bpool = Pool([sb(f"B{i}", [128, HB + T], BF16) for i in range(NB)])
        self.fpool = Pool([sb(f"F{i}", [128, HF + T], F32) for i in range(NF)])
        self.banks = Pool([Tile(f"PS{i}", nc.alloc_psum_tensor(f"PS{i}", [128, 512], F32)) for i in range(8)])
        self.small_i = 0
        self.smalls = [sb(f"SMALL{i}", [128, 16], F32) for i in range(24)]

    def dump(self, name, view, shape, dt=F32):
        import os
        if not os.environ.get("MDUMP"):
            return
        if not hasattr(self, "dbg_dsem"):
            self.dbg_dsem = self.P.new_dsem("dbg", group=True)
            self.dumps = []
        if name in self.dumps:
            return
        self.dumps.append(name)
        d = self.nc.dram_tensor(name, list(shape), dt, kind="ExternalOutput").ap()
        self.P.dma(d, view, eng="sp", dsem=self.dbg_dsem)

    def small(self):
        t = self.smalls[self.small_i % len(self.smalls)]
        self.small_i += 1
        return t

    def pcol(self, name, c0, n=1):
        o = self.pvoff[name] + c0
        return self.PVEC[:, o:o + n]

    def wload(self, name, s, nelem):
        slot = self.RING[self.ring_i % NRING]
        self.ring_i += 1
        self.P.dma(slot[:, 0:nelem], self.w_d[name][s, :, :], eng="pool")
        return slot

    def prologue(self):
        P = self.P
        P.dma(self.PVEC[:, :], self.w_d["pvec"], eng="sp")
        P.dma(self.CONST[:, :], self.w_d["consts"], eng="sp")
        P.dma(self.CIN[:, :], self.c_d, eng="sp")
        P.copy(self.IDB[:, :], self.CONST[:, 0:128])
        P.copy(self.PVECB[:, :], self.PVEC[:, :])
        P.ts(self.MASKK[:, :], self.CONST[:, 128:256], float(512 ** -0.5), None, ALU.mult)
        P.memset(self.ONESB[:, :], 1.0)
        P.memset(self.ONESF[:, :], 1.0, eng="pool")
        P.act(self.CACT[:, :], self.CIN[:, :], AF.Silu)
        for l in self.layers:
            P.memset(self.HALO_F[l][:, :, :], 0.0, eng="pool")
            if l % 2 == 0:
                P.memset(self.HALO_M[l][:, :, :], 0.0, eng="pool")
                P.memset(self.NST[l][:, :], 0.0, eng="pool")
                P.memset(self.MST[l][:, :], 0.0, eng="pool")
            else:
                P.memset(self.HALO_C[l][:, :, :], 0.0, eng="pool")

    def ada(self, l):
        P = self.P
        bank = self.banks.get()
        for s in range(24):
            slot = self.wload(f"ada{l}", s, 2048)
            w = slot.t[:, :].rearrange("p (k n) -> p k n", k=DC)
            for jj in range(2):
                oc = 2 * s + jj
                for kc in range(DC):
                    P.mm(bank[:, oc:oc + 1], slot.v(w[:, kc, jj * 128:(jj + 1) * 128]), self.CACT[:, kc:kc + 1],
                         start=(kc == 0), stop=(kc == DC - 1))
        M = self.MOD[l]
        P.tt(M[:, :], bank[:, 0:48], self.pcol(f"adab{l}", 0, 48), ALU.add)
        self.banks.put(bank)
        AB = self.AB[l]
        P.stt(AB[:, 0:8], M[:, 8:16], 1.0, self.pcol(f"gmix{l}", 0, 8), ALU.add, ALU.mult)
        P.stt(AB[:, 8:16], M[:, 32:40], 1.0, self.pcol(f"gffn{l}", 0, 8), ALU.add, ALU.mult)

    def rstd_from_bank(self, bank):
        P = self.P
        r = self.fpool.get()
        P.ts(r[:, 0:T], bank[:, 0:T], 1.0 / D, EPS, ALU.mult, ALU.add)
        P.act(r[:, 0:T], r[:, 0:T], AF.Sqrt)
        P.recip(r[:, 0:T], r[:, 0:T])
        return r

    def sumsq(self):
        P = self.P
        bank = self.banks.get()
        for dc in range(DC):
            sq = self.bpool.get()
            P.act(sq[:, 0:T], self.X[:, dc, :], AF.Square)
            P.mm(bank[:, 0:T], self.ONESB[:, :], sq[:, 0:T], start=(dc == 0), stop=(dc == DC - 1))
            self.bpool.put(sq)
        r = self.rstd_from_bank(bank)
        self.banks.put(bank)
        return r

    def norm_mod(self, l, which):
        P = self.P
        r = self.sumsq()
        a0 = 0 if which == 0 else 8
        sh0 = 0 if which == 0 else 24
        H = []
        for dc in range(DC):
            tmp = self.fpool.get()
            P.tt(tmp[:, 0:T], self.X[:, dc, :], r[:, 0:T], ALU.mult)
            h = self.bpool.get()
            P.act(h[:, 0:T], tmp[:, 0:T], AF.Identity, bias=self.MOD[l][:, sh0 + dc:sh0 + dc + 1],
                  scale=self.AB[l][:, a0 + dc:a0 + dc + 1])
            self.fpool.put(tmp)
            H.append(h)
        self.fpool.put(r)
        return H


    def conv_pe(self, bank, wname, c0, cstep, rhs_list):
        P = self.P
        nc = self.nc
        n = len(rhs_list)
        idb = self.IDB.t[:, :]
        pb = self.PVECB.t[:, :]
        slabs = []
        for k0 in range(0, n, 4):
            K = min(4, n - k0)
            dslab = self.bpool.get()
            slabs.append(dslab)
            out3 = dslab.t[:, 0:K * 128].rearrange("p (k m) -> p k m", k=K)
            in0 = bass.AP(idb.tensor, idb.offset, [list(idb.ap[0]), [0, K], [1, 128]])
            col = self.pvoff[wname] + c0 + k0 * cstep
            wv = self.PVECB.t[:, col:col + 1]
            in1 = bass.AP(wv.tensor, wv.offset, [list(wv.ap[0]), [cstep, K], [0, 128]])
            P.add("dve", (lambda o=out3, a=in0, b_=in1: nc.vector.tensor_tensor(o, a, b_, ALU.mult)),
                  reads=[self.IDB[:, :], self.PVECB[:, :]], writes=[dslab[:, :]])
        for k, rhs in enumerate(rhs_list):
            dslab = slabs[k // 4]
            dg = dslab[:, (k % 4) * 128:(k % 4 + 1) * 128]
            P.mm(bank[:, 0:T], dg, rhs, start=(k == 0), stop=(k == n - 1))
        self.bpool.put(*slabs)

    def ffn(self, l, ti):
        P = self.P
        H = self.norm_mod(l, 1)
        HID = []
        halo = self.HALO_F[l]
        def stage1(j):
            slot = self.wload(f"fup{l}", j, 2048)
            w = slot.t[:, :].rearrange("p (k n) -> p k n", k=DC)
            us = []
            for gv in range(2):
                bank = self.banks.get()
                for kc in range(DC):
                    P.mm(bank[:, 0:T], slot.v(w[:, kc, gv * 128:(gv + 1) * 128]), H[kc][:, 0:T],
                         start=(kc == 0), stop=(kc == DC - 1))
                u = self.bpool.get()
                hj = 2 * j + gv
                P.copy(u[:, 0:2], halo[:, hj, :], eng="act")
                P.copy(u[:, 2:2 + T], bank[:, 0:T], eng="act")
                self.banks.put(bank)
                P.copy(halo[:, hj, :], u[:, T:T + 2])
                us.append(u)
            return us

        def stage2(j, us):
            cb = []
            for gv in range(2):
                nm = "fcwg" if gv == 0 else "fcwv"
                u = us[gv]
                bank = self.banks.get()
                self.conv_pe(bank, f"{nm}{l}", j, FC, [u[:, k:k + T] for k in range(3)])
                cb.append(bank)
            self.bpool.put(*us)
            sg = self.fpool.get()
            P.act(sg[:, 0:T], cb[0][:, 0:T], AF.Silu, bias=self.pcol(f"fcbg{l}", j))
            self.banks.put(cb[0])
            hid = self.bpool.get()
            P.stt(hid[:, 0:T], cb[1][:, 0:T], self.pcol(f"fcbv{l}", j), sg[:, 0:T], ALU.add, ALU.mult)
            self.banks.put(cb[1])
            self.fpool.put(sg)
            HID.append(hid)

        prev = None
        for j in range(FC):
            us = stage1(j)
            if prev is not None:
                stage2(*prev)
            prev = (j, us)
        stage2(*prev)
        self.bpool.put(*H)
        for dc in range(DC):
            bank = self.banks.get()
            for half in range(2):
                slot = self.wload(f"fdn{l}", dc * 2 + half, 11 * 128)
                w = slot.t[:, 0:11 * 128].rearrange("p (k n) -> p k n", k=11)
                for k in range(11):
                    kc = half * 11 + k
                    P.mm(bank[:, 0:T], slot.v(w[:, k, :]), HID[kc][:, 0:T], start=(kc == 0), stop=(kc == FC - 1))
            P.stt(self.X[:, dc, :], bank[:, 0:T], self.MOD[l][:, 40 + dc:41 + dc], self.X[:, dc, :], ALU.mult, ALU.add)
            self.banks.put(bank)
        self.bpool.put(*HID)

    def conformer(self, l, ti):
        P = self.P
        H = self.norm_mod(l, 0)
        halo = self.HALO_C[l]
        ACC = []
        mbank = self.banks.get()
        sbank = self.banks.get()
        for j in range(DC):
            slot = self.wload(f"cp1{l}", j, 2048)
            w = slot.t[:, :].rearrange("p (k n) -> p k n", k=DC)
            ba = self.banks.get()
            bg = self.banks.get()
            for kc in range(DC):
                P.mm(ba[:, 0:T], slot.v(w[:, kc, 0:128]), H[kc][:, 0:T], start=(kc == 0), stop=(kc == DC - 1))
            for kc in range(DC):
                P.mm(bg[:, 0:T], slot.v(w[:, kc, 128:256]), H[kc][:, 0:T], start=(kc == 0), stop=(kc == DC - 1))
            sig = self.fpool.get()
            P.act(sig[:, 0:T], bg[:, 0:T], AF.Sigmoid, bias=self.pcol(f"cb1g{l}", j))
            self.banks.put(bg)
            uf = self.fpool.get()
            ub16 = uf.t[:, :].bitcast(BF16)
            P.copy(uf.v(ub16[:, 2:32]), halo[:, j, :], eng="act")
            P.stt(uf.v(ub16[:, 32:32 + T]), ba[:, 0:T], self.pcol(f"cb1a{l}", j), sig[:, 0:T], ALU.add, ALU.mult)
            self.banks.put(ba)
            self.fpool.put(sig)
            P.copy(halo[:, j, :], uf.v(ub16[:, 2 + T:32 + T]), eng="act")
            cbank = self.banks.get()
            self.conv_pe(cbank, f"cdw{l}", j, DC, [uf.v(ub16[:, 2 + k:2 + k + T]) for k in range(CONF_K)])
            self.fpool.put(uf)
            acc = self.fpool.get()
            P.act(acc[:, 0:T], cbank[:, 0:T], AF.Identity, bias=self.pcol(f"cdb{l}", j))
            self.banks.put(cbank)
            ub = self.bpool.get()
            us = self.bpool.get()
            P.copy(ub[:, 0:T], acc[:, 0:T], eng="act")
            P.act(us[:, 0:T], acc[:, 0:T], AF.Square)
            P.mm(mbank[:, 0:T], self.ONESB[:, :], ub[:, 0:T], start=(j == 0), stop=(j == DC - 1))
            P.mm(sbank[:, 0:T], self.ONESB[:, :], us[:, 0:T], start=(j == 0), stop=(j == DC - 1))
            self.bpool.put(ub, us)
            ACC.append(acc)
        self.bpool.put(*H)
        mean = self.fpool.get()
        rstd = self.fpool.get()
        P.ts(mean[:, 0:T], mbank[:, 0:T], 1.0 / D, None, ALU.mult)
        P.tt(rstd[:, 0:T], mean[:, 0:T], mean[:, 0:T], ALU.mult)
        P.stt(rstd[:, 0:T], sbank[:, 0:T], 1.0 / D, rstd[:, 0:T], ALU.mult, ALU.subtract)
        P.ts(rstd[:, 0:T], rstd[:, 0:T], EPS, None, ALU.add)
        P.act(rstd[:, 0:T], rstd[:, 0:T], AF.Sqrt)
        P.recip(rstd[:, 0:T], rstd[:, 0:T])
        self.banks.put(mbank, sbank)
        Y = []
        for j in range(DC):
            acc = ACC[j]
            P.tt(acc[:, 0:T], acc[:, 0:T], mean[:, 0:T], ALU.subtract)
            P.tt(acc[:, 0:T], acc[:, 0:T], rstd[:, 0:T], ALU.mult)
            y = self.bpool.get()
            P.act(y[:, 0:T], acc[:, 0:T], AF.Silu, bias=self.pcol(f"clb{l}", j), scale=self.pcol(f"clg{l}", j))
            self.fpool.put(acc)
            Y.append(y)
        self.fpool.put(mean, rstd)
        for s in range(4):
            slot = self.wload(f"cp2{l}", s, 2048)
            w = slot.t[:, :].rearrange("p (k n) -> p k n", k=DC)
            for jj in range(2):
                dc = 2 * s + jj
                bank = self.banks.get()
                for kc in range(DC):
                    P.mm(bank[:, 0:T], slot.v(w[:, kc, jj * 128:(jj + 1) * 128]), Y[kc][:, 0:T],
                         start=(kc == 0), stop=(kc == DC - 1))
                tmp = self.fpool.get()
                P.ts(tmp[:, 0:T], bank[:, 0:T], self.pcol(f"cb2{l}", dc), self.MOD[l][:, 16 + dc:17 + dc], ALU.add, ALU.mult)
                self.banks.put(bank)
                P.tt(self.X[:, dc, :], self.X[:, dc, :], tmp[:, 0:T], ALU.add)
                self.fpool.put(tmp)
        self.bpool.put(*Y)

    def mlstm(self, l, ti):
        P = self.P
        nc = self.nc
        H = self.norm_mod(l, 0)
        halo = self.HALO_M[l]
        gbank = self.banks.get()
        wif = self.WIF
        self.P.dma(wif[:, 0:48 * 36], self.w_d[f"wif{l}"][0, :, :], eng="pool")
        wifv = wif.t[:, 0:48 * 36].rearrange("p (c m) -> p c m", c=48)
        kscale = float(512 ** -0.5)
        wbd = None
        XM = [None] * IC
        XC = [None] * IC
        Q = [None] * IC
        KF = [None] * IC
        st = {}

        def stageA(c):
            if c % 2 == 0:
                st["upslot"] = self.wload(f"mup{l}", c // 2, 2048)
            upslot = st["upslot"]
            upw = upslot.t[:, :].rearrange("p (k n) -> p k n", k=DC)
            jj = c % 2
            bank = self.banks.get()
            for kc in range(DC):
                P.mm(bank[:, 0:T], upslot.v(upw[:, kc, jj * 128:(jj + 1) * 128]), H[kc][:, 0:T],
                     start=(kc == 0), stop=(kc == DC - 1))
            xm = self.bpool.get()
            P.copy(xm[:, HB - 4:HB], halo[:, c, :], eng="act")
            P.copy(xm[:, HB:HB + T], bank[:, 0:T], eng="act")
            self.banks.put(bank)
            P.copy(halo[:, c, :], xm[:, HB + T - 4:HB + T])
            XM[c] = xm

        def stageB(c):
            xm = XM[c]
            cbank = self.banks.get()
            self.conv_pe(cbank, f"mcw{l}", c, IC, [xm[:, HB - 3 + k:HB - 3 + k + T] for k in range(4)])
            xc = self.bpool.get()
            P.act(xc[:, 0:T], cbank[:, 0:T], AF.Silu, bias=self.pcol(f"mcb{l}", c))
            self.banks.put(cbank)
            XC[c] = xc

        def stageC(c):
            xm, xc = XM[c], XC[c]
            if c % 4 == 0:
                st["wbd"] = self.wload(f"wbd{l}", c // 4, 1536)
            wbd = st["wbd"]
            wbdv = wbd.t[:, 0:1536].rearrange("p (c q m) -> p c q m", c=4, q=3)
            cc = c % 4
            bq = self.banks.get()
            P.mm(bq[:, 0:T], wbd.v(wbdv[:, cc, 0, :]), xc[:, 0:T])
            bk = self.banks.get()
            P.mm(bk[:, 0:T], wbd.v(wbdv[:, cc, 1, :]), xc[:, 0:T])
            bv = self.banks.get()
            P.mm(bv[:, 0:T], wbd.v(wbdv[:, cc, 2, :]), xm[:, HB:HB + T])
            q = self.bpool.get()
            P.copy(q[:, 0:T], bq[:, 0:T], eng="act")
            self.banks.put(bq)
            kf = self.bpool.get()
            P.copy(kf[:, 0:T], bk[:, 0:T])
            self.banks.put(bk)
            vf = self.bpool.get()
            P.copy(vf[:, 0:T], bv[:, 0:T], eng="act")
            self.banks.put(bv)
            P.mm(gbank[0:36, 0:T], wif.v(wifv[:, c, :]), q[:, 0:T], start=(c == 0), stop=False)
            P.mm(gbank[0:36, 0:T], wif.v(wifv[:, 16 + c, :]), kf[:, 0:T], start=False, stop=False)
            P.mm(gbank[0:36, 0:T], wif.v(wifv[:, 32 + c, :]), vf[:, 0:T], start=False, stop=(c == IC - 1))
            self.bpool.put(vf)
            Q[c] = q
            KF[c] = kf
            if c == 0:
                self.dump("d_xm0", xm[:, HB:HB + T], [128, T], BF16)
                self.dump("d_xc0", xc[:, 0:T], [128, T], BF16)
                self.dump("d_q0", q[:, 0:T], [128, T], BF16)
                self.dump("d_k0", kf[:, 0:T], [128, T], BF16)

        for c in range(IC + 2):
            if c < IC:
                stageA(c)
            if 0 <= c - 1 < IC:
                stageB(c - 1)
            if 0 <= c - 2 < IC:
                stageC(c - 2)
        import os
        DBG = int(os.environ.get("MDBG", "99"))
        if DBG == 1:
            return
        GS = self.fpool.get()
        P.copy(GS[0:36, 0:T], gbank[0:36, 0:T], eng="act")
        self.dump("d_gs", GS[0:36, 0:T], [36, T])
        P.mm(gbank[0:4, 0:T], self.CONST[0:36, 256:260], GS[0:36, 0:T])
        ipre = self.fpool.get()
        ob = self.pvoff[f"mbif{l}"]
        P.ts(ipre[0:4, 0:T], GS[0:4, 0:T], self.PVEC[0:4, ob:ob + 1], None, ALU.add)
        lf = self.fpool.get()
        P.act(lf[0:4, 0:T], gbank[0:4, 0:T], AF.Sigmoid, bias=self.PVEC[0:4, ob + 1:ob + 2])
        self.banks.put(gbank)
        self.fpool.put(GS)
        P.act(lf[0:4, 0:T], lf[0:4, 0:T], AF.Ln)
        g = self.fpool.get()
        P.scan(g[0:4, 0:T], self.ONESF[0:4, 0:T], lf[0:4, 0:T], 0.0, ALU.mult, ALU.add)
        self.fpool.put(lf)
        a = ipre
        P.tt(a[0:4, 0:T], ipre[0:4, 0:T], g[0:4, 0:T], ALU.subtract)
        sm = self.small()
        mst = self.MST[l]
        P.reduce(sm[0:4, 0:4], a.v(a.t[0:4, 0:T].rearrange("p (b t) -> p b t", b=4)), ALU.max)
        P.tt(sm[0:4, 4:5], sm[0:4, 0:1], mst[0:4, 0:1], ALU.max)
        for lb in range(1, 4):
            P.tt(sm[0:4, 4 + lb:5 + lb], sm[0:4, lb:lb + 1], sm[0:4, 3 + lb:4 + lb], ALU.max)
        P.ts(sm[0:4, 8:12], sm[0:4, 4:8], -1.0, None, ALU.mult)
        P.act(sm[0:4, 12:16], sm[0:4, 8:12], AF.Exp, bias=mst[0:4, 0:1])
        E = []
        fl = self.fpool.get()
        for lb in range(4):
            e = self.fpool.get()
            n = 128 * (lb + 1)
            P.act(e[0:4, 0:n], a[0:4, 0:n], AF.Exp, bias=sm[0:4, 8 + lb:9 + lb])
            P.act(fl[0:4, lb * 128:n], g[0:4, lb * 128:n], AF.Exp, bias=sm[0:4, 8 + lb:9 + lb], scale=-1.0)
            E.append(e)
        self.dump("d_e", E[3][0:4, 0:T], [4, T])
        self.dump("d_fl", fl[0:4, 0:T], [4, T])
        self.dump("d_g", g[0:4, 0:T], [4, T])
        self.dump("d_sm", sm[0:4, 0:16], [4, 16])
        P.tt(mst[0:4, 0:1], g[0:4, T - 1:T], sm[0:4, 7:8], ALU.add)
        self.fpool.put(g, a)
        if DBG == 2:
            return
        tb = self.banks.get()
        for lb in range(4):
            for sbi in range(lb + 1):
                c0 = (lb * 4 + sbi) * 4
                P.transpose(tb[:, c0:c0 + 4], E[lb][0:4, sbi * 128:(sbi + 1) * 128], self.CONST[0:4, 0:4])
            P.transpose(tb[:, 64 + lb * 4:68 + lb * 4], fl[0:4, lb * 128:(lb + 1) * 128], self.CONST[0:4, 0:4])
        dg = self.small()
        for lb in range(4):
            P.ts(dg[0:4, lb * 4:lb * 4 + 4], self.CONST[0:4, 0:4], sm[0:4, 12 + lb:13 + lb], None, ALU.mult)
        P.mm(tb[:, 80:96], self.ONESF[0:4, 0:128], dg[0:4, 0:16])
        TM = self.small_big()
        P.memset(TM[:, 0:64], 0.0)
        for lb in range(4):
            c0 = lb * 16
            P.copy(TM[:, c0:c0 + 4 * (lb + 1)], tb[:, c0:c0 + 4 * (lb + 1)])
        P.copy(TM[:, 64:96], tb[:, 64:96])
        self.banks.put(tb)
        self.fpool.put(fl, *E)
        self.dump("d_tm", TM[:, 0:96], [128, 96])
        ETM = lambda lb, sbi, h: TM[:, (lb * 4 + sbi) * 4 + h:(lb * 4 + sbi) * 4 + h + 1]
        WCB = lambda h: TM[:, 80 + 12 + h:80 + 13 + h]
        nst = self.NST[l]
        NB = self.small_bf()
        P.copy(NB[:, 0:16], nst[:, 0:16])
        if DBG == 3:
            return
        for h in range(NH):
            wbd = self.wload(f"wbd{l}", h, 1536)
            wbdv = wbd.t[:, 0:1536].rearrange("p (c q m) -> p c q m", c=4, q=3)
            KTM, VTM = [], []
            for sbi in range(4):
                bk = self.banks.get()
                bv = self.banks.get()
                for j in range(4):
                    c = 4 * h + j
                    P.mm(bk[:, j * 128:(j + 1) * 128], XC[c][:, sbi * 128:(sbi + 1) * 128], wbd.v(wbdv[:, j, 1, :]))
                    P.mm(bv[:, j * 128:(j + 1) * 128], XM[c][:, HB + sbi * 128:HB + (sbi + 1) * 128], wbd.v(wbdv[:, j, 2, :]))
                kt = self.bpool.get()
                vt = self.bpool.get()
                P.ts(kt[:, 0:T], bk[:, 0:T], ETM(3, sbi, h), kscale, ALU.mult, ALU.mult)
                P.copy(vt[:, 0:T], bv[:, 0:T], eng="act")
                self.banks.put(bk, bv)
                KTM.append(kt); VTM.append(vt)
            if DBG == 4:
                return
            C32, CB = [], []
            cst = self.CST[(l, h)]
            for kc in range(4):
                c32 = self.fpool.get()
                if ti == 0:
                    P.memset(c32[:, 0:T], 0.0)
                else:
                    P.dma(c32[:, 0:T], cst[kc, :, :], eng="sp")
                cb = self.bpool.get()
                P.copy(cb[:, 0:T], c32[:, 0:T], eng="act")
                C32.append(c32); CB.append(cb)
            if DBG == 5:
                return
            PT = []
            for sbi in range(4):
                bs = self.banks.get()
                n = T - sbi * 128
                for kc in range(4):
                    c = 4 * h + kc
                    P.mm(bs[:, 0:n], KF[c][:, sbi * 128:(sbi + 1) * 128], Q[c][:, sbi * 128:T], start=(kc == 0), stop=(kc == 3))
                pt = self.bpool.get()
                for lb in range(sbi, 4):
                    src = bs[:, (lb - sbi) * 128:(lb - sbi + 1) * 128]
                    dst = pt[:, lb * 128:(lb + 1) * 128]
                    if lb == sbi:
                        P.stt(dst, src, ETM(lb, sbi, h), self.MASKK[:, :], ALU.mult, ALU.mult)
                    else:
                        P.ts(dst, src, ETM(lb, sbi, h), kscale, ALU.mult, ALU.mult)
                self.banks.put(bs)
                PT.append(pt)
            if DBG == 6:
                return
            dbank = self.banks.get()
            for lb in range(4):
                for kc in range(4):
                    c = 4 * h + kc
                    P.mm(dbank[:, lb:lb + 1], Q[c][:, lb * 128:(lb + 1) * 128], NB[:, c:c + 1], start=(kc == 0), stop=(kc == 3))
            for lb in range(4):
                for sbi in range(lb + 1):
                    P.mm(dbank[:, 4 + lb:5 + lb], PT[sbi][:, lb * 128:(lb + 1) * 128], self.ONESB[:, 0:1], start=(sbi == 0), stop=(sbi == lb))
            dn = self.small()
            P.copy(dn[:, 0:8], dbank[:, 0:8])
            self.banks.put(dbank)
            wib = TM.v(TM.t[:, 80 + h:96:4])
            P.tt(dn[:, 8:12], dn[:, 0:4], wib, ALU.mult)
            P.tt(dn[:, 8:12], dn[:, 8:12], dn[:, 4:8], ALU.add)
            P.act(dn[:, 8:12], dn[:, 8:12], AF.Abs)
            P.tt(dn[:, 8:12], dn[:, 8:12], TM.v(TM.t[:, 64 + h:80:4]), ALU.max)
            P.recip(dn[:, 8:12], dn[:, 8:12])
            P.tt(dn[:, 12:16], dn[:, 8:12], wib, ALU.mult)
            self.dump("d_dn", dn[:, 0:16], [128, 16])
            self.dump("d_pt0", PT[0][:, 0:T], [128, T], BF16)
            self.dump("d_kt0", KTM[0][:, 0:T], [128, T], BF16)
            self.dump("d_vt0", VTM[0][:, 0:T], [128, T], BF16)
            HN = []
            for lb in range(4):
                b1 = self.banks.get()
                b2 = self.banks.get()
                for kc in range(4):
                    c = 4 * h + kc
                    P.mm(b1[:, 0:T], Q[c][:, lb * 128:(lb + 1) * 128], CB[kc][:, 0:T], start=(kc == 0), stop=(kc == 3))
                for sbi in range(lb + 1):
                    P.mm(b2[:, 0:T], PT[sbi][:, lb * 128:(lb + 1) * 128], VTM[sbi][:, 0:T], start=(sbi == 0), stop=(sbi == lb))
                hu = self.fpool.get()
                P.act(hu[:, 0:T], b2[:, 0:T], AF.Identity, scale=dn[:, 8 + lb:9 + lb])
                self.banks.put(b2)
                P.stt(hu[:, 0:T], b1[:, 0:T], dn[:, 12 + lb:13 + lb], hu[:, 0:T], ALU.mult, ALU.add)
                self.banks.put(b1)
                st = self.small()
                P.bn_stats(st[:, 0:6], hu[:, 0:T])
                P.bn_aggr(st[:, 6:8], st[:, 0:6])
                P.ts(st[:, 8:9], st[:, 7:8], EPS, None, ALU.add)
                P.act(st[:, 8:9], st[:, 8:9], AF.Sqrt)
                P.recip(st[:, 9:10], st[:, 8:9])
                P.stt(st[:, 10:11], st[:, 6:7], -1.0, st[:, 9:10], ALU.mult, ALU.mult)
                hn = self.bpool.get()
                P.act(hn[:, 0:T], hu[:, 0:T], AF.Identity, bias=st[:, 10:11], scale=st[:, 9:10])
                self.fpool.put(hu)
                HN.append(hn)
                if lb == 0:
                    self.dump("d_hn0", hn[:, 0:T], [128, T], BF16)
                    self.dump("d_st0", st[:, 0:12], [128, 12])
            self.bpool.put(*PT)
            self.bpool.put(*CB)
            if DBG == 7:
                return
            SZ = []
            for j in range(4):
                c = 4 * h + j
                if j % 2 == 0:
                    upslot = self.wload(f"mup{l}", 8 + c // 2, 2048)
                    upw = upslot.t[:, :].rearrange("p (k n) -> p k n", k=DC)
                bank = self.banks.get()
                for kc in range(DC):
                    P.mm(bank[:, 0:T], upslot.v(upw[:, kc, (j % 2) * 128:(j % 2 + 1) * 128]), H[kc][:, 0:T],
                         start=(kc == 0), stop=(kc == DC - 1))
                sz = self.bpool.get()
                P.act(sz[:, 0:T], bank[:, 0:T], AF.Silu)
                self.banks.put(bank)
                SZ.append(sz)
            if DBG == 8:
                return
            for j in range(4):
                c = 4 * h + j
                tbk = self.banks.get()
                tv = tbk.t[:, :].bitcast(BF16)
                for lb in range(4):
                    P.transpose(tbk.v(tv[:, lb * 128:(lb + 1) * 128]), HN[lb][:, j * 128:(j + 1) * 128], self.IDB[:, :])
                skx = self.fpool.get()
                P.act(skx[:, 0:T], XC[c][:, 0:T], AF.Identity, scale=self.pcol(f"mskip{l}", c))
                P.stt(skx[:, 0:T], tbk.v(tv[:, 0:T]), self.pcol(f"mlnw{l}", c), skx[:, 0:T], ALU.mult, ALU.add)
                self.banks.put(tbk)
                P.tt(Q[c][:, 0:T], skx[:, 0:T], SZ[j][:, 0:T], ALU.mult)
                self.fpool.put(skx)
            self.bpool.put(*HN)
            self.bpool.put(*SZ)
            if DBG == 9:
                return
            for kc in range(4):
                ub = self.banks.get()
                for sbi in range(4):
                    P.mm(ub[:, 0:T], KTM[sbi][:, kc * 128:(kc + 1) * 128], VTM[sbi][:, 0:T], start=(sbi == 0), stop=(sbi == 3))
                P.stt(C32[kc][:, 0:T], C32[kc][:, 0:T], WCB(h), ub[:, 0:T], ALU.mult, ALU.add)
                self.banks.put(ub)
                if ti < self.nt - 1:
                    P.dma(cst[kc, :, :], C32[kc][:, 0:T], eng="sp")
            self.fpool.put(*C32)
            nbk = self.banks.get()
            for kc in range(4):
                for sbi in range(4):
                    P.mm(nbk[:, kc:kc + 1], KTM[sbi][:, kc * 128:(kc + 1) * 128], self.ONESB[:, 0:1], start=(sbi == 0), stop=(sbi == 3))
            P.stt(nst[:, 4 * h:4 * h + 4], nst[:, 4 * h:4 * h + 4], WCB(h), nbk[:, 0:4], ALU.mult, ALU.add)
            self.banks.put(nbk)
            self.bpool.put(*KTM)
            self.bpool.put(*VTM)
        self.bpool.put(*H)
        self.bpool.put(*XM)
        self.bpool.put(*XC)
        self.bpool.put(*KF)
        for dc in range(DC):
            slot = self.wload(f"mdn{l}", dc, 2048)
            w = slot.t[:, :].rearrange("p (k n) -> p k n", k=IC)
            bank = self.banks.get()
            for kc in range(IC):
                P.mm(bank[:, 0:T], slot.v(w[:, kc, :]), Q[kc][:, 0:T], start=(kc == 0), stop=(kc == IC - 1))
            P.stt(self.X[:, dc, :], bank[:, 0:T], self.MOD[l][:, 16 + dc:17 + dc], self.X[:, dc, :], ALU.mult, ALU.add)
            self.banks.put(bank)
        self.bpool.put(*Q)

    _sb_i = 0

    def small_big(self):
        if not hasattr(self, "_tm"):
            self._tm = [self.sb(f"TM{i}", [128, 96], F32) for i in range(2)]
        t = self._tm[Builder._sb_i % 2]
        Builder._sb_i += 1
        return t

    def small_bf(self):
        if not hasattr(self, "_nb"):
            self._nb = [self.sb(f"NBF{i}", [128, 16], BF16) for i in range(2)]
            self._nbi = 0
        t = self._nb[self._nbi % 2]
        self._nbi += 1
        return t

    def build(self):
        P = self.P
        self.prologue()
        for ti in range(self.nt):
            P.dma(self.X[:, :, :], self.x_d[:, :, ti * T:(ti + 1) * T], eng="sp")
            for l in self.layers:
                if ti == 0:
                    self.ada(l)
                if l % 2 == 0:
                    self.mlstm(l, ti)
                else:
                    self.conformer(l, ti)
                import os
                if int(os.environ.get("MDBG", "99")) >= 99:
                    self.ffn(l, ti)
            if self.final_norm:
                r = self.sumsq()
                for dc in range(DC):
                    P.tt(self.X[:, dc, :], self.X[:, dc, :], r[:, 0:T], ALU.mult)
                    P.ts(self.X[:, dc, :], self.X[:, dc, :], self.pcol("fing", dc), None, ALU.mult)
                self.fpool.put(r)
            P.dma(self.out_d[:, :, ti * T:(ti + 1) * T], self.X[:, :, :], eng="sp")
        P.emit()
        self.nc.sync.wait_ge(P.sems[self.X.dsem], P.dtotal[self.X.dsem])
        if hasattr(self, "dbg_dsem"):
            self.nc.sync.wait_ge(P.sems[self.dbg_dsem], P.dtotal[self.dbg_dsem])
        return self.nc


def run(inputs, nt=S_FULL // T, layers=tuple(range(DEPTH)), final_norm=True, ncores=8, trace=False):
    inp = {k: np.asarray(v) for k, v in inputs.items()}
    shared, pvoff, pvn = host_prep(inp, layers)
    S = nt * T
    b = Builder(nt, list(layers), {k: v.shape for k, v in shared.items()}, pvoff, pvn, final_norm=final_norm)
    nc = b.build()
    in_maps = []
    for core in range(ncores):
        xb = inp["x"][core, :S, :]
        xt = np.ascontiguousarray(xb.T.reshape(DC, 128, S).transpose(1, 0, 2))
        ct = np.ascontiguousarray(inp["c"][core].reshape(DC, 128).T)
        m = {"x": xt, "c": ct}
        m.update(shared)
        in_maps.append(m)
    res = run_bass_kernel_spmd(nc, in_maps, core_ids=list(range(ncores)), trace=trace)
    outs = []
    for core in range(ncores):
        o = res.results[core]["out"]
        outs.append(np.ascontiguousarray(o.transpose(1, 0, 2).reshape(D, S).T))
    return np.stack(outs, axis=0).astype(np.float32), res, b


def kernel(**inputs):
    out, _, _ = run(inputs)
    return out
```

```python
import collections
import numpy as np
import concourse.bass as bass
import concourse.mybir as mybir
from concourse.bass_utils import run_bass_kernel_spmd

F32 = mybir.dt.float32
BF16 = mybir.dt.bfloat16
AF = mybir.ActivationFunctionType
ALU = mybir.AluOpType
AX = mybir.AxisListType

D = 1024
DC = 8
S_FULL = 4096
T = 512
INNER = 2048
IC = 16
NH = 4
DFF = 2816
FC = 22
CONF_K = 31
EPS = 1e-6
DEPTH = 4
SLOT = 2048
NRING = 8
HB = 8
HF = 32


class Dep:
    __slots__ = ("name", "last_w", "readers")

    def __init__(self, name):
        self.name = name
        self.last_w = None
        self.readers = {}


class View:
    __slots__ = ("ap", "deps", "tile")

    def __init__(self, ap, deps, tile=None):
        self.ap = ap
        self.deps = deps
        self.tile = tile


class Tile:
    def __init__(self, name, tensor, nparts=1):
        self.name = name
        self.t = tensor
        self.nparts = nparts
        self.deps = [Dep(f"{name}.{i}") for i in range(nparts)]
        self.dsem = None

    def __getitem__(self, key):
        ap = self.t[key]
        if self.nparts == 1:
            return View(ap, self.deps, self)
        k = key[1] if isinstance(key, tuple) and len(key) > 1 else slice(None)
        if isinstance(k, int):
            d = [self.deps[k]]
        else:
            d = list(self.deps[k])
        return View(ap, d, self)

    def v(self, ap, parts=None):
        return View(ap, self.deps if parts is None else [self.deps[p] for p in parts], self)


class Op:
    __slots__ = ("eng", "fn", "deps", "signal", "sigval", "dsem", "dval", "eidx", "tag")

    def __init__(self, eng, fn, dsem=None):
        self.eng = eng
        self.fn = fn
        self.deps = []
        self.signal = False
        self.sigval = None
        self.dsem = dsem
        self.dval = None


ENGS = ("pe", "act", "dve", "pool", "sp")


class Prog:
    def __init__(self, nc):
        self.nc = nc
        self.ops = []
        self.eng_objs = {"pe": nc.tensor, "act": nc.scalar, "dve": nc.vector,
                         "pool": nc.gpsimd, "sp": nc.sync}
        self.ecount = {e: 0 for e in ENGS}
        self.dsem_names = []
        self.group_dsems = set()
        self.tag = ""
        self.annotate = False

    def add(self, eng, fn, reads=(), writes=(), dsem=None):
        rd = []
        for v in reads:
            if isinstance(v, View):
                rd.extend(v.deps)
        wr = []
        for v in writes:
            if isinstance(v, View):
                wr.extend(v.deps)
        op = Op(eng, fn, dsem)
        op.tag = self.tag
        op.eidx = self.ecount[eng]
        self.ecount[eng] += 1
        deps = set()
        for d in rd:
            if d.last_w is not None:
                deps.add(d.last_w)
        for d in wr:
            if d.last_w is not None:
                deps.add(d.last_w)
            for r in d.readers.values():
                deps.add(r)
        deps.discard(op)
        op.deps = list(deps)
        rkey = eng if dsem is None else ("d", dsem)
        for d in rd:
            d.readers[rkey] = op
        for d in wr:
            d.last_w = op
            d.readers = {}
        self.ops.append(op)
        return op

    def new_dsem(self, name, group=False):
        self.dsem_names.append(name)
        if group:
            self.group_dsems.add(name)
        return name

    def tile_dsem(self, tile):
        if tile.dsem is None:
            tile.dsem = self.new_dsem(f"t{len(self.dsem_names)}")
        return tile.dsem

    def emit(self):
        nc = self.nc
        for op in self.ops:
            for p in op.deps:
                if p.dsem is None:
                    if p.eng == "pe" and op.eng == "pe":
                        continue
                    p.signal = True
        cnt = {e: 0 for e in ENGS}
        dcnt = {n: 0 for n in self.dsem_names}
        for op in self.ops:
            if op.dsem is not None:
                dcnt[op.dsem] += 16
                op.dval = dcnt[op.dsem]
            elif op.signal:
                cnt[op.eng] += 1
                op.sigval = cnt[op.eng]
        sems = {}
        for e in ENGS:
            sems[e] = nc.alloc_semaphore(f"s_{e}")
        for n in self.dsem_names:
            sems[n] = nc.alloc_semaphore(f"d_{n}")
        known = {e: {} for e in ENGS}
        nw = 0
        for op in self.ops:
            eo = self.eng_objs[op.eng]
            need = {}
            for p in op.deps:
                if p.dsem is not None:
                    key = p.dsem
                    val = dcnt[key] if key in self.group_dsems else p.dval
                else:
                    if p.sigval is None or (p.eng == "pe" and op.eng == "pe"):
                        continue
                    key = p.eng
                    val = p.sigval
                if need.get(key, 0) < val:
                    need[key] = val
            kn = known[op.eng]
            for key, val in need.items():
                if kn.get(key, 0) >= val:
                    continue
                eo.wait_ge(sems[key], val)
                kn[key] = val
                nw += 1
            ins = op.fn()
            if self.annotate:
                ins.annotate(op.tag)
            if op.dsem is not None:
                ins.then_inc(sems[op.dsem], 16)
            elif op.signal:
                ins.then_inc(sems[op.eng], 1)
        self.nwaits = nw
        self.sems = sems
        self.dtotal = dcnt

    def mm(self, out, lhsT, rhs, start=True, stop=True):
        nc = self.nc
        return self.add("pe", lambda: nc.tensor.matmul(out.ap, lhsT.ap, rhs.ap, start=start, stop=stop),
                        reads=[lhsT, rhs], writes=[out])

    def transpose(self, out, in_, ident):
        nc = self.nc
        return self.add("pe", lambda: nc.tensor.transpose(out.ap, in_.ap, ident.ap),
                        reads=[in_, ident], writes=[out])

    def act(self, out, in_, func, bias=None, scale=None):
        nc = self.nc
        kw = {}
        if bias is not None:
            kw["bias"] = bias.ap if isinstance(bias, View) else bias
        if scale is not None:
            kw["scale"] = scale.ap if isinstance(scale, View) else scale
        return self.add("act", lambda: nc.scalar.activation(out.ap, in_.ap, func, **kw),
                        reads=[in_, bias, scale], writes=[out])

    def _veng(self, eng):
        return self.nc.vector if eng == "dve" else self.nc.gpsimd

    def tt(self, out, in0, in1, op, eng="dve"):
        e = self._veng(eng)
        return self.add(eng, lambda: e.tensor_tensor(out.ap, in0.ap, in1.ap, op), reads=[in0, in1], writes=[out])

    def ts(self, out, in0, s1, s2, op0, op1=None, eng="dve"):
        e = self._veng(eng)
        a1 = s1.ap if isinstance(s1, View) else s1
        a2 = s2.ap if isinstance(s2, View) else s2
        if op1 is None:
            return self.add(eng, lambda: e.tensor_single_scalar(out.ap, in0.ap, a1, op0), reads=[in0, s1], writes=[out])
        return self.add(eng, lambda: e.tensor_scalar(out.ap, in0.ap, a1, a2, op0, op1), reads=[in0, s1, s2], writes=[out])

    def stt(self, out, in0, scalar, in1, op0, op1):
        nc = self.nc
        a = scalar.ap if isinstance(scalar, View) else scalar
        return self.add("dve", lambda: nc.vector.scalar_tensor_tensor(out.ap, in0.ap, a, in1.ap, op0, op1),
                        reads=[in0, scalar, in1], writes=[out])

    def copy(self, out, in_, eng="dve"):
        if eng == "act":
            nc = self.nc
            return self.add("act", lambda: nc.scalar.copy(out.ap, in_.ap), reads=[in_], writes=[out])
        e = self._veng(eng)
        return self.add(eng, lambda: e.tensor_copy(out.ap, in_.ap), reads=[in_], writes=[out])

    def memset(self, out, val, eng="dve"):
        e = self._veng(eng)
        return self.add(eng, lambda: e.memset(out.ap, val), writes=[out])

    def scan(self, out, d0, d1, init, op0, op1):
        nc = self.nc
        return self.add("dve", lambda: nc.vector.tensor_tensor_scan(out.ap, d0.ap, d1.ap, init, op0, op1),
                        reads=[d0, d1], writes=[out])

    def reduce(self, out, in_, op):
        nc = self.nc
        return self.add("dve", lambda: nc.vector.tensor_reduce(out.ap, in_.ap, AX.X, op), reads=[in_], writes=[out])

    def recip(self, out, in_):
        nc = self.nc
        return self.add("dve", lambda: nc.vector.reciprocal(out.ap, in_.ap), reads=[in_], writes=[out])

    def bn_stats(self, out, in_):
        nc = self.nc
        return self.add("dve", lambda: nc.vector.bn_stats(out.ap, in_.ap), reads=[in_], writes=[out])

    def bn_aggr(self, out, in_):
        nc = self.nc
        return self.add("dve", lambda: nc.vector.bn_aggr(out.ap, in_.ap), reads=[in_], writes=[out])

    def dma(self, out, in_, eng="sp", dsem=None):
        eo = self.eng_objs[eng]
        oap = out.ap if isinstance(out, View) else out
        iap = in_.ap if isinstance(in_, View) else in_
        if dsem is None:
            sb = out if (isinstance(out, View) and out.tile is not None and not getattr(out.tile, "is_dram", False)) else in_
            dsem = self.tile_dsem(sb.tile)
        return self.add(eng, lambda: eo.dma_start(out=oap, in_=iap), reads=[in_], writes=[out], dsem=dsem)


class Pool:
    def __init__(self, tiles):
        self.free = collections.deque(tiles)
        self.n = len(tiles)
        self.low = len(tiles)

    def get(self):
        assert self.free, "pool exhausted"
        t = self.free.popleft()
        self.low = min(self.low, len(self.free))
        return t

    def put(self, *ts):
        for t in ts:
            self.free.append(t)


def kmajor_slots(W, ncols, col_idx=None):
    K, N = W.shape
    KC = K // 128
    if col_idx is not None:
        W = W[:, col_idx]
        N = W.shape[1]
    NS = N // ncols
    A = W.reshape(KC, 128, NS, ncols).transpose(2, 1, 0, 3)
    return np.ascontiguousarray(A).reshape(NS, 128, KC * ncols)


def fm_cols(v):
    v = np.asarray(v, np.float32)
    lead = v.shape[:-1]
    C = v.shape[-1] // 128
    A = v.reshape(*lead, C, 128)
    A = np.moveaxis(A, -1, 0)
    return np.ascontiguousarray(A).reshape(128, -1)


class PV:
    def __init__(self):
        self.cols = []
        self.off = {}
        self.n = 0

    def add(self, name, arr):
        arr = np.asarray(arr, np.float32)
        assert arr.shape[0] == 128
        self.off[name] = self.n
        self.cols.append(arr)
        self.n += arr.shape[1]

    def build(self):
        return np.ascontiguousarray(np.concatenate(self.cols, axis=1))


def blockdiag_chunks(w):
    out = np.zeros((16, 128, 128), np.float32)
    wr = w.reshape(16, 32, 4, 4)
    for b in range(32):
        out[:, 4 * b:4 * b + 4, 4 * b:4 * b + 4] = wr[:, b]
    return out


def host_prep(inp, layers):
    pv = PV()
    shared = {}
    ffn_cols = np.concatenate([np.concatenate([np.arange(j * 128, (j + 1) * 128),
                                               DFF + np.arange(j * 128, (j + 1) * 128)]) for j in range(FC)])
    conf_cols = np.concatenate([np.concatenate([np.arange(j * 128, (j + 1) * 128),
                                                D + np.arange(j * 128, (j + 1) * 128)]) for j in range(DC)])
    for l in layers:
        pv.add(f"adab{l}", fm_cols(inp["ada_b"][l]))
        pv.add(f"gmix{l}", fm_cols(inp["norm_mix_g"][l]))
        pv.add(f"gffn{l}", fm_cols(inp["norm_ffn_g"][l]))
        shared[f"ada{l}"] = kmajor_slots(inp["ada_w"][l], 256)
        shared[f"fup{l}"] = kmajor_slots(inp["ffn_w_up"][l], 256, ffn_cols)
        fd = inp["ffn_w_down"][l]
        A = fd.reshape(2, 11, 128, DC, 128).transpose(3, 0, 2, 1, 4)
        shared[f"fdn{l}"] = np.ascontiguousarray(A).reshape(16, 128, 11 * 128)
        cw = inp["ffn_conv_w"][l]
        cwg = fm_cols(cw[:, :DFF])
        cwv = fm_cols(cw[:, DFF:])
        pv.add(f"fcwg{l}", cwg)
        pv.add(f"fcwv{l}", cwv)
        pv.add(f"fcbg{l}", fm_cols(inp["ffn_conv_b"][l][:DFF]))
        pv.add(f"fcbv{l}", fm_cols(inp["ffn_conv_b"][l][DFF:]))
        j = l // 2
        if l % 2 == 0:
            shared[f"mup{l}"] = kmajor_slots(inp["mlstm_w_up"][j], 256)
            shared[f"mdn{l}"] = kmajor_slots(inp["mlstm_w_down"][j], 128)
            bq = blockdiag_chunks(inp["mlstm_wq"][j])
            bk = blockdiag_chunks(inp["mlstm_wk"][j])
            bv = blockdiag_chunks(inp["mlstm_wv"][j])
            B = np.stack([bq, bk, bv], axis=1)
            B = B.reshape(4, 4, 3, 128, 128).transpose(0, 3, 1, 2, 4)
            shared[f"wbd{l}"] = np.ascontiguousarray(B).reshape(4, 128, 4 * 3 * 128)
            wif = inp["mlstm_w_if"][j]
            W = np.zeros((48, 128, 36), np.float32)
            W[:, :, 0:4] = wif[:, 0:4].reshape(48, 128, 4)
            W[:, :, 32:36] = wif[:, 4:8].reshape(48, 128, 4)
            shared[f"wif{l}"] = np.ascontiguousarray(W.transpose(1, 0, 2)).reshape(1, 128, 48 * 36)
            pv.add(f"mcw{l}", fm_cols(inp["mlstm_conv_w"][j]))
            pv.add(f"mcb{l}", fm_cols(inp["mlstm_conv_b"][j]))
            pv.add(f"mlnw{l}", fm_cols(inp["mlstm_ln_w"][j]))
            pv.add(f"mskip{l}", fm_cols(inp["mlstm_skip"][j]))
            bif = np.zeros((128, 2), np.float32)
            bif[0:4, 0] = inp["mlstm_b_if"][j][0:4]
            bif[0:4, 1] = inp["mlstm_b_if"][j][4:8]
            pv.add(f"mbif{l}", bif)
        else:
            shared[f"cp1{l}"] = kmajor_slots(inp["conf_w_pw1"][j], 256, conf_cols)
            shared[f"cp2{l}"] = kmajor_slots(inp["conf_w_pw2"][j], 256)
            pv.add(f"cb1a{l}", fm_cols(inp["conf_b_pw1"][j][:D]))
            pv.add(f"cb1g{l}", fm_cols(inp["conf_b_pw1"][j][D:]))
            pv.add(f"cdw{l}", fm_cols(inp["conf_dw_w"][j]))
            pv.add(f"cdb{l}", fm_cols(inp["conf_dw_b"][j]))
            pv.add(f"clg{l}", fm_cols(inp["conf_ln_g"][j]))
            pv.add(f"clb{l}", fm_cols(inp["conf_ln_b"][j]))
            pv.add(f"cb2{l}", fm_cols(inp["conf_b_pw2"][j]))
    pv.add("fing", fm_cols(inp["final_g"]))
    ident = np.eye(128, dtype=np.float32)
    mask = np.triu(np.ones((128, 128), np.float32))
    self_ = np.zeros((128, 4), np.float32)
    for jj in range(4):
        self_[32 + jj, jj] = 1.0
    shared["consts"] = np.ascontiguousarray(np.concatenate([ident, mask, self_], axis=1))
    shared["pvec"] = pv.build()
    return shared, pv.off, pv.n


class Builder:
    def __init__(self, nt, layers, shared_shapes, pvoff, pvn, final_norm=True):
        self.nt = nt
        self.layers = layers
        self.final_norm = final_norm
        self.pvoff = pvoff
        nc = bass.Bass("TRN2", target_bir_lowering=False)
        self.nc = nc
        self.P = Prog(nc)
        P = self.P
        S = nt * T
        self.x_d = nc.dram_tensor("x", [128, DC, S], F32, kind="ExternalInput").ap()
        self.c_d = nc.dram_tensor("c", [128, DC], F32, kind="ExternalInput").ap()
        self.out_d = nc.dram_tensor("out", [128, DC, S], F32, kind="ExternalOutput").ap()
        self.w_d = {}
        for name, shp in shared_shapes.items():
            self.w_d[name] = nc.dram_tensor(name, list(shp), F32, kind="ExternalInput").ap()

        def sb(name, shape, dt=F32, nparts=1):
            return Tile(name, nc.alloc_sbuf_tensor(name, shape, dt), nparts)

        self.sb = sb
        self.X = sb("X", [128, DC, T], F32, nparts=DC)
        self.PVEC = sb("PVEC", [128, pvn], F32)
        self.CONST = sb("CONST", [128, 260], F32)
        self.PVECB = sb("PVECB", [128, pvn], BF16)
        self.IDB = sb("IDB", [128, 128], BF16)
        self.ONESB = sb("ONESB", [128, 128], BF16)
        self.ONESF = sb("ONESF", [128, 512], F32)
        self.CACT = sb("CACT", [128, DC], BF16)
        self.CIN = sb("CIN", [128, DC], F32)
        self.MOD = {l: sb(f"MOD{l}", [128, 48], F32) for l in layers}
        self.AB = {l: sb(f"AB{l}", [128, 16], F32) for l in layers}
        self.HALO_F = {l: sb(f"HALOF{l}", [128, 2 * FC, 2], BF16) for l in layers}
        self.HALO_M = {l: sb(f"HALOM{l}", [128, IC, 4], BF16) for l in layers if l % 2 == 0}
        self.HALO_C = {l: sb(f"HALOC{l}", [128, DC, 30], BF16) for l in layers if l % 2 == 1}
        self.NST = {l: sb(f"NST{l}", [128, IC], F32) for l in layers if l % 2 == 0}
        self.MST = {l: sb(f"MST{l}", [4, 1], F32) for l in layers if l % 2 == 0}
        self.WIF = sb("WIF", [128, 48 * 36], BF16)
        self.MASKK = sb("MASKK", [128, 128], F32)
        self.CST = {}
        for l in layers:
            if l % 2 == 0:
                for h in range(NH):
                    t = Tile(f"CST{l}_{h}", nc.dram_tensor(f"cst{l}_{h}", [4, 128, T], F32, kind="Internal"))
                    t.is_dram = True
                    self.CST[(l, h)] = t
        self.RING = [sb(f"RING{i}", [128, SLOT], BF16) for i in range(NRING)]
        self.ring_i = 0
        NB, NF = 100, 14
        self.bpool = Pool([sb(f"B{i}", [128, HB + T], BF16) for i in range(NB)])
        self.fpool = Pool([sb(f"F{i}", [128, HF + T], F32) for i in range(NF)])
        self.banks = Pool([Tile(f"PS{i}", nc.alloc_psum_tensor(f"PS{i}", [128, 512], F32)) for i in range(8)])
        self.small_i = 0
        self.smalls = [sb(f"SMALL{i}", [128, 16], F32) for i in range(24)]

    def dump(self, name, view, shape, dt=F32):
        import os
        if not os.environ.get("MDUMP"):
            return
        if not hasattr(self, "dbg_dsem"):
            self.dbg_dsem = self.P.new_dsem("dbg", group=True)
            self.dumps = []
        if name in self.dumps:
            return
        self.dumps.append(name)
        d = self.nc.dram_tensor(name, list(shape), dt, kind="ExternalOutput").ap()
        self.P.dma(d, view, eng="sp", dsem=self.dbg_dsem)

    def small(self):
        t = self.smalls[self.small_i % len(self.smalls)]
        self.small_i += 1
        return t

    def pcol(self, name, c0, n=1):
        o = self.pvoff[name] + c0
        return self.PVEC[:, o:o + n]

    def wload(self, name, s, nelem):
        slot = self.RING[self.ring_i % NRING]
        self.ring_i += 1
        self.P.dma(slot[:, 0:nelem], self.w_d[name][s, :, :], eng="pool")
        return slot

    def prologue(self):
        P = self.P
        P.dma(self.PVEC[:, :], self.w_d["pvec"], eng="sp")
        P.dma(self.CONST[:, :], self.w_d["consts"], eng="sp")
        P.dma(self.CIN[:, :], self.c_d, eng="sp")
        P.copy(self.IDB[:, :], self.CONST[:, 0:128])
        P.copy(self.PVECB[:, :], self.PVEC[:, :])
        P.ts(self.MASKK[:, :], self.CONST[:, 128:256], float(512 ** -0.5), None, ALU.mult)
        P.memset(self.ONESB[:, :], 1.0)
        P.memset(self.ONESF[:, :], 1.0, eng="pool")
        P.act(self.CACT[:, :], self.CIN[:, :], AF.Silu)
        for l in self.layers:
            P.memset(self.HALO_F[l][:, :, :], 0.0, eng="pool")
            if l % 2 == 0:
                P.memset(self.HALO_M[l][:, :, :], 0.0, eng="pool")
                P.memset(self.NST[l][:, :], 0.0, eng="pool")
                P.memset(self.MST[l][:, :], 0.0, eng="pool")
            else:
                P.memset(self.HALO_C[l][:, :, :], 0.0, eng="pool")

    def ada(self, l):
        P = self.P
        bank = self.banks.get()
        for s in range(24):
            slot = self.wload(f"ada{l}", s, 2048)
            w = slot.t[:, :].rearrange("p (k n) -> p k n", k=DC)
            for jj in range(2):
                oc = 2 * s + jj
                for kc in range(DC):
                    P.mm(bank[:, oc:oc + 1], slot.v(w[:, kc, jj * 128:(jj + 1) * 128]), self.CACT[:, kc:kc + 1],
                         start=(kc == 0), stop=(kc == DC - 1))
        M = self.MOD[l]
        P.tt(M[:, :], bank[:, 0:48], self.pcol(f"adab{l}", 0, 48), ALU.add)
        self.banks.put(bank)
        AB = self.AB[l]
        P.stt(AB[:, 0:8], M[:, 8:16], 1.0, self.pcol(f"gmix{l}", 0, 8), ALU.add, ALU.mult)
        P.stt(AB[:, 8:16], M[:, 32:40], 1.0, self.pcol(f"gffn{l}", 0, 8), ALU.add, ALU.mult)

    def rstd_from_bank(self, bank):
        P = self.P
        r = self.fpool.get()
        P.ts(r[:, 0:T], bank[:, 0:T], 1.0 / D, EPS, ALU.mult, ALU.add)
        P.act(r[:, 0:T], r[:, 0:T], AF.Sqrt)
        P.recip(r[:, 0:T], r[:, 0:T])
        return r

    def sumsq(self):
        P = self.P
        bank = self.banks.get()
        for dc in range(DC):
            sq = self.bpool.get()
            P.act(sq[:, 0:T], self.X[:, dc, :], AF.Square)
            P.mm(bank[:, 0:T], self.ONESB[:, :], sq[:, 0:T], start=(dc == 0), stop=(dc == DC - 1))
            self.bpool.put(sq)
        r = self.rstd_from_bank(bank)
        self.banks.put(bank)
        return r

    def norm_mod(self, l, which):
        P = self.P
        P.tag = "norm"
        r = self.sumsq()
        a0 = 0 if which == 0 else 8
        sh0 = 0 if which == 0 else 24
        H = []
        for dc in range(DC):
            tmp = self.fpool.get()
            P.tt(tmp[:, 0:T], self.X[:, dc, :], r[:, 0:T], ALU.mult)
            h = self.bpool.get()
            P.act(h[:, 0:T], tmp[:, 0:T], AF.Identity, bias=self.MOD[l][:, sh0 + dc:sh0 + dc + 1],
                  scale=self.AB[l][:, a0 + dc:a0 + dc + 1])
            self.fpool.put(tmp)
            H.append(h)
        self.fpool.put(r)
        return H


    def conv_build(self, wname, c0, cstep, n):
        P = self.P
        nc = self.nc
        idb = self.IDB.t[:, :]
        slabs = []
        for k0 in range(0, n, 4):
            K = min(4, n - k0)
            dslab = self.bpool.get()
            slabs.append(dslab)
            out3 = dslab.t[:, 0:K * 128].rearrange("p (k m) -> p k m", k=K)
            in0 = bass.AP(idb.tensor, idb.offset, [list(idb.ap[0]), [0, K], [1, 128]])
            col = self.pvoff[wname] + c0 + k0 * cstep
            wv = self.PVECB.t[:, col:col + 1]
            in1 = bass.AP(wv.tensor, wv.offset, [list(wv.ap[0]), [cstep, K], [0, 128]])
            P.add("dve", (lambda o=out3, a=in0, b_=in1: nc.vector.tensor_tensor(o, a, b_, ALU.mult)),
                  reads=[self.IDB[:, :], self.PVECB[:, :]], writes=[dslab[:, :]])
        return slabs

    def conv_mm(self, bank, slabs, rhs_list):
        P = self.P
        n = len(rhs_list)
        for k, rhs in enumerate(rhs_list):
            dslab = slabs[k // 4]
            dg = dslab[:, (k % 4) * 128:(k % 4 + 1) * 128]
            P.mm(bank[:, 0:T], dg, rhs, start=(k == 0), stop=(k == n - 1))
        self.bpool.put(*slabs)

    def ffn(self, l, ti):
        P = self.P
        H = self.norm_mod(l, 1)
        P.tag = "ffn.up"
        HID = []
        halo = self.HALO_F[l]
        def stage1(j):
            dgs = [self.conv_build(f"{nm}{l}", j, FC, 3) for nm in ("fcwg", "fcwv")]
            slot = self.wload(f"fup{l}", j, 2048)
            w = slot.t[:, :].rearrange("p (k n) -> p k n", k=DC)
            us = []
            for gv in range(2):
                bank = self.banks.get()
                for kc in range(DC):
                    P.mm(bank[:, 0:T], slot.v(w[:, kc, gv * 128:(gv + 1) * 128]), H[kc][:, 0:T],
                         start=(kc == 0), stop=(kc == DC - 1))
                u = self.bpool.get()
                hj = 2 * j + gv
                P.copy(u[:, 0:2], halo[:, hj, :], eng="act")
                P.copy(u[:, 2:2 + T], bank[:, 0:T], eng="act")
                self.banks.put(bank)
                P.copy(halo[:, hj, :], u[:, T:T + 2])
                us.append(u)
            return us, dgs

        def stage2(j, us, dgs):
            cb = []
            for gv in range(2):
                nm = "fcwg" if gv == 0 else "fcwv"
                u = us[gv]
                bank = self.banks.get()
                self.conv_mm(bank, dgs[gv], [u[:, k:k + T] for k in range(3)])
                cb.append(bank)
            self.bpool.put(*us)
            sg = self.fpool.get()
            P.act(sg[:, 0:T], cb[0][:, 0:T], AF.Silu, bias=self.pcol(f"fcbg{l}", j))
            self.banks.put(cb[0])
            hid = self.bpool.get()
            P.stt(hid[:, 0:T], cb[1][:, 0:T], self.pcol(f"fcbv{l}", j), sg[:, 0:T], ALU.add, ALU.mult)
            self.banks.put(cb[1])
            self.fpool.put(sg)
            HID.append(hid)

        prev = None
        for j in range(FC):
            us, dgs = stage1(j)
            if prev is not None:
                stage2(*prev)
            prev = (j, us, dgs)
        stage2(*prev)
        self.bpool.put(*H)
        P.tag = "ffn.down"
        for dc in range(DC):
            bank = self.banks.get()
            for half in range(2):
                slot = self.wload(f"fdn{l}", dc * 2 + half, 11 * 128)
                w = slot.t[:, 0:11 * 128].rearrange("p (k n) -> p k n", k=11)
                for k in range(11):
                    kc = half * 11 + k
                    P.mm(bank[:, 0:T], slot.v(w[:, k, :]), HID[kc][:, 0:T], start=(kc == 0), stop=(kc == FC - 1))
            P.stt(self.X[:, dc, :], bank[:, 0:T], self.MOD[l][:, 40 + dc:41 + dc], self.X[:, dc, :], ALU.mult, ALU.add)
            self.banks.put(bank)
        self.bpool.put(*HID)

    def conformer(self, l, ti):
        P = self.P
        H = self.norm_mod(l, 0)
        P.tag = "conf.pw1conv"
        halo = self.HALO_C[l]
        ACC = []
        mbank = self.banks.get()
        sbank = self.banks.get()
        for j in range(DC):
            cdg = self.conv_build(f"cdw{l}", j, DC, CONF_K)
            slot = self.wload(f"cp1{l}", j, 2048)
            w = slot.t[:, :].rearrange("p (k n) -> p k n", k=DC)
            ba = self.banks.get()
            bg = self.banks.get()
            for kc in range(DC):
                P.mm(ba[:, 0:T], slot.v(w[:, kc, 0:128]), H[kc][:, 0:T], start=(kc == 0), stop=(kc == DC - 1))
            for kc in range(DC):
                P.mm(bg[:, 0:T], slot.v(w[:, kc, 128:256]), H[kc][:, 0:T], start=(kc == 0), stop=(kc == DC - 1))
            sig = self.fpool.get()
            P.act(sig[:, 0:T], bg[:, 0:T], AF.Sigmoid, bias=self.pcol(f"cb1g{l}", j))
            self.banks.put(bg)
            uf = self.fpool.get()
            ub16 = uf.t[:, :].bitcast(BF16)
            P.copy(uf.v(ub16[:, 2:32]), halo[:, j, :], eng="act")
            P.stt(uf.v(ub16[:, 32:32 + T]), ba[:, 0:T], self.pcol(f"cb1a{l}", j), sig[:, 0:T], ALU.add, ALU.mult)
            self.banks.put(ba)
            self.fpool.put(sig)
            P.copy(halo[:, j, :], uf.v(ub16[:, 2 + T:32 + T]), eng="act")
            cbank = self.banks.get()
            self.conv_mm(cbank, cdg, [uf.v(ub16[:, 2 + k:2 + k + T]) for k in range(CONF_K)])
            self.fpool.put(uf)
            acc = self.fpool.get()
            P.act(acc[:, 0:T], cbank[:, 0:T], AF.Identity, bias=self.pcol(f"cdb{l}", j))
            self.banks.put(cbank)
            ub = self.bpool.get()
            us = self.bpool.get()
            P.copy(ub[:, 0:T], acc[:, 0:T], eng="act")
            P.act(us[:, 0:T], acc[:, 0:T], AF.Square)
            P.mm(mbank[:, 0:T], self.ONESB[:, :], ub[:, 0:T], start=(j == 0), stop=(j == DC - 1))
            P.mm(sbank[:, 0:T], self.ONESB[:, :], us[:, 0:T], start=(j == 0), stop=(j == DC - 1))
            self.bpool.put(ub, us)
            ACC.append(acc)
        self.bpool.put(*H)
        P.tag = "conf.ln"
        mean = self.fpool.get()
        rstd = self.fpool.get()
        P.ts(mean[:, 0:T], mbank[:, 0:T], 1.0 / D, None, ALU.mult)
        P.tt(rstd[:, 0:T], mean[:, 0:T], mean[:, 0:T], ALU.mult)
        P.stt(rstd[:, 0:T], sbank[:, 0:T], 1.0 / D, rstd[:, 0:T], ALU.mult, ALU.subtract)
        P.ts(rstd[:, 0:T], rstd[:, 0:T], EPS, None, ALU.add)
        P.act(rstd[:, 0:T], rstd[:, 0:T], AF.Sqrt)
        P.recip(rstd[:, 0:T], rstd[:, 0:T])
        self.banks.put(mbank, sbank)
        Y = []
        for j in range(DC):
            acc = ACC[j]
            P.tt(acc[:, 0:T], acc[:, 0:T], mean[:, 0:T], ALU.subtract)
            P.tt(acc[:, 0:T], acc[:, 0:T], rstd[:, 0:T], ALU.mult)
            y = self.bpool.get()
            P.act(y[:, 0:T], acc[:, 0:T], AF.Silu, bias=self.pcol(f"clb{l}", j), scale=self.pcol(f"clg{l}", j))
            self.fpool.put(acc)
            Y.append(y)
        self.fpool.put(mean, rstd)
        P.tag = "conf.pw2"
        for s in range(4):
            slot = self.wload(f"cp2{l}", s, 2048)
            w = slot.t[:, :].rearrange("p (k n) -> p k n", k=DC)
            for jj in range(2):
                dc = 2 * s + jj
                bank = self.banks.get()
                for kc in range(DC):
                    P.mm(bank[:, 0:T], slot.v(w[:, kc, jj * 128:(jj + 1) * 128]), Y[kc][:, 0:T],
                         start=(kc == 0), stop=(kc == DC - 1))
                tmp = self.fpool.get()
                P.ts(tmp[:, 0:T], bank[:, 0:T], self.pcol(f"cb2{l}", dc), self.MOD[l][:, 16 + dc:17 + dc], ALU.add, ALU.mult)
                self.banks.put(bank)
                P.tt(self.X[:, dc, :], self.X[:, dc, :], tmp[:, 0:T], ALU.add)
                self.fpool.put(tmp)
        self.bpool.put(*Y)

    def mlstm(self, l, ti):
        P = self.P
        nc = self.nc
        H = self.norm_mod(l, 0)
        P.tag = "ml.A"
        halo = self.HALO_M[l]
        gbank = self.banks.get()
        wif = self.WIF
        self.P.dma(wif[:, 0:48 * 36], self.w_d[f"wif{l}"][0, :, :], eng="pool")
        wifv = wif.t[:, 0:48 * 36].rearrange("p (c m) -> p c m", c=48)
        kscale = float(512 ** -0.5)
        wbd = None
        XM = [None] * IC
        XC = [None] * IC
        Q = [None] * IC
        KF = [None] * IC
        st = {}

        DG = [None] * IC

        def stageA(c):
            DG[c] = self.conv_build(f"mcw{l}", c, IC, 4)
            if c % 2 == 0:
                st["upslot"] = self.wload(f"mup{l}", c // 2, 2048)
            upslot = st["upslot"]
            upw = upslot.t[:, :].rearrange("p (k n) -> p k n", k=DC)
            jj = c % 2
            bank = self.banks.get()
            for kc in range(DC):
                P.mm(bank[:, 0:T], upslot.v(upw[:, kc, jj * 128:(jj + 1) * 128]), H[kc][:, 0:T],
                     start=(kc == 0), stop=(kc == DC - 1))
            xm = self.bpool.get()
            P.copy(xm[:, HB - 4:HB], halo[:, c, :], eng="act")
            P.copy(xm[:, HB:HB + T], bank[:, 0:T], eng="act")
            self.banks.put(bank)
            P.copy(halo[:, c, :], xm[:, HB + T - 4:HB + T])
            XM[c] = xm

        def stageB(c):
            xm = XM[c]
            cbank = self.banks.get()
            self.conv_mm(cbank, DG[c], [xm[:, HB - 3 + k:HB - 3 + k + T] for k in range(4)])
            xc = self.bpool.get()
            P.act(xc[:, 0:T], cbank[:, 0:T], AF.Silu, bias=self.pcol(f"mcb{l}", c))
            self.banks.put(cbank)
            XC[c] = xc

        def stageC(c):
            xm, xc = XM[c], XC[c]
            if c % 4 == 0:
                st["wbd"] = self.wload(f"wbd{l}", c // 4, 1536)
            wbd = st["wbd"]
            wbdv = wbd.t[:, 0:1536].rearrange("p (c q m) -> p c q m", c=4, q=3)
            cc = c % 4
            bq = self.banks.get()
            P.mm(bq[:, 0:T], wbd.v(wbdv[:, cc, 0, :]), xc[:, 0:T])
            bk = self.banks.get()
            P.mm(bk[:, 0:T], wbd.v(wbdv[:, cc, 1, :]), xc[:, 0:T])
            bv = self.banks.get()
            P.mm(bv[:, 0:T], wbd.v(wbdv[:, cc, 2, :]), xm[:, HB:HB + T])
            q = self.bpool.get()
            P.copy(q[:, 0:T], bq[:, 0:T], eng="act")
            self.banks.put(bq)
            kf = self.bpool.get()
            P.copy(kf[:, 0:T], bk[:, 0:T])
            self.banks.put(bk)
            vf = self.bpool.get()
            P.copy(vf[:, 0:T], bv[:, 0:T], eng="act")
            self.banks.put(bv)
            P.mm(gbank[0:36, 0:T], wif.v(wifv[:, c, :]), q[:, 0:T], start=(c == 0), stop=False)
            P.mm(gbank[0:36, 0:T], wif.v(wifv[:, 16 + c, :]), kf[:, 0:T], start=False, stop=False)
            P.mm(gbank[0:36, 0:T], wif.v(wifv[:, 32 + c, :]), vf[:, 0:T], start=False, stop=(c == IC - 1))
            self.bpool.put(vf)
            Q[c] = q
            KF[c] = kf
            if c == 0:
                self.dump("d_xm0", xm[:, HB:HB + T], [128, T], BF16)
                self.dump("d_xc0", xc[:, 0:T], [128, T], BF16)
                self.dump("d_q0", q[:, 0:T], [128, T], BF16)
                self.dump("d_k0", kf[:, 0:T], [128, T], BF16)

        for c in range(IC + 2):
            if c < IC:
                stageA(c)
            if 0 <= c - 1 < IC:
                stageB(c - 1)
            if 0 <= c - 2 < IC:
                stageC(c - 2)
        import os
        DBG = int(os.environ.get("MDBG", "99"))
        if DBG == 1:
            return
        P.tag = "ml.gate"
        GS = self.fpool.get()
        P.copy(GS[0:36, 0:T], gbank[0:36, 0:T], eng="act")
        self.dump("d_gs", GS[0:36, 0:T], [36, T])
        P.mm(gbank[0:4, 0:T], self.CONST[0:36, 256:260], GS[0:36, 0:T])
        ipre = self.fpool.get()
        ob = self.pvoff[f"mbif{l}"]
        P.ts(ipre[0:4, 0:T], GS[0:4, 0:T], self.PVEC[0:4, ob:ob + 1], None, ALU.add)
        lf = self.fpool.get()
        P.act(lf[0:4, 0:T], gbank[0:4, 0:T], AF.Sigmoid, bias=self.PVEC[0:4, ob + 1:ob + 2])
        self.banks.put(gbank)
        self.fpool.put(GS)
        P.act(lf[0:4, 0:T], lf[0:4, 0:T], AF.Ln)
        g = self.fpool.get()
        P.scan(g[0:4, 0:T], self.ONESF[0:4, 0:T], lf[0:4, 0:T], 0.0, ALU.mult, ALU.add)
        self.fpool.put(lf)
        a = ipre
        P.tt(a[0:4, 0:T], ipre[0:4, 0:T], g[0:4, 0:T], ALU.subtract)
        sm = self.small()
        mst = self.MST[l]
        P.reduce(sm[0:4, 0:4], a.v(a.t[0:4, 0:T].rearrange("p (b t) -> p b t", b=4)), ALU.max)
        P.tt(sm[0:4, 4:5], sm[0:4, 0:1], mst[0:4, 0:1], ALU.max)
        for lb in range(1, 4):
            P.tt(sm[0:4, 4 + lb:5 + lb], sm[0:4, lb:lb + 1], sm[0:4, 3 + lb:4 + lb], ALU.max)
        P.ts(sm[0:4, 8:12], sm[0:4, 4:8], -1.0, None, ALU.mult)
        P.act(sm[0:4, 12:16], sm[0:4, 8:12], AF.Exp, bias=mst[0:4, 0:1])
        E = []
        fl = self.fpool.get()
        for lb in range(4):
            e = self.fpool.get()
            n = 128 * (lb + 1)
            P.act(e[0:4, 0:n], a[0:4, 0:n], AF.Exp, bias=sm[0:4, 8 + lb:9 + lb])
            P.act(fl[0:4, lb * 128:n], g[0:4, lb * 128:n], AF.Exp, bias=sm[0:4, 8 + lb:9 + lb], scale=-1.0)
            E.append(e)
        self.dump("d_e", E[3][0:4, 0:T], [4, T])
        self.dump("d_fl", fl[0:4, 0:T], [4, T])
        self.dump("d_g", g[0:4, 0:T], [4, T])
        self.dump("d_sm", sm[0:4, 0:16], [4, 16])
        P.tt(mst[0:4, 0:1], g[0:4, T - 1:T], sm[0:4, 7:8], ALU.add)
        self.fpool.put(g, a)
        if DBG == 2:
            return
        tb = self.banks.get()
        for lb in range(4):
            for sbi in range(lb + 1):
                c0 = (lb * 4 + sbi) * 4
                P.transpose(tb[:, c0:c0 + 4], E[lb][0:4, sbi * 128:(sbi + 1) * 128], self.CONST[0:4, 0:4])
            P.transpose(tb[:, 64 + lb * 4:68 + lb * 4], fl[0:4, lb * 128:(lb + 1) * 128], self.CONST[0:4, 0:4])
        dg = self.small()
        for lb in range(4):
            P.ts(dg[0:4, lb * 4:lb * 4 + 4], self.CONST[0:4, 0:4], sm[0:4, 12 + lb:13 + lb], None, ALU.mult)
        P.mm(tb[:, 80:96], self.ONESF[0:4, 0:128], dg[0:4, 0:16])
        TM = self.small_big()
        P.memset(TM[:, 0:64], 0.0)
        for lb in range(4):
            c0 = lb * 16
            P.copy(TM[:, c0:c0 + 4 * (lb + 1)], tb[:, c0:c0 + 4 * (lb + 1)])
        P.copy(TM[:, 64:96], tb[:, 64:96])
        self.banks.put(tb)
        self.fpool.put(fl, *E)
        self.dump("d_tm", TM[:, 0:96], [128, 96])
        ETM = lambda lb, sbi, h: TM[:, (lb * 4 + sbi) * 4 + h:(lb * 4 + sbi) * 4 + h + 1]
        WCB = lambda h: TM[:, 80 + 12 + h:80 + 13 + h]
        nst = self.NST[l]
        NB = self.small_bf()
        P.copy(NB[:, 0:16], nst[:, 0:16])
        if DBG == 3:
            return
        for h in range(NH):
            P.tag = "ml.B.ktm"
            wbd = self.wload(f"wbd{l}", h, 1536)
            wbdv = wbd.t[:, 0:1536].rearrange("p (c q m) -> p c q m", c=4, q=3)
            KTM, VTM = [], []
            for sbi in range(4):
                bk = self.banks.get()
                bv = self.banks.get()
                for j in range(4):
                    c = 4 * h + j
                    P.mm(bk[:, j * 128:(j + 1) * 128], XC[c][:, sbi * 128:(sbi + 1) * 128], wbd.v(wbdv[:, j, 1, :]))
                    P.mm(bv[:, j * 128:(j + 1) * 128], XM[c][:, HB + sbi * 128:HB + (sbi + 1) * 128], wbd.v(wbdv[:, j, 2, :]))
                kt = self.bpool.get()
                vt = self.bpool.get()
                P.ts(kt[:, 0:T], bk[:, 0:T], ETM(3, sbi, h), kscale, ALU.mult, ALU.mult)
                P.copy(vt[:, 0:T], bv[:, 0:T], eng="act")
                self.banks.put(bk, bv)
                KTM.append(kt); VTM.append(vt)
            if DBG == 4:
                return
            P.tag = "ml.B.cload"
            C32, CB = [], []
            cst = self.CST[(l, h)]
            for kc in range(4):
                c32 = self.fpool.get()
                if ti == 0:
                    P.memset(c32[:, 0:T], 0.0)
                else:
                    P.dma(c32[:, 0:T], cst[kc, :, :], eng="sp")
                cb = self.bpool.get()
                P.copy(cb[:, 0:T], c32[:, 0:T], eng="act")
                C32.append(c32); CB.append(cb)
            if DBG == 5:
                return
            P.tag = "ml.B.st"
            PT = []
            for sbi in range(4):
                bs = self.banks.get()
                n = T - sbi * 128
                for kc in range(4):
                    c = 4 * h + kc
                    P.mm(bs[:, 0:n], KF[c][:, sbi * 128:(sbi + 1) * 128], Q[c][:, sbi * 128:T], start=(kc == 0), stop=(kc == 3))
                pt = self.bpool.get()
                for lb in range(sbi, 4):
                    src = bs[:, (lb - sbi) * 128:(lb - sbi + 1) * 128]
                    dst = pt[:, lb * 128:(lb + 1) * 128]
                    if lb == sbi:
                        P.stt(dst, src, ETM(lb, sbi, h), self.MASKK[:, :], ALU.mult, ALU.mult)
                    else:
                        P.ts(dst, src, ETM(lb, sbi, h), kscale, ALU.mult, ALU.mult)
                self.banks.put(bs)
                PT.append(pt)
            if DBG == 6:
                return
            P.tag = "ml.B.den"
            dbank = self.banks.get()
            for lb in range(4):
                for kc in range(4):
                    c = 4 * h + kc
                    P.mm(dbank[:, lb:lb + 1], Q[c][:, lb * 128:(lb + 1) * 128], NB[:, c:c + 1], start=(kc == 0), stop=(kc == 3))
            for lb in range(4):
                for sbi in range(lb + 1):
                    P.mm(dbank[:, 4 + lb:5 + lb], PT[sbi][:, lb * 128:(lb + 1) * 128], self.ONESB[:, 0:1], start=(sbi == 0), stop=(sbi == lb))
            dn = self.small()
            P.copy(dn[:, 0:8], dbank[:, 0:8])
            self.banks.put(dbank)
            wib = TM.v(TM.t[:, 80 + h:96:4])
            P.tt(dn[:, 8:12], dn[:, 0:4], wib, ALU.mult)
            P.tt(dn[:, 8:12], dn[:, 8:12], dn[:, 4:8], ALU.add)
            P.act(dn[:, 8:12], dn[:, 8:12], AF.Abs)
            P.tt(dn[:, 8:12], dn[:, 8:12], TM.v(TM.t[:, 64 + h:80:4]), ALU.max)
            P.recip(dn[:, 8:12], dn[:, 8:12])
            P.tt(dn[:, 12:16], dn[:, 8:12], wib, ALU.mult)
            self.dump("d_dn", dn[:, 0:16], [128, 16])
            self.dump("d_pt0", PT[0][:, 0:T], [128, T], BF16)
            self.dump("d_kt0", KTM[0][:, 0:T], [128, T], BF16)
            self.dump("d_vt0", VTM[0][:, 0:T], [128, T], BF16)
            P.tag = "ml.B.num"
            HN = []
            for lb in range(4):
                b1 = self.banks.get()
                b2 = self.banks.get()
                for kc in range(4):
                    c = 4 * h + kc
                    P.mm(b1[:, 0:T], Q[c][:, lb * 128:(lb + 1) * 128], CB[kc][:, 0:T], start=(kc == 0), stop=(kc == 3))
                for sbi in range(lb + 1):
                    P.mm(b2[:, 0:T], PT[sbi][:, lb * 128:(lb + 1) * 128], VTM[sbi][:, 0:T], start=(sbi == 0), stop=(sbi == lb))
                hu = self.fpool.get()
                P.act(hu[:, 0:T], b2[:, 0:T], AF.Identity, scale=dn[:, 8 + lb:9 + lb])
                self.banks.put(b2)
                P.stt(hu[:, 0:T], b1[:, 0:T], dn[:, 12 + lb:13 + lb], hu[:, 0:T], ALU.mult, ALU.add)
                self.banks.put(b1)
                st = self.small()
                P.bn_stats(st[:, 0:6], hu[:, 0:T])
                P.bn_aggr(st[:, 6:8], st[:, 0:6])
                P.ts(st[:, 8:9], st[:, 7:8], EPS, None, ALU.add)
                P.act(st[:, 8:9], st[:, 8:9], AF.Sqrt)
                P.recip(st[:, 9:10], st[:, 8:9])
                P.stt(st[:, 10:11], st[:, 6:7], -1.0, st[:, 9:10], ALU.mult, ALU.mult)
                hn = self.bpool.get()
                P.act(hn[:, 0:T], hu[:, 0:T], AF.Identity, bias=st[:, 10:11], scale=st[:, 9:10])
                self.fpool.put(hu)
                HN.append(hn)
                if lb == 0:
                    self.dump("d_hn0", hn[:, 0:T], [128, T], BF16)
                    self.dump("d_st0", st[:, 0:12], [128, 12])
            self.bpool.put(*PT)
            self.bpool.put(*CB)
            if DBG == 7:
                return
            P.tag = "ml.B.z"
            SZ = []
            for j in range(4):
                c = 4 * h + j
                if j % 2 == 0:
                    upslot = self.wload(f"mup{l}", 8 + c // 2, 2048)
                    upw = upslot.t[:, :].rearrange("p (k n) -> p k n", k=DC)
                bank = self.banks.get()
                for kc in range(DC):
                    P.mm(bank[:, 0:T], upslot.v(upw[:, kc, (j % 2) * 128:(j % 2 + 1) * 128]), H[kc][:, 0:T],
                         start=(kc == 0), stop=(kc == DC - 1))
                sz = self.bpool.get()
                P.act(sz[:, 0:T], bank[:, 0:T], AF.Silu)
                self.banks.put(bank)
                SZ.append(sz)
            if DBG == 9:
                return
            P.tag = "ml.B.upd"
            for kc in range(4):
                ub = self.banks.get()
                for sbi in range(4):
                    P.mm(ub[:, 0:T], KTM[sbi][:, kc * 128:(kc + 1) * 128], VTM[sbi][:, 0:T], start=(sbi == 0), stop=(sbi == 3))
                P.stt(C32[kc][:, 0:T], C32[kc][:, 0:T], WCB(h), ub[:, 0:T], ALU.mult, ALU.add)
                self.banks.put(ub)
                if ti < self.nt - 1:
                    P.dma(cst[kc, :, :], C32[kc][:, 0:T], eng="sp")
            self.fpool.put(*C32)
            nbk = self.banks.get()
            for kc in range(4):
                for sbi in range(4):
                    P.mm(nbk[:, kc:kc + 1], KTM[sbi][:, kc * 128:(kc + 1) * 128], self.ONESB[:, 0:1], start=(sbi == 0), stop=(sbi == 3))
            P.stt(nst[:, 4 * h:4 * h + 4], nst[:, 4 * h:4 * h + 4], WCB(h), nbk[:, 0:4], ALU.mult, ALU.add)
            self.banks.put(nbk)
            self.bpool.put(*KTM)
            self.bpool.put(*VTM)
            if DBG == 8:
                return
            P.tag = "ml.B.tr"
            for j in range(4):
                c = 4 * h + j
                tbk = self.banks.get()
                tv = tbk.t[:, :].bitcast(BF16)
                for lb in range(4):
                    P.transpose(tbk.v(tv[:, lb * 128:(lb + 1) * 128]), HN[lb][:, j * 128:(j + 1) * 128], self.IDB[:, :])
                skx = self.fpool.get()
                P.act(skx[:, 0:T], XC[c][:, 0:T], AF.Identity, scale=self.pcol(f"mskip{l}", c))
                P.stt(skx[:, 0:T], tbk.v(tv[:, 0:T]), self.pcol(f"mlnw{l}", c), skx[:, 0:T], ALU.mult, ALU.add)
                self.banks.put(tbk)
                P.tt(Q[c][:, 0:T], skx[:, 0:T], SZ[j][:, 0:T], ALU.mult)
                self.fpool.put(skx)
            self.bpool.put(*HN)
            self.bpool.put(*SZ)
        self.bpool.put(*H)
        self.bpool.put(*XM)
        self.bpool.put(*XC)
        self.bpool.put(*KF)
        P.tag = "ml.down"
        for dc in range(DC):
            slot = self.wload(f"mdn{l}", dc, 2048)
            w = slot.t[:, :].rearrange("p (k n) -> p k n", k=IC)
            bank = self.banks.get()
            for kc in range(IC):
                P.mm(bank[:, 0:T], slot.v(w[:, kc, :]), Q[kc][:, 0:T], start=(kc == 0), stop=(kc == IC - 1))
            P.stt(self.X[:, dc, :], bank[:, 0:T], self.MOD[l][:, 16 + dc:17 + dc], self.X[:, dc, :], ALU.mult, ALU.add)
            self.banks.put(bank)
        self.bpool.put(*Q)

    _sb_i = 0

    def small_big(self):
        if not hasattr(self, "_tm"):
            self._tm = [self.sb(f"TM{i}", [128, 96], F32) for i in range(2)]
        t = self._tm[Builder._sb_i % 2]
        Builder._sb_i += 1
        return t

    def small_bf(self):
        if not hasattr(self, "_nb"):
            self._nb = [self.sb(f"NBF{i}", [128, 16], BF16) for i in range(2)]
            self._nbi = 0
        t = self._nb[self._nbi % 2]
        self._nbi += 1
        return t

    def build(self):
        P = self.P
        self.prologue()
        for ti in range(self.nt):
            P.dma(self.X[:, :, :], self.x_d[:, :, ti * T:(ti + 1) * T], eng="sp")
            for l in self.layers:
                if ti == 0:
                    self.ada(l)
                if l % 2 == 0:
                    self.mlstm(l, ti)
                else:
                    self.conformer(l, ti)
                import os
                if int(os.environ.get("MDBG", "99")) >= 99:
                    self.ffn(l, ti)
            if self.final_norm:
                r = self.sumsq()
                for dc in range(DC):
                    P.tt(self.X[:, dc, :], self.X[:, dc, :], r[:, 0:T], ALU.mult)
                    P.ts(self.X[:, dc, :], self.X[:, dc, :], self.pcol("fing", dc), None, ALU.mult)
                self.fpool.put(r)
            P.dma(self.out_d[:, :, ti * T:(ti + 1) * T], self.X[:, :, :], eng="sp")
        import os
        P.annotate = bool(os.environ.get("MANNOT"))
        P.emit()
        self.nc.sync.wait_ge(P.sems[self.X.dsem], P.dtotal[self.X.dsem])
        if hasattr(self, "dbg_dsem"):
            self.nc.sync.wait_ge(P.sems[self.dbg_dsem], P.dtotal[self.dbg_dsem])
        return self.nc


def run(inputs, nt=S_FULL // T, layers=tuple(range(DEPTH)), final_norm=True, ncores=8, trace=False):
    inp = {k: np.asarray(v) for k, v in inputs.items()}
    shared, pvoff, pvn = host_prep(inp, layers)
    S = nt * T
    b = Builder(nt, list(layers), {k: v.shape for k, v in shared.items()}, pvoff, pvn, final_norm=final_norm)
    nc = b.build()
    in_maps = []
    for core in range(ncores):
        xb = inp["x"][core, :S, :]
        xt = np.ascontiguousarray(xb.T.reshape(DC, 128, S).transpose(1, 0, 2))
        ct = np.ascontiguousarray(inp["c"][core].reshape(DC, 128).T)
        m = {"x": xt, "c": ct}
        m.update(shared)
        in_maps.append(m)
    res = run_bass_kernel_spmd(nc, in_maps, core_ids=list(range(ncores)), trace=trace)
    outs = []
    for core in range(ncores):
        o = res.results[core]["out"]
        outs.append(np.ascontiguousarray(o.transpose(1, 0, 2).reshape(D, S).T))
    return np.stack(outs, axis=0).astype(np.float32), res, b


def kernel(**inputs):
    out, _, _ = run(inputs)
    return out
```

```python
import collections
import numpy as np
import concourse.bass as bass
import concourse.mybir as mybir
from concourse.bass_utils import run_bass_kernel_spmd

F32 = mybir.dt.float32
BF16 = mybir.dt.bfloat16
AF = mybir.ActivationFunctionType
ALU = mybir.AluOpType
AX = mybir.AxisListType

D = 1024
DC = 8
S_FULL = 4096
T = 512
INNER = 2048
IC = 16
NH = 4
DFF = 2816
FC = 22
CONF_K = 31
EPS = 1e-6
DEPTH = 4
SLOT = 2048
NRING = 8
HB = 8
HF = 32


class Dep:
    __slots__ = ("name", "last_w", "readers")

    def __init__(self, name):
        self.name = name
        self.last_w = None
        self.readers = {}


class View:
    __slots__ = ("ap", "deps", "tile")

    def __init__(self, ap, deps, tile=None):
        self.ap = ap
        self.deps = deps
        self.tile = tile


class Tile:
    def __init__(self, name, tensor, nparts=1):
        self.name = name
        self.t = tensor
        self.nparts = nparts
        self.deps = [Dep(f"{name}.{i}") for i in range(nparts)]
        self.dsem = None

    def __getitem__(self, key):
        ap = self.t[key]
        if self.nparts == 1:
            return View(ap, self.deps, self)
        k = key[1] if isinstance(key, tuple) and len(key) > 1 else slice(None)
        if isinstance(k, int):
            d = [self.deps[k]]
        else:
            d = list(self.deps[k])
        return View(ap, d, self)

    def v(self, ap, parts=None):
        return View(ap, self.deps if parts is None else [self.deps[p] for p in parts], self)


class Op:
    __slots__ = ("eng", "fn", "deps", "signal", "sigval", "dsem", "dval", "eidx", "tag")

    def __init__(self, eng, fn, dsem=None):
        self.eng = eng
        self.fn = fn
        self.deps = []
        self.signal = False
        self.sigval = None
        self.dsem = dsem
        self.dval = None


ENGS = ("pe", "act", "dve", "pool", "sp")


class Prog:
    def __init__(self, nc):
        self.nc = nc
        self.ops = []
        self.eng_objs = {"pe": nc.tensor, "act": nc.scalar, "dve": nc.vector,
                         "pool": nc.gpsimd, "sp": nc.sync}
        self.ecount = {e: 0 for e in ENGS}
        self.dsem_names = []
        self.group_dsems = set()
        self.tag = ""
        self.annotate = False

    def add(self, eng, fn, reads=(), writes=(), dsem=None):
        rd = []
        for v in reads:
            if isinstance(v, View):
                rd.extend(v.deps)
        wr = []
        for v in writes:
            if isinstance(v, View):
                wr.extend(v.deps)
        op = Op(eng, fn, dsem)
        op.tag = self.tag
        op.eidx = self.ecount[eng]
        self.ecount[eng] += 1
        deps = set()
        for d in rd:
            if d.last_w is not None:
                deps.add(d.last_w)
        for d in wr:
            if d.last_w is not None:
                deps.add(d.last_w)
            for r in d.readers.values():
                deps.add(r)
        deps.discard(op)
        op.deps = list(deps)
        rkey = eng if dsem is None else ("d", dsem)
        for d in rd:
            d.readers[rkey] = op
        for d in wr:
            d.last_w = op
            d.readers = {}
        self.ops.append(op)
        return op

    def new_dsem(self, name, group=False):
        self.dsem_names.append(name)
        if group:
            self.group_dsems.add(name)
        return name

    def tile_dsem(self, tile):
        if tile.dsem is None:
            tile.dsem = self.new_dsem(f"t{len(self.dsem_names)}")
        return tile.dsem

    def emit(self):
        nc = self.nc
        for op in self.ops:
            for p in op.deps:
                if p.dsem is None:
                    if p.eng == "pe" and op.eng == "pe":
                        continue
                    p.signal = True
        cnt = {e: 0 for e in ENGS}
        dcnt = {n: 0 for n in self.dsem_names}
        for op in self.ops:
            if op.dsem is not None:
                dcnt[op.dsem] += 16
                op.dval = dcnt[op.dsem]
            elif op.signal:
                cnt[op.eng] += 1
                op.sigval = cnt[op.eng]
        sems = {}
        for e in ENGS:
            sems[e] = nc.alloc_semaphore(f"s_{e}")
        for n in self.dsem_names:
            sems[n] = nc.alloc_semaphore(f"d_{n}")
        known = {e: {} for e in ENGS}
        nw = 0
        for op in self.ops:
            eo = self.eng_objs[op.eng]
            need = {}
            for p in op.deps:
                if p.dsem is not None:
                    key = p.dsem
                    val = dcnt[key] if key in self.group_dsems else p.dval
                else:
                    if p.sigval is None or (p.eng == "pe" and op.eng == "pe"):
                        continue
                    key = p.eng
                    val = p.sigval
                if need.get(key, 0) < val:
                    need[key] = val
            kn = known[op.eng]
            for key, val in need.items():
                if kn.get(key, 0) >= val:
                    continue
                eo.wait_ge(sems[key], val)
                kn[key] = val
                nw += 1
            ins = op.fn()
            if self.annotate:
                ins.annotate(op.tag)
            if op.dsem is not None:
                ins.then_inc(sems[op.dsem], 16)
            elif op.signal:
                ins.then_inc(sems[op.eng], 1)
        self.nwaits = nw
        self.sems = sems
        self.dtotal = dcnt

    def mm(self, out, lhsT, rhs, start=True, stop=True):
        nc = self.nc
        return self.add("pe", lambda: nc.tensor.matmul(out.ap, lhsT.ap, rhs.ap, start=start, stop=stop),
                        reads=[lhsT, rhs], writes=[out])

    def transpose(self, out, in_, ident):
        nc = self.nc
        return self.add("pe", lambda: nc.tensor.transpose(out.ap, in_.ap, ident.ap),
                        reads=[in_, ident], writes=[out])

    def act(self, out, in_, func, bias=None, scale=None):
        nc = self.nc
        kw = {}
        if bias is not None:
            kw["bias"] = bias.ap if isinstance(bias, View) else bias
        if scale is not None:
            kw["scale"] = scale.ap if isinstance(scale, View) else scale
        return self.add("act", lambda: nc.scalar.activation(out.ap, in_.ap, func, **kw),
                        reads=[in_, bias, scale], writes=[out])

    def _veng(self, eng):
        return self.nc.vector if eng == "dve" else self.nc.gpsimd

    def tt(self, out, in0, in1, op, eng="dve"):
        e = self._veng(eng)
        return self.add(eng, lambda: e.tensor_tensor(out.ap, in0.ap, in1.ap, op), reads=[in0, in1], writes=[out])

    def ts(self, out, in0, s1, s2, op0, op1=None, eng="dve"):
        e = self._veng(eng)
        a1 = s1.ap if isinstance(s1, View) else s1
        a2 = s2.ap if isinstance(s2, View) else s2
        if op1 is None:
            return self.add(eng, lambda: e.tensor_single_scalar(out.ap, in0.ap, a1, op0), reads=[in0, s1], writes=[out])
        return self.add(eng, lambda: e.tensor_scalar(out.ap, in0.ap, a1, a2, op0, op1), reads=[in0, s1, s2], writes=[out])

    def stt(self, out, in0, scalar, in1, op0, op1):
        nc = self.nc
        a = scalar.ap if isinstance(scalar, View) else scalar
        return self.add("dve", lambda: nc.vector.scalar_tensor_tensor(out.ap, in0.ap, a, in1.ap, op0, op1),
                        reads=[in0, scalar, in1], writes=[out])

    def copy(self, out, in_, eng="dve"):
        if eng == "act":
            nc = self.nc
            return self.add("act", lambda: nc.scalar.copy(out.ap, in_.ap), reads=[in_], writes=[out])
        e = self._veng(eng)
        return self.add(eng, lambda: e.tensor_copy(out.ap, in_.ap), reads=[in_], writes=[out])

    def memset(self, out, val, eng="dve"):
        e = self._veng(eng)
        return self.add(eng, lambda: e.memset(out.ap, val), writes=[out])

    def scan(self, out, d0, d1, init, op0, op1):
        nc = self.nc
        return self.add("dve", lambda: nc.vector.tensor_tensor_scan(out.ap, d0.ap, d1.ap, init, op0, op1),
                        reads=[d0, d1], writes=[out])

    def reduce(self, out, in_, op):
        nc = self.nc
        return self.add("dve", lambda: nc.vector.tensor_reduce(out.ap, in_.ap, AX.X, op), reads=[in_], writes=[out])

    def recip(self, out, in_):
        nc = self.nc
        return self.add("dve", lambda: nc.vector.reciprocal(out.ap, in_.ap), reads=[in_], writes=[out])

    def bn_stats(self, out, in_):
        nc = self.nc
        return self.add("dve", lambda: nc.vector.bn_stats(out.ap, in_.ap), reads=[in_], writes=[out])

    def bn_aggr(self, out, in_):
        nc = self.nc
        return self.add("dve", lambda: nc.vector.bn_aggr(out.ap, in_.ap), reads=[in_], writes=[out])

    def dma(self, out, in_, eng="sp", dsem=None):
        eo = self.eng_objs[eng]
        oap = out.ap if isinstance(out, View) else out
        iap = in_.ap if isinstance(in_, View) else in_
        if dsem is None:
            sb = out if (isinstance(out, View) and out.tile is not None and not getattr(out.tile, "is_dram", False)) else in_
            dsem = self.tile_dsem(sb.tile)
        return self.add(eng, lambda: eo.dma_start(out=oap, in_=iap), reads=[in_], writes=[out], dsem=dsem)


class Pool:
    def __init__(self, tiles):
        self.free = collections.deque(tiles)
        self.n = len(tiles)
        self.low = len(tiles)

    def get(self):
        assert self.free, "pool exhausted"
        t = self.free.popleft()
        self.low = min(self.low, len(self.free))
        return t

    def put(self, *ts):
        for t in ts:
            self.free.append(t)


def kmajor_slots(W, ncols, col_idx=None):
    K, N = W.shape
    KC = K // 128
    if col_idx is not None:
        W = W[:, col_idx]
        N = W.shape[1]
    NS = N // ncols
    A = W.reshape(KC, 128, NS, ncols).transpose(2, 1, 0, 3)
    return np.ascontiguousarray(A).reshape(NS, 128, KC * ncols)


def fm_cols(v):
    v = np.asarray(v, np.float32)
    lead = v.shape[:-1]
    C = v.shape[-1] // 128
    A = v.reshape(*lead, C, 128)
    A = np.moveaxis(A, -1, 0)
    return np.ascontiguousarray(A).reshape(128, -1)


class PV:
    def __init__(self):
        self.cols = []
        self.off = {}
        self.n = 0

    def add(self, name, arr):
        arr = np.asarray(arr, np.float32)
        assert arr.shape[0] == 128
        self.off[name] = self.n
        self.cols.append(arr)
        self.n += arr.shape[1]

    def build(self):
        return np.ascontiguousarray(np.concatenate(self.cols, axis=1))


def blockdiag_chunks(w):
    out = np.zeros((16, 128, 128), np.float32)
    wr = w.reshape(16, 32, 4, 4)
    for b in range(32):
        out[:, 4 * b:4 * b + 4, 4 * b:4 * b + 4] = wr[:, b]
    return out


def host_prep(inp, layers):
    pv = PV()
    shared = {}
    ffn_cols = np.concatenate([np.concatenate([np.arange(j * 128, (j + 1) * 128),
                                               DFF + np.arange(j * 128, (j + 1) * 128)]) for j in range(FC)])
    conf_cols = np.concatenate([np.concatenate([np.arange(j * 128, (j + 1) * 128),
                                                D + np.arange(j * 128, (j + 1) * 128)]) for j in range(DC)])
    for l in layers:
        pv.add(f"adab{l}", fm_cols(inp["ada_b"][l]))
        pv.add(f"gmix{l}", fm_cols(inp["norm_mix_g"][l]))
        pv.add(f"gffn{l}", fm_cols(inp["norm_ffn_g"][l]))
        shared[f"ada{l}"] = kmajor_slots(inp["ada_w"][l], 256)
        shared[f"fup{l}"] = kmajor_slots(inp["ffn_w_up"][l], 256, ffn_cols)
        fd = inp["ffn_w_down"][l]
        A = fd.reshape(2, 11, 128, DC, 128).transpose(3, 0, 2, 1, 4)
        shared[f"fdn{l}"] = np.ascontiguousarray(A).reshape(16, 128, 11 * 128)
        cw = inp["ffn_conv_w"][l]
        cwg = fm_cols(cw[:, :DFF])
        cwv = fm_cols(cw[:, DFF:])
        pv.add(f"fcwg{l}", cwg)
        pv.add(f"fcwv{l}", cwv)
        pv.add(f"fcbg{l}", fm_cols(inp["ffn_conv_b"][l][:DFF]))
        pv.add(f"fcbv{l}", fm_cols(inp["ffn_conv_b"][l][DFF:]))
        j = l // 2
        if l % 2 == 0:
            shared[f"mup{l}"] = kmajor_slots(inp["mlstm_w_up"][j], 256)
            shared[f"mdn{l}"] = kmajor_slots(inp["mlstm_w_down"][j], 128)
            bq = blockdiag_chunks(inp["mlstm_wq"][j])
            bk = blockdiag_chunks(inp["mlstm_wk"][j])
            bv = blockdiag_chunks(inp["mlstm_wv"][j])
            B = np.stack([bq, bk, bv], axis=1)
            B = B.reshape(4, 4, 3, 128, 128).transpose(0, 3, 1, 2, 4)
            shared[f"wbd{l}"] = np.ascontiguousarray(B).reshape(4, 128, 4 * 3 * 128)
            wif = inp["mlstm_w_if"][j]
            W = np.zeros((48, 128, 36), np.float32)
            W[:, :, 0:4] = wif[:, 0:4].reshape(48, 128, 4)
            W[:, :, 32:36] = wif[:, 4:8].reshape(48, 128, 4)
            shared[f"wif{l}"] = np.ascontiguousarray(W.transpose(1, 0, 2)).reshape(1, 128, 48 * 36)
            pv.add(f"mcw{l}", fm_cols(inp["mlstm_conv_w"][j]))
            pv.add(f"mcb{l}", fm_cols(inp["mlstm_conv_b"][j]))
            pv.add(f"mlnw{l}", fm_cols(inp["mlstm_ln_w"][j]))
            pv.add(f"mskip{l}", fm_cols(inp["mlstm_skip"][j]))
            bif = np.zeros((128, 2), np.float32)
            bif[0:4, 0] = inp["mlstm_b_if"][j][0:4]
            bif[0:4, 1] = inp["mlstm_b_if"][j][4:8]
            pv.add(f"mbif{l}", bif)
        else:
            shared[f"cp1{l}"] = kmajor_slots(inp["conf_w_pw1"][j], 256, conf_cols)
            shared[f"cp2{l}"] = kmajor_slots(inp["conf_w_pw2"][j], 256)
            pv.add(f"cb1a{l}", fm_cols(inp["conf_b_pw1"][j][:D]))
            pv.add(f"cb1g{l}", fm_cols(inp["conf_b_pw1"][j][D:]))
            pv.add(f"cdw{l}", fm_cols(inp["conf_dw_w"][j]))
            pv.add(f"cdb{l}", fm_cols(inp["conf_dw_b"][j]))
            pv.add(f"clg{l}", fm_cols(inp["conf_ln_g"][j]))
            pv.add(f"clb{l}", fm_cols(inp["conf_ln_b"][j]))
            pv.add(f"cb2{l}", fm_cols(inp["conf_b_pw2"][j]))
    pv.add("fing", fm_cols(inp["final_g"]))
    ident = np.eye(128, dtype=np.float32)
    mask = np.triu(np.ones((128, 128), np.float32))
    self_ = np.zeros((128, 4), np.float32)
    for jj in range(4):
        self_[32 + jj, jj] = 1.0
    shared["consts"] = np.ascontiguousarray(np.concatenate([ident, mask, self_], axis=1))
    shared["pvec"] = pv.build()
    return shared, pv.off, pv.n


class Builder:
    def __init__(self, nt, layers, shared_shapes, pvoff, pvn, final_norm=True):
        self.nt = nt
        self.layers = layers
        self.final_norm = final_norm
        self.pvoff = pvoff
        nc = bass.Bass("TRN2", target_bir_lowering=False)
        self.nc = nc
        self.P = Prog(nc)
        P = self.P
        S = nt * T
        self.x_d = nc.dram_tensor("x", [128, DC, S], F32, kind="ExternalInput").ap()
        self.c_d = nc.dram_tensor("c", [128, DC], F32, kind="ExternalInput").ap()
        self.out_d = nc.dram_tensor("out", [128, DC, S], F32, kind="ExternalOutput").ap()
        self.w_d = {}
        for name, shp in shared_shapes.items():
            self.w_d[name] = nc.dram_tensor(name, list(shp), F32, kind="ExternalInput").ap()

        def sb(name, shape, dt=F32, nparts=1):
            return Tile(name, nc.alloc_sbuf_tensor(name, shape, dt), nparts)

        self.sb = sb
        self.X = sb("X", [128, DC, T], F32, nparts=DC)
        self.PVEC = sb("PVEC", [128, pvn], F32)
        self.CONST = sb("CONST", [128, 260], F32)
        self.PVECB = sb("PVECB", [128, pvn], BF16)
        self.IDB = sb("IDB", [128, 128], BF16)
        self.ONESB = sb("ONESB", [128, 128], BF16)
        self.ONESF = sb("ONESF", [128, 512], F32)
        self.CACT = sb("CACT", [128, DC], BF16)
        self.CIN = sb("CIN", [128, DC], F32)
        self.MOD = {l: sb(f"MOD{l}", [128, 48], F32) for l in layers}
        self.AB = {l: sb(f"AB{l}", [128, 16], F32) for l in layers}
        self.HALO_F = {l: sb(f"HALOF{l}", [128, 2 * FC, 2], BF16) for l in layers}
        self.HALO_M = {l: sb(f"HALOM{l}", [128, IC, 4], BF16) for l in layers if l % 2 == 0}
        self.HALO_C = {l: sb(f"HALOC{l}", [128, DC, 30], BF16) for l in layers if l % 2 == 1}
        self.NST = {l: sb(f"NST{l}", [128, IC], F32) for l in layers if l % 2 == 0}
        self.MST = {l: sb(f"MST{l}", [4, 1], F32) for l in layers if l % 2 == 0}
        self.WIF = sb("WIF", [128, 48 * 36], BF16)
        self.MASKK = sb("MASKK", [128, 128], F32)
        self.CST = {}
        for l in layers:
            if l % 2 == 0:
                for h in range(NH):
                    t = Tile(f"CST{l}_{h}", nc.dram_tensor(f"cst{l}_{h}", [4, 128, T], F32, kind="Internal"))
                    t.is_dram = True
                    self.CST[(l, h)] = t
        self.RING = [sb(f"RING{i}", [128, SLOT], BF16) for i in range(NRING)]
        self.ring_i = 0
        NB, NF = 100, 14
        self.bpool = Pool([sb(f"B{i}", [128, HB + T], BF16) for i in range(NB)])
        self.fpool = Pool([sb(f"F{i}", [128, HF + T], F32) for i in range(NF)])
        self.banks = Pool([Tile(f"PS{i}", nc.alloc_psum_tensor(f"PS{i}", [128, 512], F32)) for i in range(8)])
        self.small_i = 0
        self.out_tiles = set()
        self.smalls = [sb(f"SMALL{i}", [128, 16], F32) for i in range(24)]

    def dump(self, name, view, shape, dt=F32):
        import os
        if not os.environ.get("MDUMP"):
            return
        if not hasattr(self, "dbg_dsem"):
            self.dbg_dsem = self.P.new_dsem("dbg", group=True)
            self.dumps = []
        if name in self.dumps:
            return
        self.dumps.append(name)
        d = self.nc.dram_tensor(name, list(shape), dt, kind="ExternalOutput").ap()
        self.P.dma(d, view, eng="sp", dsem=self.dbg_dsem)

    def small(self):
        t = self.smalls[self.small_i % len(self.smalls)]
        self.small_i += 1
        return t

    def pcol(self, name, c0, n=1):
        o = self.pvoff[name] + c0
        return self.PVEC[:, o:o + n]

    def wload(self, name, s, nelem):
        slot = self.RING[self.ring_i % NRING]
        self.ring_i += 1
        self.P.dma(slot[:, 0:nelem], self.w_d[name][s, :, :], eng="pool")
        return slot

    def prologue(self):
        P = self.P
        P.dma(self.PVEC[:, :], self.w_d["pvec"], eng="sp")
        P.dma(self.CONST[:, :], self.w_d["consts"], eng="sp")
        P.dma(self.CIN[:, :], self.c_d, eng="sp")
        P.copy(self.IDB[:, :], self.CONST[:, 0:128])
        P.copy(self.PVECB[:, :], self.PVEC[:, :])
        P.ts(self.MASKK[:, :], self.CONST[:, 128:256], float(512 ** -0.5), None, ALU.mult)
        P.memset(self.ONESB[:, :], 1.0)
        P.memset(self.ONESF[:, :], 1.0, eng="pool")
        P.act(self.CACT[:, :], self.CIN[:, :], AF.Silu)
        for l in self.layers:
            P.memset(self.HALO_F[l][:, :, :], 0.0, eng="pool")
            if l % 2 == 0:
                P.memset(self.HALO_M[l][:, :, :], 0.0, eng="pool")
                P.memset(self.NST[l][:, :], 0.0, eng="pool")
                P.memset(self.MST[l][:, :], 0.0, eng="pool")
            else:
                P.memset(self.HALO_C[l][:, :, :], 0.0, eng="pool")

    def ada(self, l):
        P = self.P
        bank = self.banks.get()
        for s in range(24):
            slot = self.wload(f"ada{l}", s, 2048)
            w = slot.t[:, :].rearrange("p (k n) -> p k n", k=DC)
            for jj in range(2):
                oc = 2 * s + jj
                for kc in range(DC):
                    P.mm(bank[:, oc:oc + 1], slot.v(w[:, kc, jj * 128:(jj + 1) * 128]), self.CACT[:, kc:kc + 1],
                         start=(kc == 0), stop=(kc == DC - 1))
        M = self.MOD[l]
        P.tt(M[:, :], bank[:, 0:48], self.pcol(f"adab{l}", 0, 48), ALU.add)
        self.banks.put(bank)
        AB = self.AB[l]
        P.stt(AB[:, 0:8], M[:, 8:16], 1.0, self.pcol(f"gmix{l}", 0, 8), ALU.add, ALU.mult)
        P.stt(AB[:, 8:16], M[:, 32:40], 1.0, self.pcol(f"gffn{l}", 0, 8), ALU.add, ALU.mult)

    def rstd_from_bank(self, bank):
        P = self.P
        r = self.fpool.get()
        P.ts(r[:, 0:T], bank[:, 0:T], 1.0 / D, EPS, ALU.mult, ALU.add)
        P.act(r[:, 0:T], r[:, 0:T], AF.Sqrt)
        P.recip(r[:, 0:T], r[:, 0:T])
        return r

    ssbank = None

    ss_pending = None

    def ss_flush(self):
        if self.ss_pending is not None:
            dc, sq = self.ss_pending
            self.P.mm(self.ssbank[:, 0:T], self.ONESB[:, :], sq[:, 0:T], start=(dc == 0), stop=(dc == DC - 1))
            self.bpool.put(sq)
            self.ss_pending = None

    def ss_partial(self, dc):
        P = self.P
        if dc == 0:
            self.ssbank = self.banks.get()
        self.ss_flush()
        sq = self.bpool.get()
        P.act(sq[:, 0:T], self.X[:, dc, :], AF.Square)
        self.ss_pending = (dc, sq)

    def sumsq(self):
        P = self.P
        if self.ssbank is not None:
            self.ss_flush()
            bank = self.ssbank
            self.ssbank = None
            r = self.rstd_from_bank(bank)
            self.banks.put(bank)
            return r
        bank = self.banks.get()
        for dc in range(DC):
            sq = self.bpool.get()
            P.act(sq[:, 0:T], self.X[:, dc, :], AF.Square)
            P.mm(bank[:, 0:T], self.ONESB[:, :], sq[:, 0:T], start=(dc == 0), stop=(dc == DC - 1))
            self.bpool.put(sq)
        r = self.rstd_from_bank(bank)
        self.banks.put(bank)
        return r

    def norm_mod(self, l, which):
        P = self.P
        P.tag = "norm"
        r = self.sumsq()
        a0 = 0 if which == 0 else 8
        sh0 = 0 if which == 0 else 24
        H = []
        for dc in range(DC):
            tmp = self.fpool.get()
            P.tt(tmp[:, 0:T], self.X[:, dc, :], r[:, 0:T], ALU.mult)
            h = self.bpool.get()
            P.act(h[:, 0:T], tmp[:, 0:T], AF.Identity, bias=self.MOD[l][:, sh0 + dc:sh0 + dc + 1],
                  scale=self.AB[l][:, a0 + dc:a0 + dc + 1])
            self.fpool.put(tmp)
            H.append(h)
        self.fpool.put(r)
        return H


    def conv_build(self, wname, c0, cstep, n):
        P = self.P
        nc = self.nc
        idb = self.IDB.t[:, :]
        slabs = []
        for k0 in range(0, n, 4):
            K = min(4, n - k0)
            dslab = self.bpool.get()
            slabs.append(dslab)
            out3 = dslab.t[:, 0:K * 128].rearrange("p (k m) -> p k m", k=K)
            in0 = bass.AP(idb.tensor, idb.offset, [list(idb.ap[0]), [0, K], [1, 128]])
            col = self.pvoff[wname] + c0 + k0 * cstep
            wv = self.PVECB.t[:, col:col + 1]
            in1 = bass.AP(wv.tensor, wv.offset, [list(wv.ap[0]), [cstep, K], [0, 128]])
            P.add("dve", (lambda o=out3, a=in0, b_=in1: nc.vector.tensor_tensor(o, a, b_, ALU.mult)),
                  reads=[self.IDB[:, :], self.PVECB[:, :]], writes=[dslab[:, :]])
        return slabs

    def conv_mm(self, bank, slabs, rhs_list):
        P = self.P
        n = len(rhs_list)
        for k, rhs in enumerate(rhs_list):
            dslab = slabs[k // 4]
            dg = dslab[:, (k % 4) * 128:(k % 4 + 1) * 128]
            P.mm(bank[:, 0:T], dg, rhs, start=(k == 0), stop=(k == n - 1))
        self.bpool.put(*slabs)

    def ffn(self, l, ti):
        P = self.P
        H = self.norm_mod(l, 1)
        P.tag = "ffn.up"
        HID = []
        halo = self.HALO_F[l]
        def stage1(j):
            dgs = [self.conv_build(f"{nm}{l}", j, FC, 3) for nm in ("fcwg", "fcwv")]
            slot = self.wload(f"fup{l}", j, 2048)
            w = slot.t[:, :].rearrange("p (k n) -> p k n", k=DC)
            us = []
            for gv in range(2):
                bank = self.banks.get()
                for kc in range(DC):
                    P.mm(bank[:, 0:T], slot.v(w[:, kc, gv * 128:(gv + 1) * 128]), H[kc][:, 0:T],
                         start=(kc == 0), stop=(kc == DC - 1))
                u = self.bpool.get()
                hj = 2 * j + gv
                P.copy(u[:, 0:2], halo[:, hj, :], eng="act")
                P.copy(u[:, 2:2 + T], bank[:, 0:T], eng="act")
                self.banks.put(bank)
                P.copy(halo[:, hj, :], u[:, T:T + 2])
                us.append(u)
            return us, dgs

        def stage2(j, us, dgs):
            cb = []
            for gv in range(2):
                nm = "fcwg" if gv == 0 else "fcwv"
                u = us[gv]
                bank = self.banks.get()
                self.conv_mm(bank, dgs[gv], [u[:, k:k + T] for k in range(3)])
                cb.append(bank)
            self.bpool.put(*us)
            sg = self.fpool.get()
            P.act(sg[:, 0:T], cb[0][:, 0:T], AF.Silu, bias=self.pcol(f"fcbg{l}", j))
            self.banks.put(cb[0])
            hid = self.bpool.get()
            P.stt(hid[:, 0:T], cb[1][:, 0:T], self.pcol(f"fcbv{l}", j), sg[:, 0:T], ALU.add, ALU.mult)
            self.banks.put(cb[1])
            self.fpool.put(sg)
            HID.append(hid)

        prev = None
        for j in range(FC):
            us, dgs = stage1(j)
            if prev is not None:
                stage2(*prev)
            prev = (j, us, dgs)
        stage2(*prev)
        self.bpool.put(*H)
        P.tag = "ffn.down"
        for dc in range(DC):
            bank = self.banks.get()
            for half in range(2):
                slot = self.wload(f"fdn{l}", dc * 2 + half, 11 * 128)
                w = slot.t[:, 0:11 * 128].rearrange("p (k n) -> p k n", k=11)
                for k in range(11):
                    kc = half * 11 + k
                    P.mm(bank[:, 0:T], slot.v(w[:, k, :]), HID[kc][:, 0:T], start=(kc == 0), stop=(kc == FC - 1))
            P.stt(self.X[:, dc, :], bank[:, 0:T], self.MOD[l][:, 40 + dc:41 + dc], self.X[:, dc, :], ALU.mult, ALU.add)
            self.banks.put(bank)
            self.ss_partial(dc)
        self.bpool.put(*HID)

    def conformer(self, l, ti):
        P = self.P
        H = self.norm_mod(l, 0)
        P.tag = "conf.pw1conv"
        halo = self.HALO_C[l]
        ACC = []
        mbank = self.banks.get()
        sbank = self.banks.get()
        S = {}

        def s1(j):
            cdg = self.conv_build(f"cdw{l}", j, DC, CONF_K)
            slot = self.wload(f"cp1{l}", j, 2048)
            w = slot.t[:, :].rearrange("p (k n) -> p k n", k=DC)
            ba = self.banks.get()
            bg = self.banks.get()
            for kc in range(DC):
                P.mm(ba[:, 0:T], slot.v(w[:, kc, 0:128]), H[kc][:, 0:T], start=(kc == 0), stop=(kc == DC - 1))
            for kc in range(DC):
                P.mm(bg[:, 0:T], slot.v(w[:, kc, 128:256]), H[kc][:, 0:T], start=(kc == 0), stop=(kc == DC - 1))
            sig = self.fpool.get()
            P.act(sig[:, 0:T], bg[:, 0:T], AF.Sigmoid, bias=self.pcol(f"cb1g{l}", j))
            self.banks.put(bg)
            uf = self.fpool.get()
            ub16 = uf.t[:, :].bitcast(BF16)
            P.copy(uf.v(ub16[:, 2:32]), halo[:, j, :], eng="act")
            P.stt(uf.v(ub16[:, 32:32 + T]), ba[:, 0:T], self.pcol(f"cb1a{l}", j), sig[:, 0:T], ALU.add, ALU.mult)
            self.banks.put(ba)
            self.fpool.put(sig)
            P.copy(halo[:, j, :], uf.v(ub16[:, 2 + T:32 + T]), eng="act")
            S[j] = (cdg, uf, ub16)

        def s2(j):
            cdg, uf, ub16 = S[j]
            cbank = self.banks.get()
            self.conv_mm(cbank, cdg, [uf.v(ub16[:, 2 + k:2 + k + T]) for k in range(CONF_K)])
            self.fpool.put(uf)
            acc = self.fpool.get()
            P.act(acc[:, 0:T], cbank[:, 0:T], AF.Identity, bias=self.pcol(f"cdb{l}", j))
            self.banks.put(cbank)
            ub = self.bpool.get()
            us = self.bpool.get()
            P.copy(ub[:, 0:T], acc[:, 0:T], eng="act")
            P.act(us[:, 0:T], acc[:, 0:T], AF.Square)
            ACC.append(acc)
            S[j] = (ub, us)

        def s3(j):
            ub, us = S[j]
            P.mm(mbank[:, 0:T], self.ONESB[:, :], ub[:, 0:T], start=(j == 0), stop=(j == DC - 1))
            P.mm(sbank[:, 0:T], self.ONESB[:, :], us[:, 0:T], start=(j == 0), stop=(j == DC - 1))
            self.bpool.put(ub, us)

        for i in range(DC + 2):
            if i < DC:
                s1(i)
            if 0 <= i - 1 < DC:
                s2(i - 1)
            if 0 <= i - 2 < DC:
                s3(i - 2)
        self.bpool.put(*H)
        P.tag = "conf.ln"
        mean = self.fpool.get()
        rstd = self.fpool.get()
        P.ts(mean[:, 0:T], mbank[:, 0:T], 1.0 / D, None, ALU.mult)
        P.tt(rstd[:, 0:T], mean[:, 0:T], mean[:, 0:T], ALU.mult)
        P.stt(rstd[:, 0:T], sbank[:, 0:T], 1.0 / D, rstd[:, 0:T], ALU.mult, ALU.subtract)
        P.ts(rstd[:, 0:T], rstd[:, 0:T], EPS, None, ALU.add)
        P.act(rstd[:, 0:T], rstd[:, 0:T], AF.Sqrt)
        P.recip(rstd[:, 0:T], rstd[:, 0:T])
        self.banks.put(mbank, sbank)
        Y = []
        for j in range(DC):
            acc = ACC[j]
            P.tt(acc[:, 0:T], acc[:, 0:T], mean[:, 0:T], ALU.subtract)
            P.tt(acc[:, 0:T], acc[:, 0:T], rstd[:, 0:T], ALU.mult)
            y = self.bpool.get()
            P.act(y[:, 0:T], acc[:, 0:T], AF.Silu, bias=self.pcol(f"clb{l}", j), scale=self.pcol(f"clg{l}", j))
            self.fpool.put(acc)
            Y.append(y)
        self.fpool.put(mean, rstd)
        P.tag = "conf.pw2"
        for s in range(4):
            slot = self.wload(f"cp2{l}", s, 2048)
            w = slot.t[:, :].rearrange("p (k n) -> p k n", k=DC)
            for jj in range(2):
                dc = 2 * s + jj
                bank = self.banks.get()
                for kc in range(DC):
                    P.mm(bank[:, 0:T], slot.v(w[:, kc, jj * 128:(jj + 1) * 128]), Y[kc][:, 0:T],
                         start=(kc == 0), stop=(kc == DC - 1))
                tmp = self.fpool.get()
                P.ts(tmp[:, 0:T], bank[:, 0:T], self.pcol(f"cb2{l}", dc), self.MOD[l][:, 16 + dc:17 + dc], ALU.add, ALU.mult)
                self.banks.put(bank)
                P.tt(self.X[:, dc, :], self.X[:, dc, :], tmp[:, 0:T], ALU.add)
                self.fpool.put(tmp)
                self.ss_partial(dc)
        self.bpool.put(*Y)

    def mlstm(self, l, ti):
        P = self.P
        nc = self.nc
        H = self.norm_mod(l, 0)
        P.tag = "ml.A"
        halo = self.HALO_M[l]
        gbank = self.banks.get()
        wif = self.WIF
        self.P.dma(wif[:, 0:48 * 36], self.w_d[f"wif{l}"][0, :, :], eng="pool")
        wifv = wif.t[:, 0:48 * 36].rearrange("p (c m) -> p c m", c=48)
        kscale = float(512 ** -0.5)
        wbd = None
        XM = [None] * IC
        XC = [None] * IC
        Q = [None] * IC
        KF = [None] * IC
        st = {}

        DG = [None] * IC

        def stageA(c):
            DG[c] = self.conv_build(f"mcw{l}", c, IC, 4)
            if c % 2 == 0:
                st["upslot"] = self.wload(f"mup{l}", c // 2, 2048)
            upslot = st["upslot"]
            upw = upslot.t[:, :].rearrange("p (k n) -> p k n", k=DC)
            jj = c % 2
            bank = self.banks.get()
            for kc in range(DC):
                P.mm(bank[:, 0:T], upslot.v(upw[:, kc, jj * 128:(jj + 1) * 128]), H[kc][:, 0:T],
                     start=(kc == 0), stop=(kc == DC - 1))
            xm = self.bpool.get()
            P.copy(xm[:, HB - 4:HB], halo[:, c, :], eng="act")
            P.copy(xm[:, HB:HB + T], bank[:, 0:T], eng="act")
            self.banks.put(bank)
            P.copy(halo[:, c, :], xm[:, HB + T - 4:HB + T])
            XM[c] = xm

        def stageB(c):
            xm = XM[c]
            cbank = self.banks.get()
            self.conv_mm(cbank, DG[c], [xm[:, HB - 3 + k:HB - 3 + k + T] for k in range(4)])
            xc = self.bpool.get()
            P.act(xc[:, 0:T], cbank[:, 0:T], AF.Silu, bias=self.pcol(f"mcb{l}", c))
            self.banks.put(cbank)
            XC[c] = xc

        def stageC(c):
            xm, xc = XM[c], XC[c]
            if c % 4 == 0:
                st["wbd"] = self.wload(f"wbd{l}", c // 4, 1536)
            wbd = st["wbd"]
            wbdv = wbd.t[:, 0:1536].rearrange("p (c q m) -> p c q m", c=4, q=3)
            cc = c % 4
            bq = self.banks.get()
            P.mm(bq[:, 0:T], wbd.v(wbdv[:, cc, 0, :]), xc[:, 0:T])
            bk = self.banks.get()
            P.mm(bk[:, 0:T], wbd.v(wbdv[:, cc, 1, :]), xc[:, 0:T])
            bv = self.banks.get()
            P.mm(bv[:, 0:T], wbd.v(wbdv[:, cc, 2, :]), xm[:, HB:HB + T])
            q = self.bpool.get()
            P.copy(q[:, 0:T], bq[:, 0:T], eng="act")
            self.banks.put(bq)
            kf = self.bpool.get()
            P.copy(kf[:, 0:T], bk[:, 0:T])
            self.banks.put(bk)
            vf = self.bpool.get()
            P.copy(vf[:, 0:T], bv[:, 0:T], eng="act")
            self.banks.put(bv)
            P.mm(gbank[0:36, 0:T], wif.v(wifv[:, c, :]), q[:, 0:T], start=(c == 0), stop=False)
            P.mm(gbank[0:36, 0:T], wif.v(wifv[:, 16 + c, :]), kf[:, 0:T], start=False, stop=False)
            P.mm(gbank[0:36, 0:T], wif.v(wifv[:, 32 + c, :]), vf[:, 0:T], start=False, stop=(c == IC - 1))
            self.bpool.put(vf)
            Q[c] = q
            KF[c] = kf
            if c == 0:
                self.dump("d_xm0", xm[:, HB:HB + T], [128, T], BF16)
                self.dump("d_xc0", xc[:, 0:T], [128, T], BF16)
                self.dump("d_q0", q[:, 0:T], [128, T], BF16)
                self.dump("d_k0", kf[:, 0:T], [128, T], BF16)

        for c in range(IC + 2):
            if c < IC:
                stageA(c)
            if 0 <= c - 1 < IC:
                stageB(c - 1)
            if 0 <= c - 2 < IC:
                stageC(c - 2)
        import os
        DBG = int(os.environ.get("MDBG", "99"))
        if DBG == 1:
            return
        def zproj(h):
            SZ = []
            for j in range(4):
                c = 4 * h + j
                if j % 2 == 0:
                    upslot = self.wload(f"mup{l}", 8 + c // 2, 2048)
                    upw = upslot.t[:, :].rearrange("p (k n) -> p k n", k=DC)
                bank = self.banks.get()
                for kc in range(DC):
                    P.mm(bank[:, 0:T], upslot.v(upw[:, kc, (j % 2) * 128:(j % 2 + 1) * 128]), H[kc][:, 0:T],
                         start=(kc == 0), stop=(kc == DC - 1))
                sz = self.bpool.get()
                P.act(sz[:, 0:T], bank[:, 0:T], AF.Silu)
                self.banks.put(bank)
                SZ.append(sz)
            return SZ

        P.tag = "ml.B.z"
        SZn = zproj(0)
        P.tag = "ml.gate"
        GS = self.fpool.get()
        P.copy(GS[0:36, 0:T], gbank[0:36, 0:T], eng="act")
        self.dump("d_gs", GS[0:36, 0:T], [36, T])
        P.mm(gbank[0:4, 0:T], self.CONST[0:36, 256:260], GS[0:36, 0:T])
        ipre = self.fpool.get()
        ob = self.pvoff[f"mbif{l}"]
        P.ts(ipre[0:4, 0:T], GS[0:4, 0:T], self.PVEC[0:4, ob:ob + 1], None, ALU.add)
        lf = self.fpool.get()
        P.act(lf[0:4, 0:T], gbank[0:4, 0:T], AF.Sigmoid, bias=self.PVEC[0:4, ob + 1:ob + 2])
        self.banks.put(gbank)
        self.fpool.put(GS)
        P.act(lf[0:4, 0:T], lf[0:4, 0:T], AF.Ln)
        g = self.fpool.get()
        P.scan(g[0:4, 0:T], self.ONESF[0:4, 0:T], lf[0:4, 0:T], 0.0, ALU.mult, ALU.add)
        self.fpool.put(lf)
        a = ipre
        P.tt(a[0:4, 0:T], ipre[0:4, 0:T], g[0:4, 0:T], ALU.subtract)
        sm = self.small()
        mst = self.MST[l]
        P.reduce(sm[0:4, 0:4], a.v(a.t[0:4, 0:T].rearrange("p (b t) -> p b t", b=4)), ALU.max)
        P.tt(sm[0:4, 4:5], sm[0:4, 0:1], mst[0:4, 0:1], ALU.max)
        for lb in range(1, 4):
            P.tt(sm[0:4, 4 + lb:5 + lb], sm[0:4, lb:lb + 1], sm[0:4, 3 + lb:4 + lb], ALU.max)
        P.ts(sm[0:4, 8:12], sm[0:4, 4:8], -1.0, None, ALU.mult)
        P.act(sm[0:4, 12:16], sm[0:4, 8:12], AF.Exp, bias=mst[0:4, 0:1])
        E = []
        fl = self.fpool.get()
        for lb in range(4):
            e = self.fpool.get()
            n = 128 * (lb + 1)
            P.act(e[0:4, 0:n], a[0:4, 0:n], AF.Exp, bias=sm[0:4, 8 + lb:9 + lb])
            P.act(fl[0:4, lb * 128:n], g[0:4, lb * 128:n], AF.Exp, bias=sm[0:4, 8 + lb:9 + lb], scale=-1.0)
            E.append(e)
        self.dump("d_e", E[3][0:4, 0:T], [4, T])
        self.dump("d_fl", fl[0:4, 0:T], [4, T])
        self.dump("d_g", g[0:4, 0:T], [4, T])
        self.dump("d_sm", sm[0:4, 0:16], [4, 16])
        P.tt(mst[0:4, 0:1], g[0:4, T - 1:T], sm[0:4, 7:8], ALU.add)
        self.fpool.put(g, a)
        if DBG == 2:
            return
        tb = self.banks.get()
        for lb in range(4):
            for sbi in range(lb + 1):
                c0 = (lb * 4 + sbi) * 4
                P.transpose(tb[:, c0:c0 + 4], E[lb][0:4, sbi * 128:(sbi + 1) * 128], self.CONST[0:4, 0:4])
            P.transpose(tb[:, 64 + lb * 4:68 + lb * 4], fl[0:4, lb * 128:(lb + 1) * 128], self.CONST[0:4, 0:4])
        dg = self.small()
        for lb in range(4):
            P.ts(dg[0:4, lb * 4:lb * 4 + 4], self.CONST[0:4, 0:4], sm[0:4, 12 + lb:13 + lb], None, ALU.mult)
        P.mm(tb[:, 80:96], self.ONESF[0:4, 0:128], dg[0:4, 0:16])
        TM = self.small_big()
        P.memset(TM[:, 0:64], 0.0)
        for lb in range(4):
            c0 = lb * 16
            P.copy(TM[:, c0:c0 + 4 * (lb + 1)], tb[:, c0:c0 + 4 * (lb + 1)])
        P.copy(TM[:, 64:96], tb[:, 64:96])
        self.banks.put(tb)
        self.fpool.put(fl, *E)
        self.dump("d_tm", TM[:, 0:96], [128, 96])
        ETM = lambda lb, sbi, h: TM[:, (lb * 4 + sbi) * 4 + h:(lb * 4 + sbi) * 4 + h + 1]
        WCB = lambda h: TM[:, 80 + 12 + h:80 + 13 + h]
        nst = self.NST[l]
        NB = self.small_bf()
        P.copy(NB[:, 0:16], nst[:, 0:16])
        if DBG == 3:
            return
        for h in range(NH):
            P.tag = "ml.B.ktm"
            wbd = self.wload(f"wbd{l}", h, 1536)
            wbdv = wbd.t[:, 0:1536].rearrange("p (c q m) -> p c q m", c=4, q=3)
            KTM, VTM = [], []
            for sbi in range(4):
                bk = self.banks.get()
                bv = self.banks.get()
                for j in range(4):
                    c = 4 * h + j
                    P.mm(bk[:, j * 128:(j + 1) * 128], XC[c][:, sbi * 128:(sbi + 1) * 128], wbd.v(wbdv[:, j, 1, :]))
                    P.mm(bv[:, j * 128:(j + 1) * 128], XM[c][:, HB + sbi * 128:HB + (sbi + 1) * 128], wbd.v(wbdv[:, j, 2, :]))
                kt = self.bpool.get()
                vt = self.bpool.get()
                P.ts(kt[:, 0:T], bk[:, 0:T], ETM(3, sbi, h), kscale, ALU.mult, ALU.mult)
                P.copy(vt[:, 0:T], bv[:, 0:T], eng="act")
                self.banks.put(bk, bv)
                KTM.append(kt); VTM.append(vt)
            if DBG == 4:
                return
            P.tag = "ml.B.cload"
            C32, CB = [], []
            cst = self.CST[(l, h)]
            for kc in range(4):
                c32 = self.fpool.get()
                if ti == 0:
                    P.memset(c32[:, 0:T], 0.0)
                else:
                    P.dma(c32[:, 0:T], cst[kc, :, :], eng="sp")
                cb = self.bpool.get()
                P.copy(cb[:, 0:T], c32[:, 0:T], eng="act")
                C32.append(c32); CB.append(cb)
            if DBG == 5:
                return
            P.tag = "ml.B.st"
            PT = []
            for sbi in range(4):
                bs = self.banks.get()
                n = T - sbi * 128
                for kc in range(4):
                    c = 4 * h + kc
                    P.mm(bs[:, 0:n], KF[c][:, sbi * 128:(sbi + 1) * 128], Q[c][:, sbi * 128:T], start=(kc == 0), stop=(kc == 3))
                pt = self.bpool.get()
                for lb in range(sbi, 4):
                    src = bs[:, (lb - sbi) * 128:(lb - sbi + 1) * 128]
                    dst = pt[:, lb * 128:(lb + 1) * 128]
                    if lb == sbi:
                        P.stt(dst, src, ETM(lb, sbi, h), self.MASKK[:, :], ALU.mult, ALU.mult)
                    else:
                        P.ts(dst, src, ETM(lb, sbi, h), kscale, ALU.mult, ALU.mult)
                self.banks.put(bs)
                PT.append(pt)
            if DBG == 6:
                return
            P.tag = "ml.B.den"
            dbank = self.banks.get()
            for lb in range(4):
                for kc in range(4):
                    c = 4 * h + kc
                    P.mm(dbank[:, lb:lb + 1], Q[c][:, lb * 128:(lb + 1) * 128], NB[:, c:c + 1], start=(kc == 0), stop=(kc == 3))
            for lb in range(4):
                for sbi in range(lb + 1):
                    P.mm(dbank[:, 4 + lb:5 + lb], PT[sbi][:, lb * 128:(lb + 1) * 128], self.ONESB[:, 0:1], start=(sbi == 0), stop=(sbi == lb))
            dn = self.small()
            P.copy(dn[:, 0:8], dbank[:, 0:8])
            self.banks.put(dbank)
            wib = TM.v(TM.t[:, 80 + h:96:4])
            P.tt(dn[:, 8:12], dn[:, 0:4], wib, ALU.mult)
            P.tt(dn[:, 8:12], dn[:, 8:12], dn[:, 4:8], ALU.add)
            P.act(dn[:, 8:12], dn[:, 8:12], AF.Abs)
            P.tt(dn[:, 8:12], dn[:, 8:12], TM.v(TM.t[:, 64 + h:80:4]), ALU.max)
            P.recip(dn[:, 8:12], dn[:, 8:12])
            P.tt(dn[:, 12:16], dn[:, 8:12], wib, ALU.mult)
            self.dump("d_dn", dn[:, 0:16], [128, 16])
            self.dump("d_pt0", PT[0][:, 0:T], [128, T], BF16)
            self.dump("d_kt0", KTM[0][:, 0:T], [128, T], BF16)
            self.dump("d_vt0", VTM[0][:, 0:T], [128, T], BF16)
            P.tag = "ml.B.num"
            HN = []
            for lb in range(4):
                b1 = self.banks.get()
                b2 = self.banks.get()
                for kc in range(4):
                    c = 4 * h + kc
                    P.mm(b1[:, 0:T], Q[c][:, lb * 128:(lb + 1) * 128], CB[kc][:, 0:T], start=(kc == 0), stop=(kc == 3))
                for sbi in range(lb + 1):
                    P.mm(b2[:, 0:T], PT[sbi][:, lb * 128:(lb + 1) * 128], VTM[sbi][:, 0:T], start=(sbi == 0), stop=(sbi == lb))
                hu = self.fpool.get()
                P.act(hu[:, 0:T], b2[:, 0:T], AF.Identity, scale=dn[:, 8 + lb:9 + lb])
                self.banks.put(b2)
                P.stt(hu[:, 0:T], b1[:, 0:T], dn[:, 12 + lb:13 + lb], hu[:, 0:T], ALU.mult, ALU.add)
                self.banks.put(b1)
                st = self.small()
                P.bn_stats(st[:, 0:6], hu[:, 0:T])
                P.bn_aggr(st[:, 6:8], st[:, 0:6])
                P.ts(st[:, 8:9], st[:, 7:8], EPS, None, ALU.add)
                P.act(st[:, 8:9], st[:, 8:9], AF.Sqrt)
                P.recip(st[:, 9:10], st[:, 8:9])
                P.stt(st[:, 10:11], st[:, 6:7], -1.0, st[:, 9:10], ALU.mult, ALU.mult)
                hn = self.bpool.get()
                P.act(hn[:, 0:T], hu[:, 0:T], AF.Identity, bias=st[:, 10:11], scale=st[:, 9:10])
                self.fpool.put(hu)
                HN.append(hn)
                if lb == 0:
                    self.dump("d_hn0", hn[:, 0:T], [128, T], BF16)
                    self.dump("d_st0", st[:, 0:12], [128, 12])
            self.bpool.put(*PT)
            self.bpool.put(*CB)
            if DBG == 7:
                return
            P.tag = "ml.B.z"
            SZ = SZn
            if h < NH - 1:
                SZn = zproj(h + 1)
            if DBG == 9:
                return
            P.tag = "ml.B.upd"
            for kc in range(4):
                ub = self.banks.get()
                for sbi in range(4):
                    P.mm(ub[:, 0:T], KTM[sbi][:, kc * 128:(kc + 1) * 128], VTM[sbi][:, 0:T], start=(sbi == 0), stop=(sbi == 3))
                P.stt(C32[kc][:, 0:T], C32[kc][:, 0:T], WCB(h), ub[:, 0:T], ALU.mult, ALU.add)
                self.banks.put(ub)
                if ti < self.nt - 1:
                    P.dma(cst[kc, :, :], C32[kc][:, 0:T], eng="sp")
            self.fpool.put(*C32)
            nbk = self.banks.get()
            for kc in range(4):
                for sbi in range(4):
                    P.mm(nbk[:, kc:kc + 1], KTM[sbi][:, kc * 128:(kc + 1) * 128], self.ONESB[:, 0:1], start=(sbi == 0), stop=(sbi == 3))
            P.stt(nst[:, 4 * h:4 * h + 4], nst[:, 4 * h:4 * h + 4], WCB(h), nbk[:, 0:4], ALU.mult, ALU.add)
            self.banks.put(nbk)
            self.bpool.put(*KTM)
            self.bpool.put(*VTM)
            if DBG == 8:
                return
            P.tag = "ml.B.tr"
            for j in range(4):
                c = 4 * h + j
                tbk = self.banks.get()
                tv = tbk.t[:, :].bitcast(BF16)
                for lb in range(4):
                    P.transpose(tbk.v(tv[:, lb * 128:(lb + 1) * 128]), HN[lb][:, j * 128:(j + 1) * 128], self.IDB[:, :])
                skx = self.fpool.get()
                P.act(skx[:, 0:T], XC[c][:, 0:T], AF.Identity, scale=self.pcol(f"mskip{l}", c))
                P.stt(skx[:, 0:T], tbk.v(tv[:, 0:T]), self.pcol(f"mlnw{l}", c), skx[:, 0:T], ALU.mult, ALU.add)
                self.banks.put(tbk)
                P.tt(Q[c][:, 0:T], skx[:, 0:T], SZ[j][:, 0:T], ALU.mult)
                self.fpool.put(skx)
            self.bpool.put(*HN)
            self.bpool.put(*SZ)
        self.bpool.put(*H)
        self.bpool.put(*XM)
        self.bpool.put(*XC)
        self.bpool.put(*KF)
        P.tag = "ml.down"
        for dc in range(DC):
            slot = self.wload(f"mdn{l}", dc, 2048)
            w = slot.t[:, :].rearrange("p (k n) -> p k n", k=IC)
            bank = self.banks.get()
            for kc in range(IC):
                P.mm(bank[:, 0:T], slot.v(w[:, kc, :]), Q[kc][:, 0:T], start=(kc == 0), stop=(kc == IC - 1))
            P.stt(self.X[:, dc, :], bank[:, 0:T], self.MOD[l][:, 16 + dc:17 + dc], self.X[:, dc, :], ALU.mult, ALU.add)
            self.banks.put(bank)
            self.ss_partial(dc)
        self.bpool.put(*Q)

    _sb_i = 0

    def small_big(self):
        if not hasattr(self, "_tm"):
            self._tm = [self.sb(f"TM{i}", [128, 96], F32) for i in range(2)]
        t = self._tm[Builder._sb_i % 2]
        Builder._sb_i += 1
        return t

    def small_bf(self):
        if not hasattr(self, "_nb"):
            self._nb = [self.sb(f"NBF{i}", [128, 16], BF16) for i in range(2)]
            self._nbi = 0
        t = self._nb[self._nbi % 2]
        self._nbi += 1
        return t

    def build(self):
        P = self.P
        self.prologue()
        for ti in range(self.nt):
            P.dma(self.X[:, :, :], self.x_d[:, :, ti * T:(ti + 1) * T], eng="sp")
            for l in self.layers:
                if ti == 0:
                    self.ada(l)
                if l % 2 == 0:
                    self.mlstm(l, ti)
                else:
                    self.conformer(l, ti)
                import os
                if int(os.environ.get("MDBG", "99")) >= 99:
                    self.ffn(l, ti)
            if not self.final_norm and self.ssbank is not None:
                self.ss_flush()
                self.banks.put(self.ssbank)
                self.ssbank = None
            if self.final_norm:
                r = self.sumsq()
                for dc in range(DC):
                    o = self.fpool.get()
                    P.stt(o[:, 0:T], self.X[:, dc, :], self.pcol("fing", dc), r[:, 0:T], ALU.mult, ALU.mult)
                    P.dma(self.out_d[:, dc, ti * T:(ti + 1) * T], o[:, 0:T], eng="sp")
                    self.out_tiles.add(o)
                    self.fpool.put(o)
                self.fpool.put(r)
            else:
                P.dma(self.out_d[:, :, ti * T:(ti + 1) * T], self.X[:, :, :], eng="sp")
        import os
        P.annotate = bool(os.environ.get("MANNOT"))
        P.emit()
        self.nc.sync.wait_ge(P.sems[self.X.dsem], P.dtotal[self.X.dsem])
        for o in self.out_tiles:
            self.nc.sync.wait_ge(P.sems[o.dsem], P.dtotal[o.dsem])
        if hasattr(self, "dbg_dsem"):
            self.nc.sync.wait_ge(P.sems[self.dbg_dsem], P.dtotal[self.dbg_dsem])
        return self.nc


def run(inputs, nt=S_FULL // T, layers=tuple(range(DEPTH)), final_norm=True, ncores=8, trace=False):
    inp = {k: np.asarray(v) for k, v in inputs.items()}
    shared, pvoff, pvn = host_prep(inp, layers)
    S = nt * T
    b = Builder(nt, list(layers), {k: v.shape for k, v in shared.items()}, pvoff, pvn, final_norm=final_norm)
    nc = b.build()
    in_maps = []
    for core in range(ncores):
        xb = inp["x"][core, :S, :]
        xt = np.ascontiguousarray(xb.T.reshape(DC, 128, S).transpose(1, 0, 2))
        ct = np.ascontiguousarray(inp["c"][core].reshape(DC, 128).T)
        m = {"x": xt, "c": ct}
        m.update(shared)
        in_maps.append(m)
    res = run_bass_kernel_spmd(nc, in_maps, core_ids=list(range(ncores)), trace=trace)
    outs = []
    for core in range(ncores):
        o = res.results[core]["out"]
        outs.append(np.ascontiguousarray(o.transpose(1, 0, 2).reshape(D, S).T))
    return np.stack(outs, axis=0).astype(np.float32), res, b


def kernel(**inputs):
    out, _, _ = run(inputs)
    return out
```
